# Optimizing a Trainium2 kernel written in Bass

```python
import math
import jax, jax.numpy as jnp
from jax import lax
import numpy as np

D_MODEL = 2048
BATCH = 2
SEQ = 4096
DEPTH = 2

RMS_EPS = 1e-6

GLA_WIDTH = D_MODEL // 2
GLA_HEADS = 4
GLA_KEY_WIDTH = GLA_WIDTH // 2
GLA_DK = GLA_KEY_WIDTH // GLA_HEADS
GLA_DV = GLA_WIDTH // GLA_HEADS
GLA_GATE_RANK = 16
GLA_GATE_TAU = 16.0
GLA_CHUNK = 64
GLA_SPLITS = (GLA_KEY_WIDTH, GLA_KEY_WIDTH, GLA_WIDTH, GLA_WIDTH, GLA_GATE_RANK)
GLA_COLS = sum(GLA_SPLITS)

RWKV_WIDTH = D_MODEL - GLA_WIDTH
RWKV_HEAD = 64
RWKV_HEADS = RWKV_WIDTH // RWKV_HEAD
RWKV_DECAY_LORA = max(32, int(round(1.8 * RWKV_WIDTH ** 0.5 / 32)) * 32)
RWKV_AAA_LORA = max(32, int(round(1.8 * RWKV_WIDTH ** 0.5 / 32)) * 32)
RWKV_GATE_LORA = max(32, int(round(0.6 * RWKV_WIDTH ** 0.8 / 32)) * 32)
RWKV_SPLITS = (RWKV_WIDTH, RWKV_WIDTH, RWKV_WIDTH, RWKV_DECAY_LORA, RWKV_AAA_LORA, RWKV_GATE_LORA)
RWKV_COLS = sum(RWKV_SPLITS)
RWKV_LN_EPS = RWKV_HEAD * 1e-5

MIX_IN_COLS = GLA_COLS + RWKV_COLS

MOBA_HEADS = 16
MOBA_HD = D_MODEL // MOBA_HEADS
MOBA_BLOCK = 256
MOBA_TOPK = 3
MOBA_Q_CHUNK = 16
NEG_INF = -1e30

FFN_HIDDEN = -(-8 * D_MODEL // (3 * 256)) * 256

kernel_name = 'hybrid_gla_rwkv7_moba_alibi_swiglu'


def rmsnorm(x, g):
    xf = x.astype(jnp.float32)
    y = xf * lax.rsqrt(jnp.mean(xf * xf, axis=-1, keepdims=True) + RMS_EPS)
    return (y * g.astype(jnp.float32)).astype(x.dtype)


def _split(t, sizes):
    out, start = [], 0
    for s in sizes:
        out.append(t[..., start:start + s])
        start += s
    return out


def token_shift(p):
    return jnp.pad(p, ((0, 0), (1, 0), (0, 0)))[:, :-1]


def gla_chunked(q, k, v, log_a):
    B, S, H, DK = q.shape
    DV = v.shape[-1]
    n = S // GLA_CHUNK

    def blocks(t):
        return t.reshape(B, n, GLA_CHUNK, H, t.shape[-1]).transpose(0, 3, 1, 2, 4).astype(jnp.float32)

    q, k, v, log_a = blocks(q), blocks(k), blocks(v), blocks(log_a)
    q = q * (DK ** -0.5)
    b = jnp.cumsum(log_a, axis=3)
    b_last = b[:, :, :, -1:, :]
    q_dec = q * jnp.exp(b)
    k_inv = k * jnp.exp(-b)
    causal = jnp.tril(jnp.ones((GLA_CHUNK, GLA_CHUNK), dtype=bool))
    att = jnp.where(causal, jnp.einsum('bhncd,bhnsd->bhncs', q_dec, k_inv), 0.0)
    o_intra = jnp.einsum('bhncs,bhnsv->bhncv', att, v)
    d_state = jnp.einsum('bhncd,bhncv->bhndv', k * jnp.exp(b_last - b), v)
    chunk_decay = jnp.exp(b_last[:, :, :, 0, :])

    def step(state, inp):
        dec, ds = inp
        return dec[..., None] * state + ds, state

    _, s_in = lax.scan(step, jnp.zeros((B, H, DK, DV), jnp.float32),
                       (jnp.moveaxis(chunk_decay, 2, 0), jnp.moveaxis(d_state, 2, 0)))
    s_in = jnp.moveaxis(s_in, 0, 2)
    o = o_intra + jnp.einsum('bhncd,bhndv->bhncv', q_dec, s_in)
    return o.transpose(0, 2, 3, 1, 4).reshape(B, S, H, DV)


def rwkv7_scan(r, w, k, v, a, b):
    B, S, H, N = r.shape

    def step(state, inp):
        r_t, w_t, k_t, v_t, a_t, b_t = inp
        sa = jnp.einsum('bhvk,bhk->bhv', state, a_t)
        state = (state * w_t[:, :, None, :] + sa[..., None] * b_t[:, :, None, :]
                 + v_t[..., None] * k_t[:, :, None, :])
        return state, jnp.einsum('bhvk,bhk->bhv', state, r_t)

    xs = tuple(jnp.moveaxis(t, 1, 0) for t in (r, w, k, v, a, b))
    _, y = lax.scan(step, jnp.zeros((B, H, N, N), jnp.float32), xs)
    return jnp.moveaxis(y, 0, 1)


def gla_rwkv_mixer(h, w_in, gla_gate_w2, gla_gate_b, gla_norm, rwkv_mu, rwkv_w0, rwkv_w2,
                   rwkv_a0, rwkv_a2, rwkv_g2, rwkv_k_k, rwkv_k_a, rwkv_r_k, rwkv_ln_w,
                   rwkv_ln_b, w_out):
    B, S, _ = h.shape
    p = (h @ w_in).astype(jnp.float32)
    p_gla, p_rwkv = p[..., :GLA_COLS], p[..., GLA_COLS:]

    q, k, v, og, gate_low = _split(p_gla, GLA_SPLITS)
    log_a = jax.nn.log_sigmoid(gate_low @ gla_gate_w2 + gla_gate_b) / GLA_GATE_TAU
    o_gla = gla_chunked(q.reshape(B, S, GLA_HEADS, GLA_DK), k.reshape(B, S, GLA_HEADS, GLA_DK),
                        v.reshape(B, S, GLA_HEADS, GLA_DV), log_a.reshape(B, S, GLA_HEADS, GLA_DK))
    o_gla = rmsnorm(o_gla, gla_norm).reshape(B, S, GLA_WIDTH) * jax.nn.silu(og)

    p_rwkv = p_rwkv + (token_shift(p_rwkv) - p_rwkv) * rwkv_mu
    r, k, v, w_low, a_low, g_low = _split(p_rwkv, RWKV_SPLITS)
    w_log = -jax.nn.softplus(-(rwkv_w0 + jnp.tanh(w_low) @ rwkv_w2)) - 0.5
    decay = jnp.exp(-jnp.exp(w_log))
    a = jax.nn.sigmoid(rwkv_a0 + a_low @ rwkv_a2)
    g = jax.nn.sigmoid(g_low) @ rwkv_g2
    hs = (B, S, RWKV_HEADS, RWKV_HEAD)
    kk = (k * rwkv_k_k).reshape(hs)
    kk = kk / jnp.maximum(jnp.sqrt(jnp.sum(kk * kk, axis=-1, keepdims=True)), 1e-12)
    k = k * (1.0 + (a - 1.0) * rwkv_k_a)
    r_h, k_h, v_h, a_h = r.reshape(hs), k.reshape(hs), v.reshape(hs), a.reshape(hs)
    y = rwkv7_scan(r_h, decay.reshape(hs), k_h, v_h, -kk, kk * a_h)
    mu = jnp.mean(y, axis=-1, keepdims=True)
    var = jnp.mean(jnp.square(y - mu), axis=-1, keepdims=True)
    y = ((y - mu) * lax.rsqrt(var + RWKV_LN_EPS) * rwkv_ln_w.reshape(RWKV_HEADS, RWKV_HEAD)
         + rwkv_ln_b.reshape(RWKV_HEADS, RWKV_HEAD))
    bonus = jnp.sum(r_h * k_h * rwkv_r_k, axis=-1, keepdims=True) * v_h
    o_rwkv = (y + bonus).reshape(B, S, RWKV_WIDTH) * g

    out = jnp.concatenate([o_gla, o_rwkv], axis=-1) @ w_out
    return out.astype(h.dtype)


def moba_attention(h, w_qkv, w_out):
    B, S, _ = h.shape
    H, D, BS, QC = MOBA_HEADS, MOBA_HD, MOBA_BLOCK, MOBA_Q_CHUNK
    qkv = (h @ w_qkv).reshape(B, S, 3, H, D)
    q, k, v = [qkv[:, :, i].transpose(0, 2, 1, 3).astype(jnp.float32) for i in range(3)]
    nb = -(-S // BS)
    pad = nb * BS - S
    k_blk = jnp.pad(k, ((0, 0), (0, 0), (0, pad), (0, 0))).reshape(B, H, nb, BS, D)
    v_blk = jnp.pad(v, ((0, 0), (0, 0), (0, pad), (0, 0))).reshape(B, H, nb, BS, D)
    k_mean = jnp.mean(k_blk, axis=3)
    n_sel = min(MOBA_TOPK, max(nb - 1, 0))
    scale = D ** -0.5
    slopes = jnp.exp2(-8.0 * jnp.arange(1, H + 1, dtype=jnp.float32) / H)[None, :, None, None]
    bidx = jnp.arange(B)[:, None, None, None]
    hidx = jnp.arange(H)[None, :, None, None]

    def chunk(ci):
        q0 = ci * QC
        blk = q0 // BS
        qc = lax.dynamic_slice_in_dim(q, q0, QC, axis=2)
        t = q0 + jnp.arange(QC)
        k_own = lax.dynamic_index_in_dim(k_blk, blk, axis=2, keepdims=False)
        v_own = lax.dynamic_index_in_dim(v_blk, blk, axis=2, keepdims=False)
        dist_own = (t[:, None] - (blk * BS + jnp.arange(BS))[None, :]).astype(jnp.float32)
        logit_own = jnp.einsum('bhqd,bhkd->bhqk', qc, k_own) * scale - slopes * dist_own
        logit_own = jnp.where(dist_own >= 0, logit_own, NEG_INF)
        if n_sel == 0:
            probs = jax.nn.softmax(logit_own, axis=-1)
            return jnp.einsum('bhqk,bhkd->bhqd', probs, v_own)
        gate = jnp.einsum('bhqd,bhnd->bhqn', qc, k_mean)
        gate = jnp.where(jnp.arange(nb) < blk, gate, NEG_INF)
        _, sel = lax.top_k(gate, n_sel)
        valid = jnp.arange(n_sel) < blk
        k_sel = k_blk[bidx, hidx, sel]
        v_sel = v_blk[bidx, hidx, sel]
        s_sel = sel[..., None] * BS + jnp.arange(BS)
        dist_sel = (t[:, None, None] - s_sel).astype(jnp.float32)
        logit_sel = jnp.einsum('bhqd,bhqnkd->bhqnk', qc, k_sel) * scale - slopes[..., None] * dist_sel
        logit_sel = jnp.where(valid[:, None], logit_sel, NEG_INF).reshape(B, H, QC, n_sel * BS)
        probs = jax.nn.softmax(jnp.concatenate([logit_sel, logit_own], axis=-1), axis=-1)
        p_sel = probs[..., :n_sel * BS].reshape(B, H, QC, n_sel, BS)
        return (jnp.einsum('bhqnk,bhqnkd->bhqd', p_sel, v_sel)
                + jnp.einsum('bhqk,bhkd->bhqd', probs[..., n_sel * BS:], v_own))

    out = lax.map(chunk, jnp.arange(S // QC))
    out = out.transpose(1, 0, 3, 2, 4).reshape(B, S, H * D)
    return (out @ w_out).astype(h.dtype)


def swiglu(h, w_gate, w_up, w_down):
    return (jax.nn.silu(h @ w_gate) * (h @ w_up)) @ w_down


def setup_inputs(seed: int = 0) -> dict:
    key = jax.random.key(seed)
    ks = iter(jax.random.split(key, 32))
    n_even = (DEPTH + 1) // 2
    n_odd = DEPTH // 2
    D, F = D_MODEL, FFN_HIDDEN
    f32 = jnp.float32

    def nrm(shape, scale):
        return jax.random.normal(next(ks), shape, f32) * scale

    return {
        'x': nrm((BATCH, SEQ, D), 1.0),
        'norm_mix': 1.0 + nrm((DEPTH, D), 0.02),
        'norm_ffn': 1.0 + nrm((DEPTH, D), 0.02),
        'norm_final': 1.0 + nrm((D,), 0.02),
        'mix_in_w': nrm((n_even, D, MIX_IN_COLS), D ** -0.5),
        'gla_gate_w2': nrm((n_even, GLA_GATE_RANK, GLA_KEY_WIDTH), GLA_GATE_RANK ** -0.5),
        'gla_gate_b': nrm((n_even, GLA_KEY_WIDTH), 0.1),
        'gla_norm': 1.0 + nrm((n_even, GLA_DV), 0.02),
        'rwkv_mu': jax.random.uniform(next(ks), (n_even, RWKV_COLS), f32),
        'rwkv_w0': nrm((n_even, RWKV_WIDTH), 0.5),
        'rwkv_w2': nrm((n_even, RWKV_DECAY_LORA, RWKV_WIDTH), 0.1),
        'rwkv_a0': nrm((n_even, RWKV_WIDTH), 0.1),
        'rwkv_a2': nrm((n_even, RWKV_AAA_LORA, RWKV_WIDTH), 0.5 * RWKV_AAA_LORA ** -0.5),
        'rwkv_g2': nrm((n_even, RWKV_GATE_LORA, RWKV_WIDTH), RWKV_GATE_LORA ** -0.5),
        'rwkv_k_k': 0.85 + nrm((n_even, RWKV_WIDTH), 0.05),
        'rwkv_k_a': 1.0 + nrm((n_even, RWKV_WIDTH), 0.05),
        'rwkv_r_k': nrm((n_even, RWKV_HEADS, RWKV_HEAD), 0.1),
        'rwkv_ln_w': 1.0 + nrm((n_even, RWKV_WIDTH), 0.02),
        'rwkv_ln_b': nrm((n_even, RWKV_WIDTH), 0.01),
        'mix_out_w': nrm((n_even, D, D), D ** -0.5),
        'attn_qkv_w': nrm((n_odd, D, 3 * D), D ** -0.5),
        'attn_out_w': nrm((n_odd, D, D), D ** -0.5),
        'ffn_gate_w': nrm((DEPTH, D, F), D ** -0.5),
        'ffn_up_w': nrm((DEPTH, D, F), D ** -0.5),
        'ffn_down_w': nrm((DEPTH, F, D), F ** -0.5),
    }


def reference(x, norm_mix, norm_ffn, norm_final, mix_in_w, gla_gate_w2, gla_gate_b, gla_norm,
              rwkv_mu, rwkv_w0, rwkv_w2, rwkv_a0, rwkv_a2, rwkv_g2, rwkv_k_k, rwkv_k_a, rwkv_r_k,
              rwkv_ln_w, rwkv_ln_b, mix_out_w, attn_qkv_w, attn_out_w, ffn_gate_w, ffn_up_w,
              ffn_down_w):
    h = x
    for layer in range(DEPTH):
        i = layer // 2
        hn = rmsnorm(h, norm_mix[layer])
        if layer % 2 == 0:
            h = h + gla_rwkv_mixer(hn, mix_in_w[i], gla_gate_w2[i], gla_gate_b[i], gla_norm[i],
                                   rwkv_mu[i], rwkv_w0[i], rwkv_w2[i], rwkv_a0[i], rwkv_a2[i],
                                   rwkv_g2[i], rwkv_k_k[i], rwkv_k_a[i], rwkv_r_k[i],
                                   rwkv_ln_w[i], rwkv_ln_b[i], mix_out_w[i])
        else:
            h = h + moba_attention(hn, attn_qkv_w[i], attn_out_w[i])
        h = h + swiglu(rmsnorm(h, norm_ffn[layer]), ffn_gate_w[layer], ffn_up_w[layer], ffn_down_w[layer])
    return rmsnorm(h, norm_final)
```

```python
import numpy as np
from contextlib import ExitStack
import concourse.bass as bass
import concourse.mybir as mybir
from concourse.bass_utils import run_bass_kernel_spmd

F32 = mybir.dt.float32
BF16 = mybir.dt.bfloat16
AF = mybir.ActivationFunctionType
ALU = mybir.AluOpType
AX = mybir.AxisListType
ENGS = ['tensor', 'vector', 'scalar', 'gpsimd', 'sync']


class Buf:
    __slots__ = ('name', 'last_w', 'readers', 'dsem', 'dcount')

    def __init__(self, name):
        self.name = name
        self.last_w = None
        self.readers = []
        self.dsem = None
        self.dcount = 0


class Op:
    __slots__ = ('eng', 'seq', 'fn', 'waits', 'signals', 'sigidx', 'kind', 'dsem', 'dval')

    def __init__(self, eng, seq, fn, kind):
        self.eng = eng
        self.seq = seq
        self.fn = fn
        self.waits = []
        self.signals = False
        self.sigidx = None
        self.kind = kind
        self.dsem = None
        self.dval = 0


class Prog:
    def __init__(self, nc):
        self.nc = nc
        self.es = ExitStack()
        self.ops = {e: [] for e in ENGS}
        self.sem = {e: self.es.enter_context(nc.semaphore('sem_' + e)) for e in ENGS}
        self.seen = {e: {} for e in ENGS}
        self.nsem = 0
        self.ntens = 0

    def sbuf(self, shape, dt, name=None):
        self.ntens += 1
        return self.es.enter_context(self.nc.sbuf_tensor(name or f't{self.ntens}', list(shape), dt))

    def psum(self, shape, dt=F32, name=None):
        self.ntens += 1
        return self.es.enter_context(self.nc.psum_tensor(name or f'p{self.ntens}', list(shape), dt))

    def buf(self, name='b'):
        return Buf(name)

    def bufs(self, n, name='b'):
        return [Buf(f'{name}{i}') for i in range(n)]

    def _collect(self, eng, reads, writes):
        deps = []
        for b in reads:
            if b.last_w is not None:
                deps.append(b.last_w)
        for b in writes:
            if b.last_w is not None:
                deps.append(b.last_w)
            deps.extend(b.readers)
        waits = []
        seen = self.seen[eng]
        for d in deps:
            if d.kind == 'c':
                if d.eng == eng and eng == 'tensor':
                    continue
                key = ('e', d.eng)
                if seen.get(key, 0) >= d.seq:
                    continue
                seen[key] = d.seq
                d.signals = True
                waits.append(d)
            else:
                key = ('d', id(d.dsem))
                if seen.get(key, 0) >= d.dval:
                    continue
                seen[key] = d.dval
                waits.append(d)
        return waits

    def _commit(self, op, reads, writes):
        for b in reads:
            b.readers.append(op)
        for b in writes:
            b.last_w = op
            b.readers = []

    def op(self, eng, fn, reads=(), writes=()):
        o = Op(eng, len(self.ops[eng]) + 1, fn, 'c')
        o.waits = self._collect(eng, reads, writes)
        self.ops[eng].append(o)
        self._commit(o, reads, writes)
        return o

    def dma(self, q, out, in_, reads=(), writes=(), **kw):
        cb = writes[0] if writes else reads[0]
        if cb.dsem is None:
            self.nsem += 1
            cb.dsem = self.es.enter_context(self.nc.semaphore(f'd{self.nsem}'))
        o = Op(q, len(self.ops[q]) + 1, None, 'd')
        o.waits = self._collect(q, reads, writes)
        cb.dcount += 16
        o.dsem = cb.dsem
        o.dval = cb.dcount
        o.fn = lambda e, out=out, in_=in_, kw=kw: e.dma_start(out=out, in_=in_, **kw)
        self.ops[q].append(o)
        self._commit(o, reads, writes)
        return o

    def wait_all(self, eng, bufs):
        o = Op(eng, len(self.ops[eng]) + 1, None, 'w')
        reads = list(bufs)
        o.waits = self._collect(eng, reads, reads)
        self.ops[eng].append(o)
        return o

    def emit(self):
        for e in ENGS:
            c = 0
            for o in self.ops[e]:
                if o.kind == 'c' and o.signals:
                    c += 1
                    o.sigidx = c
        with self.nc.Block() as block:
            for e in ENGS:
                def body(eh, e=e):
                    for o in self.ops[e]:
                        ws = {}
                        for d in o.waits:
                            if d.kind == 'c':
                                s, v = self.sem[d.eng], d.sigidx
                            else:
                                s, v = d.dsem, d.dval
                            k = id(s)
                            if k not in ws or ws[k][1] < v:
                                ws[k] = (s, v)
                        for s, v in ws.values():
                            eh.wait_ge(s, v)
                        if o.kind == 'w':
                            continue
                        ins = o.fn(eh)
                        if o.kind == 'd':
                            ins.then_inc(o.dsem, 16)
                        elif o.signals:
                            ins.then_inc(self.sem[e], 1)
                getattr(block, e)(body)

    def close(self):
        self.es.close()

    def stats(self):
        return {e: len(self.ops[e]) for e in ENGS}


D = 2048
FF = 5632
TC = 1024
TH = 512
NIN = 6528
RMS_EPS = 1e-6


def build_row(mode):
    nc = bass.Bass("TRN2", target_bir_lowering=False)
    P = Prog(nc)

    def din(name, shape):
        return nc.dram_tensor(name, list(shape), F32, kind="ExternalInput").ap()

    def dout(name, shape):
        return nc.dram_tensor(name, list(shape), F32, kind="ExternalOutput").ap()

    hT_d = din("hT", [D, TC])
    if mode == 'L1':
        g1_d = din("g1", [128, 16])
        w1_d = din("w1", [D, NIN])
        out1_d = dout("pT", [NIN, TC])
    else:
        oT_d = din("oT", [D, TC])
        wo_d = din("wo", [D, D])
        gf_d = din("gf", [128, 16])
        wg_d = din("wg", [D, FF])
        wu_d = din("wu", [D, FF])
        wd_d = din("wd", [FF, D])
        g2_d = din("g2", [128, 16])
        if mode == 'L3':
            w2_d = din("w2", [D, 3 * D])
            out2_d = dout("qkvT", [3 * D, TC])
            hout_d = dout("hout", [D, TC])
        else:
            yout_d = dout("yT", [D, TC])

    h = P.sbuf([128, 16, TH], F32, 'h'); hb = P.bufs(16, 'h')
    xn = P.sbuf([128, 16, TH], BF16, 'xn'); xnb = P.bufs(16, 'xn')
    ones = P.sbuf([128, 128], F32, 'ones'); onesb = P.buf('ones')
    sq = [P.sbuf([128, TH], F32, f'sq{i}') for i in range(2)]; sqb = P.bufs(2, 'sq')
    rs = P.sbuf([128, TH], F32, 'rs'); rsb = P.buf('rs')
    rstd = P.sbuf([128, TH], F32, 'rstd'); rstdb = P.buf('rstd')
    NW = 3
    wbuf = [P.sbuf([128, 8192], BF16, f'wbuf{i}') for i in range(NW)]; wb = P.bufs(NW, 'w')
    ost = [P.sbuf([128, TH], F32, f'ost{i}') for i in range(3)]; ostb = P.bufs(3, 'ost')
    NPS = 6
    ps = [P.psum([128, TH], F32, f'ps{i}') for i in range(NPS)]; psb = P.bufs(NPS, 'ps')
    ssps = P.psum([128, TH], F32, 'ssps'); sspsb = P.buf('ssps')
    gt1 = P.sbuf([128, 16], F32, 'gt1'); gt1b = P.buf('gt1')
    if mode != 'L1':
        gt2 = P.sbuf([128, 16], F32, 'gt2'); gt2b = P.buf('gt2')
        obf = P.sbuf([128, 16, TH], BF16, 'obf'); obfb = P.bufs(16, 'obf')
        act = P.sbuf([128, 44, TH], BF16, 'act'); actb = P.bufs(44, 'act')
        sg = [P.sbuf([128, TH], F32, f'sg{i}') for i in range(2)]; sgb = P.bufs(2, 'sg')

    st = {'w': 0, 'ps': 0, 'ost': 0, 'sq': 0, 'sg': 0}

    def nxt(k, n):
        v = st[k] % n
        st[k] += 1
        return v

    P.op('vector', lambda e: e.memset(ones[:], 1.0), writes=[onesb])
    if mode == 'L1':
        P.dma('sync', gt1[:], g1_d, writes=[gt1b])
    else:
        P.dma('sync', gt1[:], gf_d, writes=[gt1b])
        P.dma('sync', gt2[:], g2_d, writes=[gt2b])

    def load_w(Wd_ap, KC, col0, GW):
        wi = nxt('w', NW)
        wt = wbuf[wi][:, 0:KC * GW].rearrange("p (c n) -> p c n", n=GW)
        Wv = Wd_ap.rearrange("(c p) n -> p c n", p=128)
        step = max(1, 512 // 128)
        for c0 in range(0, KC, step):
            c1 = min(KC, c0 + step)
            P.dma('gpsimd', wt[:, c0:c1, :], Wv[:, c0:c1, col0:col0 + GW], writes=[wb[wi]])
        return wt, wb[wi]

    def linear(xin, xinb, KC, Wd_ap, N, GW, consume):
        ng = (N + GW - 1) // GW
        for g in range(ng):
            gw = min(GW, N - g * GW)
            wt, wtb = load_w(Wd_ap, KC, g * GW, gw)
            for s in range(gw // 128):
                j = (g * GW) // 128 + s
                pi = nxt('ps', NPS)
                for c in range(KC):
                    P.op('tensor', lambda e, pi=pi, wt=wt, c=c, s=s: e.matmul(
                        ps[pi][:], wt[:, c, s * 128:(s + 1) * 128], xin[:, c, :], start=(c == 0), stop=(c == KC - 1)),
                        reads=[wtb, xinb[c]], writes=[psb[pi]])
                consume(j, ps[pi], psb[pi])

    def rmsnorm(gt, gtb):
        for c in range(16):
            si = nxt('sq', 2)
            P.op('scalar', lambda e, si=si, c=c: e.activation(sq[si][:], h[:, c, :], AF.Square),
                 reads=[hb[c]], writes=[sqb[si]])
            P.op('tensor', lambda e, si=si, c=c: e.matmul(ssps[:], ones[:], sq[si][:], start=(c == 0), stop=(c == 15)),
                 reads=[onesb, sqb[si]], writes=[sspsb])
        P.op('scalar', lambda e: e.activation(rs[:], ssps[:], AF.Sqrt, bias=RMS_EPS, scale=1.0 / D),
             reads=[sspsb], writes=[rsb])
        P.op('vector', lambda e: e.reciprocal(rstd[:], rs[:]), reads=[rsb], writes=[rstdb])
        for c in range(16):
            P.op('vector', lambda e, c=c: e.scalar_tensor_tensor(
                xn[:, c, :], h[:, c, :], gt[:, c:c + 1], rstd[:], op0=ALU.mult, op1=ALU.mult),
                reads=[hb[c], gtb, rstdb], writes=[xnb[c]])

    def store_chunk(dst_d, j, t0):
        def consume(jj, pst, pstb):
            oi = nxt('ost', 3)
            P.op('scalar', lambda e, oi=oi, pst=pst: e.copy(ost[oi][:], pst[:]), reads=[pstb], writes=[ostb[oi]])
            P.dma('sync', dst_d[jj * 128:(jj + 1) * 128, t0:t0 + TH], ost[oi][:], reads=[ostb[oi]])
        return consume

    for half in range(TC // TH):
        t0 = half * TH
        for c in range(16):
            P.dma('sync', h[:, c, :], hT_d[c * 128:(c + 1) * 128, t0:t0 + TH], writes=[hb[c]])
        if mode == 'L1':
            rmsnorm(gt1, gt1b)
            linear(xn, xnb, 16, w1_d, NIN, 512, store_chunk(out1_d, None, t0))
            continue
        for c in range(16):
            P.dma('gpsimd', obf[:, c, :], oT_d[c * 128:(c + 1) * 128, t0:t0 + TH], writes=[obfb[c]])

        def res_add(j, pst, pstb):
            P.op('vector', lambda e, j=j, pst=pst: e.tensor_tensor(h[:, j, :], pst[:], h[:, j, :], op=ALU.add),
                 reads=[pstb, hb[j]], writes=[hb[j]])
        linear(obf, obfb, 16, wo_d, D, 512, res_add)
        rmsnorm(gt1, gt1b)
        for g in range(FF // 512):
            wgt, wgb = load_w(wg_d, 16, g * 512, 512)
            wut, wub = load_w(wu_d, 16, g * 512, 512)
            for s in range(4):
                j = g * 4 + s
                pg = nxt('ps', NPS)
                for c in range(16):
                    P.op('tensor', lambda e, pg=pg, c=c, s=s, wgt=wgt: e.matmul(
                        ps[pg][:], wgt[:, c, s * 128:(s + 1) * 128], xn[:, c, :], start=(c == 0), stop=(c == 15)),
                        reads=[wgb, xnb[c]], writes=[psb[pg]])
                pu = nxt('ps', NPS)
                for c in range(16):
                    P.op('tensor', lambda e, pu=pu, c=c, s=s, wut=wut: e.matmul(
                        ps[pu][:], wut[:, c, s * 128:(s + 1) * 128], xn[:, c, :], start=(c == 0), stop=(c == 15)),
                        reads=[wub, xnb[c]], writes=[psb[pu]])
                si = nxt('sg', 2)
                P.op('scalar', lambda e, si=si, pg=pg: e.activation(sg[si][:], ps[pg][:], AF.Silu),
                     reads=[psb[pg]], writes=[sgb[si]])
                P.op('vector', lambda e, si=si, pu=pu, j=j: e.tensor_tensor(act[:, j, :], ps[pu][:], sg[si][:], op=ALU.mult),
                     reads=[psb[pu], sgb[si]], writes=[actb[j]])
        linear(act, actb, 44, wd_d, D, 128, res_add)
        if mode == 'L3':
            for c in range(16):
                P.dma('sync', hout_d[c * 128:(c + 1) * 128, t0:t0 + TH], h[:, c, :], reads=[hb[c]])
            rmsnorm(gt2, gt2b)
            linear(xn, xnb, 16, w2_d, 3 * D, 512, store_chunk(out2_d, None, t0))
        else:
            for c in range(16):
                si = nxt('sq', 2)
                P.op('scalar', lambda e, si=si, c=c: e.activation(sq[si][:], h[:, c, :], AF.Square),
                     reads=[hb[c]], writes=[sqb[si]])
                P.op('tensor', lambda e, si=si, c=c: e.matmul(ssps[:], ones[:], sq[si][:], start=(c == 0), stop=(c == 15)),
                     reads=[onesb, sqb[si]], writes=[sspsb])
            P.op('scalar', lambda e: e.activation(rs[:], ssps[:], AF.Sqrt, bias=RMS_EPS, scale=1.0 / D),
                 reads=[sspsb], writes=[rsb])
            P.op('vector', lambda e: e.reciprocal(rstd[:], rs[:]), reads=[rsb], writes=[rstdb])
            for c in range(16):
                oi = nxt('ost', 3)
                P.op('vector', lambda e, c=c, oi=oi: e.scalar_tensor_tensor(
                    ost[oi][:], h[:, c, :], gt2[:, c:c + 1], rstd[:], op0=ALU.mult, op1=ALU.mult),
                    reads=[hb[c], gt2b, rstdb], writes=[ostb[oi]])
                P.dma('sync', yout_d[c * 128:(c + 1) * 128, t0:t0 + TH], ost[oi][:], reads=[ostb[oi]])
    P.wait_all('sync', ostb + hb)
    P.emit()
    P.close()
    return nc


def gtab(g):
    return np.ascontiguousarray(np.asarray(g, np.float32).reshape(16, 128).T)


S = 4096
HD = 128
NH = 4
NT = S // 128
NEG = -1e30
MBIG = -30000.0


def moba_consts(head0):
    ident = np.eye(128, dtype=np.float32)
    tri = (np.arange(128)[None, :] >= np.arange(128)[:, None]).astype(np.float32)
    maskadd = np.zeros((128, 16, 16), np.float32)
    for bq in range(16):
        maskadd[:, bq, bq:] = NEG
    eneg = np.zeros((16, 16, 128), np.float32)
    for n in range(16):
        eneg[n, n, :] = MBIG
    slopes = np.exp2(-8.0 * np.arange(1, 17, dtype=np.float32) / 16)
    bias = np.zeros((128, NH, NT), np.float32)
    for hh in range(NH):
        for dl in range(NT):
            bias[:, hh, dl] = slopes[head0 + hh] * (np.arange(128) - 64 - 128 * dl)
    return {"ident": ident, "tri": tri, "maskadd": maskadd.reshape(128, 256), "eneg": eneg.reshape(16, 2048),
            "abias": bias.reshape(128, NH * NT)}


def build_moba():
    nc = bass.Bass("TRN2", target_bir_lowering=False)
    P = Prog(nc)

    def din(name, shape):
        return nc.dram_tensor(name, list(shape), F32, kind="ExternalInput").ap()

    qT_d = din("qT", [NH, HD, S])
    kT_d = din("kT", [NH, HD, S])
    v_d = din("v", [NH, S, HD])
    ident_d = din("ident", [128, 128])
    tri_d = din("tri", [128, 128])
    maskadd_d = din("maskadd", [128, 256])
    eneg_d = din("eneg", [16, 2048])
    abias_d = din("abias", [128, NH * NT])
    o_d = nc.dram_tensor("o", [NH, S, HD], F32, kind="ExternalOutput").ap()

    ident = P.sbuf([128, 128], F32, 'identsb'); identb = P.buf()
    tri = P.sbuf([128, 128], BF16, 'trisb'); trib = P.buf()
    maskadd = P.sbuf([128, 16, 16], F32, 'maskaddsb'); maskaddb = P.buf()
    eneg = P.sbuf([16, 16, 128], BF16, 'enegsb'); enegb = P.buf()
    abias = P.sbuf([128, NH, NT], F32, 'abiassb'); abiasb = P.buf()
    P.dma('sync', ident[:], ident_d, writes=[identb])
    P.dma('gpsimd', tri[:], tri_d, writes=[trib])
    P.dma('sync', maskadd[:].rearrange("p a b -> p (a b)"), maskadd_d, writes=[maskaddb])
    P.dma('gpsimd', eneg[:].rearrange("p a b -> p (a b)"), eneg_d, writes=[enegb])
    P.dma('sync', abias[:].rearrange("p a b -> p (a b)"), abias_d, writes=[abiasb])

    q32 = P.sbuf([128, S], F32, 'q32'); q32b = P.buf()
    k32 = P.sbuf([128, S], F32, 'k32'); k32b = P.buf()
    qbf = P.sbuf([128, S], BF16, 'qbf'); qbfb = P.buf()
    kbf = P.sbuf([128, S], BF16, 'kbf'); kbfb = P.buf()
    vaug = P.sbuf([128, NT, 132], BF16, 'vaug'); vaugb = P.buf()
    km = P.sbuf([128, 16], F32, 'km'); kmb = P.buf()
    km2 = P.sbuf([128, 16], F32, 'km2'); km2b = P.buf()
    gm = [P.sbuf([128, 16], F32, f'gm{i}') for i in range(2)]; gmb = P.bufs(2)
    m8 = [P.sbuf([128, 8], F32, f'm8{i}') for i in range(2)]; m8b = P.bufs(2)
    thr = [P.sbuf([128, 1], F32, f'thr{i}') for i in range(2)]; thrb = P.bufs(2)
    nsel = [P.sbuf([128, 16], F32, f'nsel{i}') for i in range(2)]; nselb = P.bufs(2)
    nselT = P.sbuf([16, S], BF16, 'nselT'); nselTb = P.bufs(NT)
    NPT = 4
    pt = [P.sbuf([128, 128], BF16, f'pt{i}') for i in range(NPT)]; ptb = P.bufs(NPT)
    rc = [P.sbuf([128, 1], F32, f'rc{i}') for i in range(2)]; rcb = P.bufs(2)
    osb = [P.sbuf([128, 128], F32, f'osb{i}') for i in range(2)]; osbb = P.bufs(2)

    sps = [P.psum([128, 4, 128], F32, f'sps{i}') for i in range(2)]; spsb = P.bufs(8)
    ops_ = [P.psum([128, 132], F32, f'ops{i}') for i in range(2)]; opsb = P.bufs(2)
    gps = P.psum([128, 16], F32, 'gps'); gpsb = P.buf()
    tps = P.psum([16, 128], F32, 'tps'); tpsb = P.buf()

    scale = float(HD) ** -0.5
    ctr = {'s': 0, 'pt': 0, 'g': 0, 'o': 0}

    def nxt(k, n):
        v = ctr[k] % n
        ctr[k] += 1
        return v

    P.op('vector', lambda e: e.memset(vaug[:, :, 128:129], 1.0), writes=[vaugb])

    for hh in range(NH):
        for c in range(8):
            P.dma('sync', q32[:, c * 512:(c + 1) * 512], qT_d[hh, :, c * 512:(c + 1) * 512], writes=[q32b])
            P.dma('sync', k32[:, c * 512:(c + 1) * 512], kT_d[hh, :, c * 512:(c + 1) * 512], writes=[k32b])
        vv = v_d[hh].rearrange("(t p) d -> p t d", p=128)
        for c in range(8):
            P.dma('gpsimd', vaug[:, c * 4:(c + 1) * 4, 0:128], vv[:, c * 4:(c + 1) * 4, :], writes=[vaugb])
        for c in range(4):
            P.op('scalar', lambda e, c=c: e.copy(qbf[:, c * 1024:(c + 1) * 1024], q32[:, c * 1024:(c + 1) * 1024]),
                 reads=[q32b], writes=[qbfb])
            P.op('vector', lambda e, c=c: e.tensor_copy(kbf[:, c * 1024:(c + 1) * 1024], k32[:, c * 1024:(c + 1) * 1024]),
                 reads=[k32b], writes=[kbfb])
        P.op('vector', lambda e: e.tensor_reduce(km[:], k32[:].rearrange("p (n s) -> p n s", s=256), axis=AX.X, op=ALU.add),
             reads=[k32b], writes=[kmb])
        P.op('vector', lambda e: e.tensor_scalar(km2[:], km[:], 1.0 / 256, None, op0=ALU.mult), reads=[kmb], writes=[km2b])
        for qi in range(2, NT):
            bq = qi // 2
            gi = nxt('g', 2)
            P.op('tensor', lambda e, qi=qi: e.matmul(gps[:], q32[:, qi * 128:(qi + 1) * 128], km2[:], start=True, stop=True),
                 reads=[q32b, km2b], writes=[gpsb])
            P.op('vector', lambda e, gi=gi, bq=bq: e.tensor_tensor(gm[gi][:], gps[:], maskadd[:, bq, :], op=ALU.add),
                 reads=[gpsb, maskaddb], writes=[gmb[gi]])
            P.op('vector', lambda e, gi=gi: e.max(m8[gi][:], gm[gi][:]), reads=[gmb[gi]], writes=[m8b[gi]])
            P.op('vector', lambda e, gi=gi: e.tensor_scalar(thr[gi][:], m8[gi][:, 2:3], -1e29, None, op0=ALU.max),
                 reads=[m8b[gi]], writes=[thrb[gi]])
            P.op('vector', lambda e, gi=gi: e.tensor_scalar(nsel[gi][:], gm[gi][:], thr[gi][:, 0:1], None, op0=ALU.is_lt),
                 reads=[gmb[gi], thrb[gi]], writes=[nselb[gi]])
            P.op('tensor', lambda e, gi=gi: e.transpose(tps[:], nsel[gi][:], ident[:]),
                 reads=[nselb[gi], identb], writes=[tpsb])
            P.op('scalar', lambda e, qi=qi: e.copy(nselT[:, qi * 128:(qi + 1) * 128], tps[:]),
                 reads=[tpsb], writes=[nselTb[qi]])
        for qi in range(NT):
            bq = qi // 2
            oi = nxt('o', 2)
            for kt in range(qi + 1):
                si = nxt('s', 8)
                spt = sps[si // 4][:, si % 4, :]
                own = kt >= 2 * bq
                P.op('tensor', lambda e, spt=spt, kt=kt, qi=qi, own=own: e.matmul(
                    spt, kbf[:, kt * 128:(kt + 1) * 128], qbf[:, qi * 128:(qi + 1) * 128], start=True, stop=own),
                    reads=[kbfb, qbfb], writes=[spsb[si]])
                if not own:
                    P.op('tensor', lambda e, spt=spt, kt=kt, qi=qi: e.matmul(
                        spt, eneg[:, kt // 2, :], nselT[:, qi * 128:(qi + 1) * 128], start=False, stop=True),
                        reads=[enegb, nselTb[qi]], writes=[spsb[si]])
                pi = nxt('pt', NPT)
                P.op('scalar', lambda e, pi=pi, spt=spt, hh=hh, dl=qi - kt: e.activation(
                    pt[pi][:], spt, AF.Exp, bias=abias[:, hh, dl:dl + 1], scale=scale),
                    reads=[spsb[si], abiasb], writes=[ptb[pi]])
                if kt == qi:
                    P.op('vector', lambda e, pi=pi: e.tensor_tensor(pt[pi][:], pt[pi][:], tri[:], op=ALU.mult),
                         reads=[ptb[pi], trib], writes=[ptb[pi]])
                P.op('tensor', lambda e, pi=pi, oi=oi, kt=kt, qi=qi: e.matmul(
                    ops_[oi][:, 0:129], pt[pi][:], vaug[:, kt, 0:129], start=(kt == 0), stop=(kt == qi)),
                    reads=[ptb[pi], vaugb], writes=[opsb[oi]])
            P.op('vector', lambda e, oi=oi: e.reciprocal(rc[oi][:], ops_[oi][:, 128:129]), reads=[opsb[oi]], writes=[rcb[oi]])
            P.op('vector', lambda e, oi=oi: e.tensor_scalar(osb[oi][:], ops_[oi][:, 0:128], rc[oi][:, 0:1], None, op0=ALU.mult),
                 reads=[opsb[oi], rcb[oi]], writes=[osbb[oi]])
            P.dma('sync', o_d[hh, qi * 128:(qi + 1) * 128, :], osb[oi][:], reads=[osbb[oi]])
    P.wait_all('sync', osbb)
    P.emit()
    P.close()
    return nc


def moba_ref(q, k, v, head0):
    out = np.zeros_like(q)
    slopes = np.exp2(-8.0 * np.arange(1, 17, dtype=np.float64) / 16)
    t = np.arange(S)
    for hh in range(q.shape[0]):
        qq, kk, vv = q[hh].astype(np.float64), k[hh].astype(np.float64), v[hh].astype(np.float64)
        kmean = kk.reshape(16, 256, HD).mean(1)
        gate = qq @ kmean.T
        blk = t // 256
        gate = np.where(np.arange(16)[None, :] < blk[:, None], gate, -np.inf)
        order = np.argsort(-gate, axis=1)[:, :3]
        selm = np.zeros((S, 16), bool)
        for r in range(3):
            valid = r < blk
            selm[t[valid], order[valid, r]] = True
        selm[t, blk] = True
        for b0 in range(16):
            ts = slice(b0 * 256, (b0 + 1) * 256)
            lg = qq[ts] @ kk[: (b0 + 1) * 256].T * HD ** -0.5
            dist = t[ts][:, None] - t[None, : (b0 + 1) * 256]
            lg = lg - slopes[head0 + hh] * dist
            m = np.repeat(selm[ts, : b0 + 1], 256, axis=1) & (dist >= 0)
            lg = np.where(m, lg, -np.inf)
            p = np.exp(lg - lg.max(1, keepdims=True))
            p /= p.sum(1, keepdims=True)
            out[hh, ts] = p @ vv[: (b0 + 1) * 256]
    return out


SEG = 256
RC = 64
GC = 128
LN_EPS = 64 * 1e-5


def mix_consts():
    ident = np.eye(128, dtype=np.float32)
    s = np.arange(64)[:, None]; t = np.arange(64)[None, :]
    m2 = np.concatenate([(t > s), (t >= s)], axis=1).astype(np.float32)
    m0 = (t < s).astype(np.float32)
    s2 = np.arange(128)[:, None]; t2 = np.arange(128)[None, :]
    gm = (t2 >= s2).astype(np.float32)
    r64 = np.ones((128, SEG), np.float32); r64[:, ::RC] = 0.0
    r128 = np.ones((128, SEG), np.float32); r128[:, ::GC] = 0.0
    return {"ident": ident, "m2": m2, "m0": m0, "gmask": gm, "rst64": r64, "rst128": r128}


def build_mix(S):
    nc = bass.Bass("TRN2", target_bir_lowering=False)
    P = Prog(nc)
    NSEG = S // SEG

    def din(name, shape):
        return nc.dram_tensor(name, list(shape), F32, kind="ExternalInput").ap()

    ident_d = din("ident", [128, 128]); m2_d = din("m2", [64, 128]); m0_d = din("m0", [64, 64])
    gmask_d = din("gmask", [128, 128]); rst64_d = din("rst64", [128, SEG]); rst128_d = din("rst128", [128, SEG])
    gq_d = din("gq", [128, S]); gk_d = din("gk", [128, S]); gv_d = din("gv", [S, 256]); gog_d = din("gog", [S, 256])
    glow_d = din("glow", [16, S]); gw2_d = din("gw2", [16, 128]); ggb_d = din("ggb", [128, 1]); gnorm_d = din("gnorm", [128, 256])
    rr_d = din("rr", [4, 64, S + 1]); rk_d = din("rk", [4, 64, S + 1]); rv_d = din("rv", [4, 64, S + 1])
    lw_d = din("lw", [64, S + 1]); la_d = din("la", [64, S + 1]); lg1_d = din("lg1", [128, S + 1]); lg2_d = din("lg2", [32, S + 1])
    hp_d = din("hp", [64, 4 * 10])
    mul_d = din("mul", [128, 4])
    w2_d = din("w2", [64, 256]); a2_d = din("a2", [64, 256]); g2a_d = din("g2a", [128, 256]); g2b_d = din("g2b", [32, 256])
    og_d = nc.dram_tensor("o_gla", [S, 256], F32, kind="ExternalOutput").ap()
    or_d = nc.dram_tensor("o_rwkv", [4, 64, S], F32, kind="ExternalOutput").ap()

    cnt = [0]

    def T(shape, dt=F32):
        cnt[0] += 1
        return P.sbuf(shape, dt, f'sb{cnt[0]}'), P.buf(f'sb{cnt[0]}')

    def const(d_ap, shape):
        t, b = T(shape)
        P.dma('sync', t[:], d_ap, writes=[b])
        return t, b

    ident, identb = const(ident_d, [128, 128])
    m2, m2b = const(m2_d, [64, 128]); m0, m0b = const(m0_d, [64, 64])
    gmask, gmaskb = const(gmask_d, [128, 128])
    rst64, rst64b = const(rst64_d, [128, SEG]); rst128, rst128b = const(rst128_d, [128, SEG])
    gw2, gw2b = const(gw2_d, [16, 128]); ggb, ggbb = const(ggb_d, [128, 1]); gnorm, gnormb = const(gnorm_d, [128, 256])
    hp, hpb = const(hp_d, [64, 40]); mul, mulb = const(mul_d, [128, 4])
    w2, w2b = const(w2_d, [64, 256]); a2, a2b = const(a2_d, [64, 256])
    g2a, g2ab = const(g2a_d, [128, 256]); g2b_, g2bb = const(g2b_d, [32, 256])
    ones64, ones64b = T([64, 64])
    P.op('vector', lambda e: e.memset(ones64[:], 1.0 / 64), writes=[ones64b])
    onesk, oneskb = T([64, 64])
    P.op('vector', lambda e: e.memset(onesk[:], 1.0), writes=[oneskb])
    omka, omkab = T([64, 4])
    for hh in range(4):
        P.op('vector', lambda e, hh=hh: e.tensor_scalar(omka[:, hh:hh + 1], hp[:, hh * 10 + 6:hh * 10 + 7], -1.0, 1.0,
                                                         op0=ALU.mult, op1=ALU.add), reads=[hpb], writes=[omkab])
    rkm = []
    for hh in range(4):
        t, b = T([64, 64])
        P.op('vector', lambda e, t=t, hh=hh: e.tensor_scalar(t[:], onesk[:], hp[:, hh * 10 + 7:hh * 10 + 8], None, op0=ALU.mult),
             reads=[oneskb, hpb], writes=[b])
        rkm.append((t, b))

    def HP(hh, i):
        return hp[:, hh * 10 + i:hh * 10 + i + 1]

    NPS = 8
    pss = [P.psum([128, 512], F32, f'ps{i}') for i in range(NPS)]; pssb = P.bufs(NPS, 'ps')
    pctr = [0]

    def PS():
        i = pctr[0] % NPS
        pctr[0] += 1
        return pss[i], pssb[i]

    engs = ['vector', 'gpsimd']
    ectr = [0]

    def EW():
        ectr[0] += 1
        return engs[ectr[0] % 2]

    def ew_tt(out, ob, a, ab, b, bb, op, eng=None):
        P.op(eng or 'vector', lambda e: e.tensor_tensor(out, a, b, op=op), reads=[ab, bb], writes=[ob])

    Sg = [T([128, 256]) for _ in range(2)]
    P.op('vector', lambda e: e.memset(Sg[0][0][:], 0.0), writes=[Sg[0][1]])
    Sr = [[T([64, 64]) for _ in range(2)] for _ in range(4)]
    for hh in range(4):
        P.op('vector', lambda e, hh=hh: e.memset(Sr[hh][0][0][:], 0.0), writes=[Sr[hh][0][1]])
    gpar = [0]
    rpar = [0, 0, 0, 0]

    def TS(p=128, w=SEG):
        return T([p, w])

    g_q = T([128, SEG]); g_k = T([128, SEG]); g_low = T([16, SEG])
    g_sig = TS(); g_la = TS(); g_cb = TS(); g_e = TS(); g_en = TS(); g_qd = TS(); g_ki = TS(); g_dec = TS(); g_kd = TS()
    g_v = [T([128, 256]) for _ in range(2)]; g_og = [T([128, 256]) for _ in range(2)]
    g_att = [T([128, 128]) for _ in range(2)]; g_kdt = [T([128, 128]) for _ in range(2)]
    g_sq = T([128, 256]); g_ss = T([128, 1]); g_rs = T([128, 1]); g_rstd = T([128, 1]); g_on = T([128, 256]); g_so = T([128, 256])
    g_out = [T([128, 256]) for _ in range(2)]

    l_w = T([64, SEG + 1]); l_a = T([64, SEG + 1]); l_g1 = T([128, SEG + 1]); l_g2 = T([32, SEG + 1])
    l_tmp = T([128, SEG]); l_wl = T([64, SEG]); l_al = T([64, SEG]); l_g1l = T([128, SEG]); l_g2l = T([32, SEG])
    l_th = T([64, SEG]); l_s1 = T([128, SEG]); l_s2 = T([32, SEG])

    class H:
        pass
    sc = H()
    sc.raw = [T([64, SEG + 1]) for _ in range(3)]
    sc.tmp = T([64, SEG]); sc.r = T([64, SEG]); sc.k = T([64, SEG])
    sc.lw = T([64, SEG]); sc.a = T([64, SEG])
    sc.kk2 = T([64, SEG]); sc.inv = T([64, SEG]); sc.kkn = T([64, SEG]); sc.t1 = T([64, SEG]); sc.kp = T([64, SEG]); sc.bb = T([64, SEG])
    sc.cb = T([64, SEG]); sc.cbx = T([64, SEG]); sc.en = T([64, SEG]); sc.ex = T([64, SEG]); sc.dec = T([64, SEG])
    pc = H()
    pc.yc = T([64, SEG]); pc.sq = T([64, SEG]); pc.rs = T([64, SEG]); pc.rstd = T([64, SEG]); pc.out = T([64, SEG])
    hs = []
    for hh in range(4):
        o = H()
        o.v = T([64, SEG]); o.g = T([64, SEG]); o.e = T([64, SEG]); o.rkr = T([64, SEG])
        o.AR = T([64, SEG // RC, 128])
        o.bt = T([64, SEG]); o.kt = T([64, SEG]); o.bd = T([64, SEG]); o.kd = T([64, SEG])
        o.y = T([64, SEG])
        o.ABm = T([64, 128]); o.AKm = T([64, 128]); o.M = [T([64, 64]) for _ in range(2)]; o.MT = [T([64, 64]) for _ in range(2)]
        o.TT = [T([64, 64]) for _ in range(2)]
        o.tok = T([64, 192])
        o.AkV = T([64, 64]); o.X = T([64, 64]); o.U = T([64, 64])
        hs.append(o)

    def act(out, ob, in_, ib, func, extra_reads=(), eng='scalar', **kw):
        P.op('scalar', lambda e: e.activation(out, in_, func, **kw), reads=[ib] + list(extra_reads), writes=[ob])

    for seg in range(NSEG):
        t0 = seg * SEG
        P.dma('sync', g_q[0][:], gq_d[:, t0:t0 + SEG], writes=[g_q[1]])
        P.dma('sync', g_k[0][:], gk_d[:, t0:t0 + SEG], writes=[g_k[1]])
        P.dma('sync', g_low[0][:], glow_d[:, t0:t0 + SEG], writes=[g_low[1]])
        pz, pzb = PS()
        P.op('tensor', lambda e, pz=pz: e.matmul(pz[:, 0:SEG], gw2[:], g_low[0][:], start=True, stop=True),
             reads=[gw2b, g_low[1]], writes=[pzb])
        act(g_sig[0][:], g_sig[1], pz[:, 0:SEG], pzb, AF.Sigmoid, extra_reads=[ggbb], bias=ggb[:, 0:1])
        act(g_la[0][:], g_la[1], g_sig[0][:], g_sig[1], AF.Ln)
        P.op('vector', lambda e: e.tensor_scalar(g_la[0][:], g_la[0][:], 1.0 / 16, None, op0=ALU.mult), reads=[g_la[1]], writes=[g_la[1]])
        P.op('vector', lambda e: e.tensor_tensor_scan(g_cb[0][:], rst128[:], g_la[0][:], 0.0, op0=ALU.mult, op1=ALU.add),
             reads=[rst128b, g_la[1]], writes=[g_cb[1]])
        act(g_e[0][:], g_e[1], g_cb[0][:], g_cb[1], AF.Exp)
        act(g_en[0][:], g_en[1], g_cb[0][:], g_cb[1], AF.Exp, scale=-1.0)
        P.op('vector', lambda e: e.scalar_tensor_tensor(g_qd[0][:], g_q[0][:], 128.0 ** -0.5, g_e[0][:], op0=ALU.mult, op1=ALU.mult),
             reads=[g_q[1], g_e[1]], writes=[g_qd[1]])
        ew_tt(g_ki[0][:], g_ki[1], g_k[0][:], g_k[1], g_en[0][:], g_en[1], ALU.mult, 'gpsimd')
        for n in range(SEG // GC):
            cs = slice(n * GC, (n + 1) * GC)
            act(g_dec[0][:, cs], g_dec[1], g_cb[0][:, cs], g_cb[1], AF.Exp, scale=-1.0, bias=g_cb[0][:, n * GC + GC - 1:n * GC + GC])
        ew_tt(g_kd[0][:], g_kd[1], g_k[0][:], g_k[1], g_dec[0][:], g_dec[1], ALU.mult, 'gpsimd')
        for n in range(SEG // GC):
            cs = slice(n * GC, (n + 1) * GC)
            tok0 = t0 + n * GC
            vi = n % 2
            gv, gvb = g_v[vi]; gog, gogb = g_og[vi]
            P.dma('sync', gv[:], gv_d[tok0:tok0 + GC, :], writes=[gvb])
            P.dma('sync', gog[:], gog_d[tok0:tok0 + GC, :], writes=[gogb])
            pa, pab = PS()
            P.op('tensor', lambda e, pa=pa, cs=cs: e.matmul(pa[:, 0:128], g_ki[0][:, cs], g_qd[0][:, cs], start=True, stop=True),
                 reads=[g_ki[1], g_qd[1]], writes=[pab])
            att, attb = g_att[vi]
            P.op('vector', lambda e, pa=pa, att=att: e.tensor_tensor(att[:], pa[:, 0:128], gmask[:], op=ALU.mult),
                 reads=[pab, gmaskb], writes=[attb])
            pt_, ptb_ = PS()
            P.op('tensor', lambda e, pt_=pt_, cs=cs: e.transpose(pt_[:, 0:128], g_kd[0][:, cs], ident[:]),
                 reads=[g_kd[1], identb], writes=[ptb_])
            kdt, kdtb = g_kdt[vi]
            P.op('scalar', lambda e, pt_=pt_, kdt=kdt: e.copy(kdt[:], pt_[:, 0:128]), reads=[ptb_], writes=[kdtb])
            So, Sob = Sg[gpar[0]]; Sn, Snb = Sg[1 - gpar[0]]
            po, pob = PS()
            P.op('tensor', lambda e, po=po, att=att, gv=gv: e.matmul(po[:, 0:256], att[:], gv[:], start=True, stop=False),
                 reads=[attb, gvb], writes=[pob])
            P.op('tensor', lambda e, po=po, cs=cs, So=So: e.matmul(po[:, 0:256], g_qd[0][:, cs], So[:], start=False, stop=True),
                 reads=[g_qd[1], Sob], writes=[pob])
            pst, pstb = PS()
            P.op('tensor', lambda e, pst=pst, kdt=kdt, gv=gv: e.matmul(pst[:, 0:256], kdt[:], gv[:], start=True, stop=True),
                 reads=[kdtb, gvb], writes=[pstb])
            cl = n * GC + GC - 1
            P.op('vector', lambda e, pst=pst, So=So, Sn=Sn, cl=cl: e.scalar_tensor_tensor(
                Sn[:], So[:], g_e[0][:, cl:cl + 1], pst[:, 0:256], op0=ALU.mult, op1=ALU.add),
                reads=[Sob, g_e[1], pstb], writes=[Snb])
            gpar[0] = 1 - gpar[0]
            act(g_sq[0][:], g_sq[1], po[:, 0:256], pob, AF.Square)
            P.op('vector', lambda e: e.tensor_reduce(g_ss[0][:], g_sq[0][:], axis=AX.X, op=ALU.add), reads=[g_sq[1]], writes=[g_ss[1]])
            act(g_rs[0][:], g_rs[1], g_ss[0][:], g_ss[1], AF.Sqrt, scale=1.0 / 256, bias=1e-6)
            P.op('vector', lambda e: e.reciprocal(g_rstd[0][:], g_rs[0][:]), reads=[g_rs[1]], writes=[g_rstd[1]])
            P.op('vector', lambda e, po=po: e.scalar_tensor_tensor(g_on[0][:], po[:, 0:256], g_rstd[0][:, 0:1], gnorm[:], op0=ALU.mult, op1=ALU.mult),
                 reads=[pob, g_rstd[1], gnormb], writes=[g_on[1]])
            act(g_so[0][:], g_so[1], gog[:], gogb, AF.Silu)
            go, gob = g_out[vi]
            ew_tt(go[:], gob, g_on[0][:], g_on[1], g_so[0][:], g_so[1], ALU.mult, 'gpsimd')
            P.dma('sync', og_d[tok0:tok0 + GC, :], go[:], reads=[gob])

        for (lt, ld, rows) in ((l_w, lw_d, 64), (l_a, la_d, 64), (l_g1, lg1_d, 128), (l_g2, lg2_d, 32)):
            P.dma('sync', lt[0][:], ld[:, t0:t0 + SEG + 1], writes=[lt[1]])

        def lerp(raw, rawb, out, outb, mu_ap, mub, tmp, tmpb, rows, eng):
            P.op(eng, lambda e: e.tensor_tensor(tmp, raw[:, 0:SEG], raw[:, 1:SEG + 1], op=ALU.subtract), reads=[rawb], writes=[tmpb])
            P.op('vector', lambda e: e.scalar_tensor_tensor(out, tmp, mu_ap, raw[:, 1:SEG + 1], op0=ALU.mult, op1=ALU.add),
                 reads=[tmpb, mub, rawb], writes=[outb])
        lerp(l_w[0][:], l_w[1], l_wl[0][:], l_wl[1], mul[0:64, 0:1], mulb, l_tmp[0][0:64, :], l_tmp[1], 64, 'gpsimd')
        lerp(l_a[0][:], l_a[1], l_al[0][:], l_al[1], mul[0:64, 1:2], mulb, l_tmp[0][0:64, :], l_tmp[1], 64, 'gpsimd')
        lerp(l_g1[0][:], l_g1[1], l_g1l[0][:], l_g1l[1], mul[:, 2:3], mulb, l_tmp[0][:, :], l_tmp[1], 128, 'gpsimd')
        lerp(l_g2[0][:], l_g2[1], l_g2l[0][:], l_g2l[1], mul[0:32, 3:4], mulb, l_tmp[0][0:32, :], l_tmp[1], 32, 'gpsimd')
        act(l_th[0][:], l_th[1], l_wl[0][:], l_wl[1], AF.Tanh)
        act(l_s1[0][:], l_s1[1], l_g1l[0][:], l_g1l[1], AF.Sigmoid)
        act(l_s2[0][:], l_s2[1], l_g2l[0][:], l_g2l[1], AF.Sigmoid)

        for hh in range(4):
            o = hs[hh]
            hc = slice(hh * 64, (hh + 1) * 64)
            for i, dd in enumerate((rr_d, rk_d, rv_d)):
                P.dma('sync', sc.raw[i][0][:], dd[hh, :, t0:t0 + SEG + 1], writes=[sc.raw[i][1]])
            for i, dst in enumerate((sc.r, sc.k, o.v)):
                lerp(sc.raw[i][0][:], sc.raw[i][1], dst[0][:], dst[1], HP(hh, i), hpb, sc.tmp[0][:], sc.tmp[1], 64, 'gpsimd')
            pw, pwb = PS()
            P.op('tensor', lambda e, pw=pw, hc=hc: e.matmul(pw[0:64, 0:SEG], w2[:, hc], l_th[0][:], start=True, stop=True),
                 reads=[w2b, l_th[1]], writes=[pwb])
            act(sc.lw[0][:], sc.lw[1], pw[0:64, 0:SEG], pwb, AF.Sigmoid, extra_reads=[hpb], bias=HP(hh, 3))
            P.op('vector', lambda e, o=o: e.tensor_scalar(sc.lw[0][:], sc.lw[0][:], -float(np.exp(-0.5)), None, op0=ALU.mult),
                 reads=[sc.lw[1]], writes=[sc.lw[1]])
            pa_, pab_ = PS()
            P.op('tensor', lambda e, pa_=pa_, hc=hc: e.matmul(pa_[0:64, 0:SEG], a2[:, hc], l_al[0][:], start=True, stop=True),
                 reads=[a2b, l_al[1]], writes=[pab_])
            act(sc.a[0][:], sc.a[1], pa_[0:64, 0:SEG], pab_, AF.Sigmoid, extra_reads=[hpb], bias=HP(hh, 4))
            pg, pgb = PS()
            P.op('tensor', lambda e, pg=pg, hc=hc: e.matmul(pg[0:64, 0:SEG], g2a[:, hc], l_s1[0][:], start=True, stop=False),
                 reads=[g2ab, l_s1[1]], writes=[pgb])
            P.op('tensor', lambda e, pg=pg, hc=hc: e.matmul(pg[0:64, 0:SEG], g2b_[:, hc], l_s2[0][:], start=False, stop=True),
                 reads=[g2bb, l_s2[1]], writes=[pgb])
            P.op('scalar', lambda e, pg=pg, o=o: e.copy(o.g[0][:], pg[0:64, 0:SEG]), reads=[pgb], writes=[o.g[1]])
            act(sc.kk2[0][:], sc.kk2[1], sc.k[0][:], sc.k[1], AF.Square, extra_reads=[hpb], scale=HP(hh, 5))
            pn, pnb = PS()
            P.op('tensor', lambda e, pn=pn, o=o: e.matmul(pn[0:64, 0:SEG], onesk[:], sc.kk2[0][:], start=True, stop=True),
                 reads=[oneskb, sc.kk2[1]], writes=[pnb])
            act(sc.inv[0][:], sc.inv[1], pn[0:64, 0:SEG], pnb, AF.Sqrt)
            P.op('vector', lambda e, o=o: e.tensor_scalar(sc.inv[0][:], sc.inv[0][:], 1e-12, None, op0=ALU.max), reads=[sc.inv[1]], writes=[sc.inv[1]])
            P.op('vector', lambda e, o=o: e.reciprocal(sc.inv[0][:], sc.inv[0][:]), reads=[sc.inv[1]], writes=[sc.inv[1]])
            P.op('vector', lambda e, o=o, hh=hh: e.scalar_tensor_tensor(sc.kkn[0][:], sc.k[0][:], HP(hh, 5), sc.inv[0][:], op0=ALU.mult, op1=ALU.mult),
                 reads=[sc.k[1], hpb, sc.inv[1]], writes=[sc.kkn[1]])
            P.op('vector', lambda e, o=o, hh=hh: e.tensor_scalar(sc.t1[0][:], sc.a[0][:], HP(hh, 6), omka[:, hh:hh + 1], op0=ALU.mult, op1=ALU.add),
                 reads=[sc.a[1], hpb, omkab], writes=[sc.t1[1]])
            ew_tt(sc.kp[0][:], sc.kp[1], sc.k[0][:], sc.k[1], sc.t1[0][:], sc.t1[1], ALU.mult, 'gpsimd')
            ew_tt(sc.bb[0][:], sc.bb[1], sc.kkn[0][:], sc.kkn[1], sc.a[0][:], sc.a[1], ALU.mult, 'gpsimd')
            P.op('vector', lambda e, o=o: e.tensor_tensor_scan(sc.cb[0][:], rst64[0:64, :], sc.lw[0][:], 0.0, op0=ALU.mult, op1=ALU.add),
                 reads=[rst64b, sc.lw[1]], writes=[sc.cb[1]])
            ew_tt(sc.cbx[0][:], sc.cbx[1], sc.cb[0][:], sc.cb[1], sc.lw[0][:], sc.lw[1], ALU.subtract, 'gpsimd')
            act(o.e[0][:], o.e[1], sc.cb[0][:], sc.cb[1], AF.Exp)
            act(sc.en[0][:], sc.en[1], sc.cb[0][:], sc.cb[1], AF.Exp, scale=-1.0)
            act(sc.ex[0][:], sc.ex[1], sc.cbx[0][:], sc.cbx[1], AF.Exp)
            ARv = o.AR[0][:]
            P.op('vector', lambda e, o=o, ARv=ARv: e.scalar_tensor_tensor(
                ARv[:, :, 0:64], sc.kkn[0][:].rearrange("p (n c) -> p n c", c=RC), -1.0, sc.ex[0][:].rearrange("p (n c) -> p n c", c=RC),
                op0=ALU.mult, op1=ALU.mult), reads=[sc.kkn[1], sc.ex[1]], writes=[o.AR[1]])
            P.op('vector', lambda e, o=o, ARv=ARv: e.tensor_tensor(
                ARv[:, :, 64:128], sc.r[0][:].rearrange("p (n c) -> p n c", c=RC), o.e[0][:].rearrange("p (n c) -> p n c", c=RC),
                op=ALU.mult), reads=[sc.r[1], o.e[1]], writes=[o.AR[1]])
            ew_tt(o.bt[0][:], o.bt[1], sc.bb[0][:], sc.bb[1], sc.en[0][:], sc.en[1], ALU.mult, 'gpsimd')
            ew_tt(o.kt[0][:], o.kt[1], sc.kp[0][:], sc.kp[1], sc.en[0][:], sc.en[1], ALU.mult, 'gpsimd')
            for n in range(SEG // RC):
                cs = slice(n * RC, (n + 1) * RC)
                act(sc.dec[0][:, cs], sc.dec[1], sc.cb[0][:, cs], sc.cb[1], AF.Exp, scale=-1.0, bias=sc.cb[0][:, n * RC + RC - 1:n * RC + RC])
            ew_tt(o.bd[0][:], o.bd[1], sc.bb[0][:], sc.bb[1], sc.dec[0][:], sc.dec[1], ALU.mult, 'gpsimd')
            ew_tt(o.kd[0][:], o.kd[1], sc.kp[0][:], sc.kp[1], sc.dec[0][:], sc.dec[1], ALU.mult, 'gpsimd')
            ew_tt(o.rkr[0][:], o.rkr[1], sc.r[0][:], sc.r[1], sc.kp[0][:], sc.kp[1], ALU.mult, 'gpsimd')

        def chunk_stages(hh, n):
            o = hs[hh]
            cs = slice(n * RC, (n + 1) * RC)
            AR = o.AR[0][:, n, :]
            st = []
            stt = {}

            def s_mats():
                p1, p1b = PS()
                P.op('tensor', lambda e: e.matmul(p1[0:64, 0:128], o.bt[0][:, cs], AR, start=True, stop=True),
                     reads=[o.bt[1], o.AR[1]], writes=[p1b])
                P.op('vector', lambda e: e.tensor_tensor(o.ABm[0][:], p1[0:64, 0:128], m2[:], op=ALU.mult), reads=[p1b, m2b], writes=[o.ABm[1]])
                p2, p2b = PS()
                P.op('tensor', lambda e: e.matmul(p2[0:64, 0:128], o.kt[0][:, cs], AR, start=True, stop=True),
                     reads=[o.kt[1], o.AR[1]], writes=[p2b])
                P.op('vector', lambda e: e.tensor_tensor(o.AKm[0][:], p2[0:64, 0:128], m2[:], op=ALU.mult), reads=[p2b, m2b], writes=[o.AKm[1]])
                p3, p3b = PS()
                P.op('tensor', lambda e: e.matmul(p3[0:64, 0:64], AR[:, 0:64], o.bt[0][:, cs], start=True, stop=True),
                     reads=[o.bt[1], o.AR[1]], writes=[p3b])
                P.op('vector', lambda e: e.tensor_tensor(o.M[0][0][:], p3[0:64, 0:64], m0[:], op=ALU.mult), reads=[p3b, m0b], writes=[o.M[0][1]])
                P.op('gpsimd', lambda e: e.tensor_copy(o.MT[0][0][:], o.ABm[0][:, 0:64]), reads=[o.ABm[1]], writes=[o.MT[0][1]])
                P.op('gpsimd', lambda e: e.tensor_tensor(o.TT[0][0][:], o.ABm[0][:, 0:64], ident[0:64, 0:64], op=ALU.add),
                     reads=[o.ABm[1], identb], writes=[o.TT[0][1]])
                p4, p4b = PS()
                for i, src in enumerate((o.v, o.bd, o.kd)):
                    P.op('tensor', lambda e, i=i, src=src: e.transpose(p4[0:64, i * 64:(i + 1) * 64], src[0][:, cs], ident[0:64, 0:64]),
                         reads=[src[1], identb], writes=[p4b])
                P.op('scalar', lambda e: e.copy(o.tok[0][:], p4[0:64, 0:192]), reads=[p4b], writes=[o.tok[1]])
            st.append(s_mats)

            def s_akv():
                p, pb = PS()
                P.op('tensor', lambda e: e.matmul(p[0:64, 0:64], o.AKm[0][:, 0:64], o.tok[0][:, 0:64], start=True, stop=True),
                     reads=[o.AKm[1], o.tok[1]], writes=[pb])
                P.op('scalar', lambda e: e.copy(o.AkV[0][:], p[0:64, 0:64]), reads=[pb], writes=[o.AkV[1]])
            st.append(s_akv)

            for i in range(5):
                def s_dbl(i=i):
                    a, b_ = i % 2, (i + 1) % 2
                    M, Mb = o.M[a]; MT, MTb = o.MT[a]; Mn, Mnb = o.M[b_]; MTn, MTnb = o.MT[b_]
                    TTo, TTob = o.TT[a]; TTn, TTnb = o.TT[b_]
                    p, pb = PS()
                    P.op('tensor', lambda e: e.matmul(p[0:64, 0:64], MT[:], M[:], start=True, stop=True), reads=[MTb, Mb], writes=[pb])
                    P.op('vector', lambda e: e.tensor_copy(Mn[:], p[0:64, 0:64]), reads=[pb], writes=[Mnb])
                    if i < 4:
                        q, qb = PS()
                        P.op('tensor', lambda e: e.matmul(q[0:64, 0:64], M[:], MT[:], start=True, stop=True), reads=[MTb, Mb], writes=[qb])
                        P.op('scalar', lambda e: e.copy(MTn[:], q[0:64, 0:64]), reads=[qb], writes=[MTnb])
                    u, ub = PS()
                    P.op('tensor', lambda e: e.matmul(u[0:64, 0:64], Mn[:], TTo[:], start=True, stop=True), reads=[Mnb, TTob], writes=[ub])
                    P.op('vector', lambda e: e.tensor_tensor(TTn[:], u[0:64, 0:64], TTo[:], op=ALU.add), reads=[ub, TTob], writes=[TTnb])
                st.append(s_dbl)

            def s_x():
                So, Sob = Sr[hh][rpar[hh]]
                p, pb = PS()
                P.op('tensor', lambda e: e.matmul(p[0:64, 0:64], AR[:, 0:64], So[:], start=True, stop=True), reads=[o.AR[1], Sob], writes=[pb])
                P.op('vector', lambda e: e.tensor_tensor(o.X[0][:], p[0:64, 0:64], o.AkV[0][:], op=ALU.add), reads=[pb, o.AkV[1]], writes=[o.X[1]])
            st.append(s_x)

            def s_u():
                TT, TTb = o.TT[1]
                p, pb = PS()
                P.op('tensor', lambda e: e.matmul(p[0:64, 0:64], TT[:], o.X[0][:], start=True, stop=True), reads=[TTb, o.X[1]], writes=[pb])
                P.op('scalar', lambda e: e.copy(o.U[0][:], p[0:64, 0:64]), reads=[pb], writes=[o.U[1]])
            st.append(s_u)

            def s_state_y():
                So, Sob = Sr[hh][rpar[hh]]; Sn, Snb = Sr[hh][1 - rpar[hh]]
                p, pb = PS()
                P.op('tensor', lambda e: e.matmul(p[0:64, 0:64], o.tok[0][:, 64:128], o.U[0][:], start=True, stop=False),
                     reads=[o.tok[1], o.U[1]], writes=[pb])
                P.op('tensor', lambda e: e.matmul(p[0:64, 0:64], o.tok[0][:, 128:192], o.tok[0][:, 0:64], start=False, stop=True),
                     reads=[o.tok[1]], writes=[pb])
                cl = n * RC + RC - 1
                P.op('vector', lambda e: e.scalar_tensor_tensor(Sn[:], So[:], o.e[0][:, cl:cl + 1], p[0:64, 0:64], op0=ALU.mult, op1=ALU.add),
                     reads=[Sob, o.e[1], pb], writes=[Snb])
                y, yb = PS()
                P.op('tensor', lambda e: e.matmul(y[0:64, 0:64], So[:], AR[:, 64:128], start=True, stop=False), reads=[Sob, o.AR[1]], writes=[yb])
                P.op('tensor', lambda e: e.matmul(y[0:64, 0:64], o.U[0][:], o.ABm[0][:, 64:128], start=False, stop=False),
                     reads=[o.U[1], o.ABm[1]], writes=[yb])
                P.op('tensor', lambda e: e.matmul(y[0:64, 0:64], o.tok[0][:, 0:64], o.AKm[0][:, 64:128], start=False, stop=True),
                     reads=[o.tok[1], o.AKm[1]], writes=[yb])
                P.op('scalar', lambda e: e.copy(o.y[0][:, cs], y[0:64, 0:64]), reads=[yb], writes=[o.y[1]])
                rpar[hh] = 1 - rpar[hh]
            st.append(s_state_y)
            return st

        for n in range(SEG // RC):
            stages = [chunk_stages(hh, n) for hh in range(4)]
            for k in range(len(stages[0])):
                for hh in range(4):
                    stages[hh][k]()

        for hh in range(4):
            o = hs[hh]
            pm, pmb = PS()
            P.op('tensor', lambda e, o=o, pm=pm: e.matmul(pm[0:64, 0:SEG], ones64[:], o.y[0][:], start=True, stop=True), reads=[ones64b, o.y[1]], writes=[pmb])
            P.op('vector', lambda e, o=o, pm=pm: e.tensor_tensor(pc.yc[0][:], o.y[0][:], pm[0:64, 0:SEG], op=ALU.subtract), reads=[o.y[1], pmb], writes=[pc.yc[1]])
            act(pc.sq[0][:], pc.sq[1], pc.yc[0][:], pc.yc[1], AF.Square)
            pv, pvb = PS()
            P.op('tensor', lambda e, o=o, pv=pv: e.matmul(pv[0:64, 0:SEG], ones64[:], pc.sq[0][:], start=True, stop=True), reads=[ones64b, pc.sq[1]], writes=[pvb])
            act(pc.rs[0][:], pc.rs[1], pv[0:64, 0:SEG], pvb, AF.Sqrt, bias=LN_EPS)
            P.op('vector', lambda e, o=o: e.reciprocal(pc.rstd[0][:], pc.rs[0][:]), reads=[pc.rs[1]], writes=[pc.rstd[1]])
            ew_tt(pc.yc[0][:], pc.yc[1], pc.yc[0][:], pc.yc[1], pc.rstd[0][:], pc.rstd[1], ALU.mult, 'gpsimd')
            P.op('vector', lambda e, o=o, hh=hh: e.tensor_scalar(pc.yc[0][:], pc.yc[0][:], HP(hh, 8), HP(hh, 9), op0=ALU.mult, op1=ALU.add),
                 reads=[pc.yc[1], hpb], writes=[pc.yc[1]])
            pb_, pbb = PS()
            P.op('tensor', lambda e, o=o, pb_=pb_, hh=hh: e.matmul(pb_[0:64, 0:SEG], rkm[hh][0][:], o.rkr[0][:], start=True, stop=True),
                 reads=[rkm[hh][1], o.rkr[1]], writes=[pbb])
            P.op('vector', lambda e, o=o, pb_=pb_: e.tensor_tensor(pc.out[0][:], pb_[0:64, 0:SEG], o.v[0][:], op=ALU.mult), reads=[pbb, o.v[1]], writes=[pc.out[1]])
            ew_tt(pc.out[0][:], pc.out[1], pc.out[0][:], pc.out[1], pc.yc[0][:], pc.yc[1], ALU.add, 'gpsimd')
            ew_tt(pc.out[0][:], pc.out[1], pc.out[0][:], pc.out[1], o.g[0][:], o.g[1], ALU.mult, 'gpsimd')
            P.dma('sync', or_d[hh, :, t0:t0 + SEG], pc.out[0][:], reads=[pc.out[1]])

    P.wait_all('sync', [g_out[0][1], g_out[1][1]] + [pc.out[1]])
    P.emit()
    P.close()
    return nc

GQ, GK, GV, GOG, RR, RK, RV, GLOW, WLOW, ALOW, GLR, NINP = 0, 512, 1024, 2048, 3072, 4096, 5120, 6144, 6160, 6224, 6288, 6528
PERM = np.concatenate([np.arange(0, 3072), np.arange(3088, 6160), np.arange(3072, 3088), np.arange(6160, 6448)])


def pad_win(w_in):
    w = np.zeros((w_in.shape[0], NINP), np.float32)
    w[:, :6448] = w_in[:, PERM]
    return w


def padz(a):
    return np.ascontiguousarray(np.concatenate([np.zeros(a.shape[:-1] + (1,), np.float32), a], axis=-1))


def mix_inputs(pT, prm, j):
    S = pT.shape[1]
    c = np.ascontiguousarray
    d = {}
    d["gq"] = c(pT[GQ + 128 * j:GQ + 128 * j + 128]); d["gk"] = c(pT[GK + 128 * j:GK + 128 * j + 128])
    d["gv"] = c(pT[GV + 256 * j:GV + 256 * j + 256].T); d["gog"] = c(pT[GOG + 256 * j:GOG + 256 * j + 256].T)
    d["glow"] = c(pT[GLOW:GLOW + 16])
    d["gw2"] = c(prm['gla_gate_w2'][:, 128 * j:128 * j + 128]); d["ggb"] = c(prm['gla_gate_b'][128 * j:128 * j + 128][:, None])
    d["gnorm"] = c(np.tile(prm['gla_norm'][None, :], (128, 1)))
    r0 = 256 * j
    d["rr"] = padz(pT[RR + r0:RR + r0 + 256].reshape(4, 64, S)); d["rk"] = padz(pT[RK + r0:RK + r0 + 256].reshape(4, 64, S))
    d["rv"] = padz(pT[RV + r0:RV + r0 + 256].reshape(4, 64, S))
    d["lw"] = padz(pT[WLOW:WLOW + 64]); d["la"] = padz(pT[ALOW:ALOW + 64]); d["lg1"] = padz(pT[GLR:GLR + 128]); d["lg2"] = padz(pT[GLR + 128:GLR + 160])
    mu = prm['rwkv_mu']
    hp = np.zeros((64, 40), np.float32)
    for hh in range(4):
        cols = r0 + 64 * hh + np.arange(64)
        vals = [mu[cols], mu[1024 + cols], mu[2048 + cols], prm['rwkv_w0'][cols], prm['rwkv_a0'][cols], prm['rwkv_k_k'][cols],
                prm['rwkv_k_a'][cols], prm['rwkv_r_k'].reshape(-1)[cols], prm['rwkv_ln_w'][cols], prm['rwkv_ln_b'][cols]]
        for i, v in enumerate(vals):
            hp[:, hh * 10 + i] = v
    d["hp"] = hp
    mul = np.zeros((128, 4), np.float32)
    mul[0:64, 0] = mu[3072:3136]; mul[0:64, 1] = mu[3136:3200]; mul[:, 2] = mu[3200:3328]; mul[0:32, 3] = mu[3328:3360]
    d["mul"] = mul
    d["w2"] = c(prm['rwkv_w2'][:, r0:r0 + 256]); d["a2"] = c(prm['rwkv_a2'][:, r0:r0 + 256])
    d["g2a"] = c(prm['rwkv_g2'][0:128, r0:r0 + 256]); d["g2b"] = c(prm['rwkv_g2'][128:160, r0:r0 + 256])
    return d

_NC_CACHE = {}


def _nc(key, fn):
    if key not in _NC_CACHE:
        _NC_CACHE[key] = fn()
    return _NC_CACHE[key]


def _run(nc, in_maps):
    res = run_bass_kernel_spmd(nc, in_maps, core_ids=list(range(8)))
    return res.results


def kernel(**inp):
    f32 = np.float32
    inp = {k: np.asarray(v, dtype=f32) for k, v in inp.items()}
    x = inp['x']
    B, S_, Dm = x.shape
    c = np.ascontiguousarray
    cores = [(b, q) for b in range(2) for q in range(4)]
    w1 = pad_win(inp['mix_in_w'][0])
    g1 = gtab(inp['norm_mix'][0])
    xT = [c(x[b, q * TC:(q + 1) * TC].T) for (b, q) in cores]
    r1 = _run(_nc('L1', lambda: build_row('L1')), [{"hT": xT[i], "g1": g1, "w1": w1} for i in range(8)])
    pT = [np.concatenate([r1[b * 4 + q]["pT"] for q in range(4)], axis=1) for b in range(2)]
    prm = {k: inp[k][0] for k in ['gla_gate_w2', 'gla_gate_b', 'gla_norm', 'rwkv_mu', 'rwkv_w0', 'rwkv_w2', 'rwkv_a0', 'rwkv_a2',
                                  'rwkv_g2', 'rwkv_k_k', 'rwkv_k_a', 'rwkv_r_k', 'rwkv_ln_w', 'rwkv_ln_b']}
    mc = mix_consts()
    maps = []
    for (b, j) in cores:
        d = mix_inputs(pT[b], prm, j)
        d.update(mc)
        maps.append(d)
    r2 = _run(_nc('L2', lambda: build_mix(S_)), maps)
    oT = [np.zeros((Dm, S_), f32) for _ in range(2)]
    for i, (b, j) in enumerate(cores):
        oT[b][256 * j:256 * j + 256] = r2[i]["o_gla"].T
        oT[b][1024 + 256 * j:1024 + 256 * j + 256] = r2[i]["o_rwkv"].reshape(256, S_)
    maps = []
    for i, (b, q) in enumerate(cores):
        maps.append({"hT": xT[i], "oT": c(oT[b][:, q * TC:(q + 1) * TC]), "wo": inp['mix_out_w'][0], "gf": gtab(inp['norm_ffn'][0]),
                     "wg": inp['ffn_gate_w'][0], "wu": inp['ffn_up_w'][0], "wd": inp['ffn_down_w'][0], "g2": gtab(inp['norm_mix'][1]),
                     "w2": inp['attn_qkv_w'][0]})
    r3 = _run(_nc('L3', lambda: build_row('L3')), maps)
    hT = [r3[i]["hout"] for i in range(8)]
    qkvT = [np.concatenate([r3[b * 4 + q]["qkvT"] for q in range(4)], axis=1) for b in range(2)]
    maps = []
    for (b, j) in cores:
        qq = qkvT[b][0:2048].reshape(16, 128, S_)[4 * j:4 * j + 4]
        kk = qkvT[b][2048:4096].reshape(16, 128, S_)[4 * j:4 * j + 4]
        vv = qkvT[b][4096:6144].reshape(16, 128, S_)[4 * j:4 * j + 4]
        d = {"qT": c(qq), "kT": c(kk), "v": c(vv.transpose(0, 2, 1))}
        d.update(moba_consts(4 * j))
        maps.append(d)
    r4 = _run(_nc('L4', build_moba), maps)
    oT2 = [np.zeros((Dm, S_), f32) for _ in range(2)]
    for i, (b, j) in enumerate(cores):
        o = r4[i]["o"]
        for hh in range(4):
            oT2[b][(4 * j + hh) * 128:(4 * j + hh + 1) * 128] = o[hh].T
    maps = []
    for i, (b, q) in enumerate(cores):
        maps.append({"hT": hT[i], "oT": c(oT2[b][:, q * TC:(q + 1) * TC]), "wo": inp['attn_out_w'][0], "gf": gtab(inp['norm_ffn'][1]),
                     "wg": inp['ffn_gate_w'][1], "wu": inp['ffn_up_w'][1], "wd": inp['ffn_down_w'][1], "g2": gtab(inp['norm_final'])})
    r5 = _run(_nc('L5', lambda: build_row('L5')), maps)
    out = np.zeros((B, S_, Dm), f32)
    for i, (b, q) in enumerate(cores):
        out[b, q * TC:(q + 1) * TC] = r5[i]["yT"].T
    return out
```

```python
import numpy as np
from contextlib import ExitStack
import concourse.bass as bass
import concourse.mybir as mybir
from concourse.bass_utils import run_bass_kernel_spmd

F32 = mybir.dt.float32
BF16 = mybir.dt.bfloat16
AF = mybir.ActivationFunctionType
ALU = mybir.AluOpType
AX = mybir.AxisListType
ENGS = ['tensor', 'vector', 'scalar', 'gpsimd', 'sync']


class Buf:
    __slots__ = ('name', 'last_w', 'readers', 'dsem', 'dcount')

    def __init__(self, name):
        self.name = name
        self.last_w = None
        self.readers = []
        self.dsem = None
        self.dcount = 0


class Op:
    __slots__ = ('eng', 'seq', 'fn', 'waits', 'signals', 'sigidx', 'kind', 'dsem', 'dval', 'inc')

    def __init__(self, eng, seq, fn, kind):
        self.eng = eng
        self.seq = seq
        self.fn = fn
        self.waits = []
        self.signals = False
        self.sigidx = None
        self.kind = kind
        self.dsem = None
        self.dval = 0
        self.inc = 16


class SemPool:
    def __init__(self, nc):
        self.nc = nc
        self.es = ExitStack()
        self.free = []
        self.n = 0

    def new(self, tag):
        self.n += 1
        return self.es.enter_context(self.nc.semaphore(f'{tag}{self.n}'))

    def get(self):
        if self.free:
            return self.free.pop()
        return [self.new('dp'), 0]

    def put(self, sem, count):
        self.free.append([sem, count])

    def close(self):
        self.es.close()


class Prog:
    pool = None

    def __init__(self, nc):
        self.nc = nc
        self.es = ExitStack()
        self.ops = {e: [] for e in ENGS}
        Prog._uid = getattr(Prog, '_uid', 0) + 1
        self.uid = Prog._uid
        if Prog.pool is not None:
            self.sem = {e: Prog.pool.new('ps_' + e) for e in ENGS}
        else:
            self.sem = {e: self.es.enter_context(nc.semaphore(f'sem{self.uid}_' + e)) for e in ENGS}
        self.seen = {e: {} for e in ENGS}
        self.nsem = 0
        self.ntens = 0
        self.dsems = []
        self.dbufs = []

    def sbuf(self, shape, dt, name=None):
        self.ntens += 1
        return self.es.enter_context(self.nc.sbuf_tensor(f'u{self.uid}_' + (name or f't{self.ntens}'), list(shape), dt))

    def psum(self, shape, dt=F32, name=None):
        self.ntens += 1
        return self.es.enter_context(self.nc.psum_tensor(f'u{self.uid}_' + (name or f'p{self.ntens}'), list(shape), dt))

    def buf(self, name='b'):
        return Buf(name)

    def bufs(self, n, name='b'):
        return [Buf(f'{name}{i}') for i in range(n)]

    def _collect(self, eng, reads, writes):
        deps = []
        for b in reads:
            if b.last_w is not None:
                deps.append(b.last_w)
        for b in writes:
            if b.last_w is not None:
                deps.append(b.last_w)
            deps.extend(b.readers)
        waits = []
        seen = self.seen[eng]
        for d in deps:
            if d.kind == 'c':
                if d.eng == eng and eng == 'tensor':
                    continue
                key = ('e', d.eng)
                if seen.get(key, 0) >= d.seq:
                    continue
                seen[key] = d.seq
                d.signals = True
                waits.append(d)
            else:
                key = ('d', id(d.dsem))
                if seen.get(key, 0) >= d.dval:
                    continue
                seen[key] = d.dval
                waits.append(d)
        return waits

    def _commit(self, op, reads, writes):
        for b in reads:
            b.readers.append(op)
        for b in writes:
            b.last_w = op
            b.readers = []

    def op(self, eng, fn, reads=(), writes=()):
        o = Op(eng, len(self.ops[eng]) + 1, fn, 'c')
        o.waits = self._collect(eng, reads, writes)
        self.ops[eng].append(o)
        self._commit(o, reads, writes)
        return o

    def _dsem(self, cb):
        if cb.dsem is None:
            self.nsem += 1
            if Prog.pool is not None:
                cb.dsem, cb.dcount = Prog.pool.get()
            else:
                cb.dsem = self.es.enter_context(self.nc.semaphore(f'd{self.uid}_{self.nsem}'))
            self.dsems.append(cb.dsem)
            self.dbufs.append(cb)

    def dma(self, q, out, in_, reads=(), writes=(), **kw):
        cb = writes[0] if writes else reads[0]
        self._dsem(cb)
        o = Op(q, len(self.ops[q]) + 1, None, 'd')
        o.waits = self._collect(q, reads, writes)
        cb.dcount += 16
        o.dsem = cb.dsem
        o.dval = cb.dcount
        o.fn = lambda e, out=out, in_=in_, kw=kw: e.dma_start(out=out, in_=in_, **kw)
        self.ops[q].append(o)
        self._commit(o, reads, writes)
        return o

    def collective(self, kind, in_ap, out_ap, groups, reads=(), writes=()):
        cb = writes[0]
        self._dsem(cb)
        o = Op('gpsimd', len(self.ops['gpsimd']) + 1, None, 'd')
        o.waits = self._collect('gpsimd', reads, writes)
        cb.dcount += 1
        o.dsem = cb.dsem
        o.dval = cb.dcount
        o.inc = 1
        o.fn = lambda e: e.collective_compute(kind, ALU.bypass, replica_groups=groups, ins=[in_ap], outs=[out_ap])
        self.ops['gpsimd'].append(o)
        self._commit(o, reads, writes)
        return o

    def wait_all(self, eng, bufs):
        o = Op(eng, len(self.ops[eng]) + 1, None, 'w')
        reads = list(bufs)
        o.waits = self._collect(eng, reads, reads)
        self.ops[eng].append(o)
        return o

    def emit(self):
        for e in ENGS:
            c = 0
            for o in self.ops[e]:
                if o.kind == 'c' and o.signals:
                    c += 1
                    o.sigidx = c
        with self.nc.Block() as block:
            for e in ENGS:
                def body(eh, e=e):
                    for o in self.ops[e]:
                        ws = {}
                        for d in o.waits:
                            if d.kind == 'c':
                                s, v = self.sem[d.eng], d.sigidx
                            else:
                                s, v = d.dsem, d.dval
                            k = id(s)
                            if k not in ws or ws[k][1] < v:
                                ws[k] = (s, v)
                        for s, v in ws.values():
                            eh.wait_ge(s, v)
                        if o.kind == 'w':
                            continue
                        ins = o.fn(eh)
                        if o.kind == 'd':
                            ins.then_inc(o.dsem, o.inc)
                        elif o.signals:
                            ins.then_inc(self.sem[e], 1)
                getattr(block, e)(body)

    def close(self):
        self.es.close()

    def finish(self):
        self.wait_all('sync', list(self.dbufs))
        self.emit()
        if Prog.pool is not None:
            for b in self.dbufs:
                Prog.pool.put(b.dsem, b.dcount)
        self.close()

    def stats(self):
        return {e: len(self.ops[e]) for e in ENGS}


D = 2048
FF = 5632
TC = 1024
TH = 512
RMS_EPS = 1e-6
GROUPS = [[0, 1, 2, 3], [4, 5, 6, 7]]


def wgroups(W, GW):
    K, N = W.shape
    KC, NG = K // 128, N // GW
    return np.ascontiguousarray(W.reshape(KC, 128, NG, GW).transpose(2, 1, 0, 3).reshape(NG, 128, KC * GW))


def gtab(g):
    return np.ascontiguousarray(np.asarray(g, np.float32).reshape(16, 128).T)


def row_phase(nc, mode, X):
    P = Prog(nc)
    hT_d = X['hin']; gout_d = X['gout']; selm_d = X['selm']
    wo_d = X['wo']; gf_d = X['gf']; wg_d = X['wg']; wu_d = X['wu']; wd_d = X['wd']; g2_d = X['g2']

    h = P.sbuf([128, 16, TH], F32, 'h'); hb = P.bufs(16, 'h')
    xn = P.sbuf([128, 16, TH], BF16, 'xn'); xnb = P.bufs(16, 'xn')
    ones = P.sbuf([128, 128], BF16, 'ones'); onesb = P.buf('ones')
    sq = [P.sbuf([128, TH], BF16, f'sq{i}') for i in range(2)]; sqb = P.bufs(2, 'sq')
    rs = P.sbuf([128, TH], F32, 'rs'); rsb = P.buf('rs')
    rstd = P.sbuf([128, TH], F32, 'rstd'); rstdb = P.buf('rstd')
    NW = 3
    wbuf = [P.sbuf([128, 8192], BF16, f'wbuf{i}') for i in range(NW)]; wb = P.bufs(NW, 'w')
    ost = [P.sbuf([128, TH], F32, f'ost{i}') for i in range(3)]; ostb = P.bufs(3, 'ost')
    NPS = 6
    ps = [P.psum([128, TH], F32, f'ps{i}') for i in range(NPS)]; psb = P.bufs(NPS, 'ps')
    ssps = P.psum([128, TH], F32, 'ssps'); sspsb = P.buf('ssps')
    gt1 = P.sbuf([128, 16], F32, 'gt1'); gt1b = P.buf('gt1')
    gt2 = P.sbuf([128, 16], F32, 'gt2'); gt2b = P.buf('gt2')
    selm = P.sbuf([128, 4], F32, 'selm'); selmb = P.buf('selm')
    act = P.sbuf([128, 44, TH], BF16, 'act'); actb = P.bufs(44, 'act')
    sg = [P.sbuf([128, TH], F32, f'sg{i}') for i in range(2)]; sgb = P.bufs(2, 'sg')
    cand = [P.sbuf([128, TH], F32, f'cand{i}') for i in range(4)]; candb = P.bufs(4, 'cand')
    acc = P.sbuf([128, TH], F32, 'acc'); accb = P.buf('acc')

    st = {'w': 0, 'ps': 0, 'ost': 0, 'sq': 0, 'sg': 0}

    def nxt(k, n):
        v = st[k] % n
        st[k] += 1
        return v

    P.op('vector', lambda e: e.memset(ones[:], 1.0), writes=[onesb])
    P.dma('sync', gt1[:], gf_d, writes=[gt1b])
    P.dma('sync', gt2[:], g2_d, writes=[gt2b])
    P.dma('sync', selm[:], selm_d, writes=[selmb])

    def load_w(Wd_ap, KC, g, GW):
        wi = nxt('w', NW)
        wt = wbuf[wi][:, 0:KC * GW].rearrange("p (c n) -> p c n", n=GW)
        P.dma('gpsimd', wbuf[wi][:, 0:KC * GW], Wd_ap[g], writes=[wb[wi]])
        return wt, wb[wi]

    def linear(xin, xinb, KC, Wd_ap, N, GW, consume):
        ng = (N + GW - 1) // GW
        for g in range(ng):
            gw = min(GW, N - g * GW)
            wt, wtb = load_w(Wd_ap, KC, g, gw)
            for s in range(gw // 128):
                j = (g * GW) // 128 + s
                pi = nxt('ps', NPS)
                for c in range(KC):
                    P.op('tensor', lambda e, pi=pi, wt=wt, c=c, s=s: e.matmul(
                        ps[pi][:], wt[:, c, s * 128:(s + 1) * 128], xin[:, c, :], start=(c == 0), stop=(c == KC - 1)),
                        reads=[wtb, xinb[c]], writes=[psb[pi]])
                consume(j, ps[pi], psb[pi])

    def sumsq_rstd():
        for c in range(16):
            si = nxt('sq', 2)
            P.op('scalar', lambda e, si=si, c=c: e.activation(sq[si][:], h[:, c, :], AF.Square),
                 reads=[hb[c]], writes=[sqb[si]])
            P.op('tensor', lambda e, si=si, c=c: e.matmul(ssps[:], ones[:], sq[si][:], start=(c == 0), stop=(c == 15)),
                 reads=[onesb, sqb[si]], writes=[sspsb])
        P.op('scalar', lambda e: e.activation(rs[:], ssps[:], AF.Sqrt, bias=RMS_EPS, scale=1.0 / D),
             reads=[sspsb], writes=[rsb])
        P.op('vector', lambda e: e.reciprocal(rstd[:], rs[:]), reads=[rsb], writes=[rstdb])

    def rmsnorm(gt, gtb):
        sumsq_rstd()
        for c in range(16):
            P.op('vector', lambda e, c=c: e.scalar_tensor_tensor(
                xn[:, c, :], h[:, c, :], gt[:, c:c + 1], rstd[:], op0=ALU.mult, op1=ALU.mult),
                reads=[hb[c], gtb, rstdb], writes=[xnb[c]])

    def res_add(j, pst, pstb):
        P.op('vector', lambda e, j=j, pst=pst: e.tensor_tensor(h[:, j, :], pst[:], h[:, j, :], op=ALU.add),
             reads=[pstb, hb[j]], writes=[hb[j]])

    for half in range(TC // TH):
        t0 = half * TH
        for c in range(16):
            P.dma('sync', h[:, c, :], hT_d[c * 128:(c + 1) * 128, t0:t0 + TH], writes=[hb[c]])
        for c in range(16):
            for r in range(4):
                rk_, lr_ = c // 4, (c % 4) * 128
                P.dma('sync', cand[r][:], gout_d[lr_ // 256][r][rk_ * 256 + lr_ % 256:rk_ * 256 + lr_ % 256 + 128, t0:t0 + TH], writes=[candb[r]])
            P.op('vector', lambda e: e.tensor_scalar(acc[:], cand[0][:], selm[:, 0:1], None, op0=ALU.mult),
                 reads=[candb[0], selmb], writes=[accb])
            for r in (1, 2):
                P.op('vector', lambda e, r=r: e.scalar_tensor_tensor(acc[:], cand[r][:], selm[:, r:r + 1], acc[:], op0=ALU.mult, op1=ALU.add),
                     reads=[candb[r], selmb, accb], writes=[accb])
            P.op('vector', lambda e, c=c: e.scalar_tensor_tensor(act[:, c, :], cand[3][:], selm[:, 3:4], acc[:], op0=ALU.mult, op1=ALU.add),
                 reads=[candb[3], selmb, accb], writes=[actb[c]])
        linear(act, actb, 16, wo_d, D, 512, res_add)
        rmsnorm(gt1, gt1b)
        for g in range(FF // 512):
            wgt, wgb = load_w(wg_d, 16, g, 512)
            wut, wub = load_w(wu_d, 16, g, 512)
            for s in range(4):
                j = g * 4 + s
                pg = nxt('ps', NPS)
                for c in range(16):
                    P.op('tensor', lambda e, pg=pg, c=c, s=s, wgt=wgt: e.matmul(
                        ps[pg][:], wgt[:, c, s * 128:(s + 1) * 128], xn[:, c, :], start=(c == 0), stop=(c == 15)),
                        reads=[wgb, xnb[c]], writes=[psb[pg]])
                pu = nxt('ps', NPS)
                for c in range(16):
                    P.op('tensor', lambda e, pu=pu, c=c, s=s, wut=wut: e.matmul(
                        ps[pu][:], wut[:, c, s * 128:(s + 1) * 128], xn[:, c, :], start=(c == 0), stop=(c == 15)),
                        reads=[wub, xnb[c]], writes=[psb[pu]])
                si = nxt('sg', 2)
                P.op('scalar', lambda e, si=si, pg=pg: e.activation(sg[si][:], ps[pg][:], AF.Silu),
                     reads=[psb[pg]], writes=[sgb[si]])
                P.op('vector', lambda e, si=si, pu=pu, j=j: e.tensor_tensor(act[:, j, :], ps[pu][:], sg[si][:], op=ALU.mult),
                     reads=[psb[pu], sgb[si]], writes=[actb[j]])
        linear(act, actb, 44, wd_d, D, 128, res_add)
        if mode == 'C':
            for c in range(16):
                P.dma('sync', X['h2s'][c * 128:(c + 1) * 128, t0:t0 + TH], h[:, c, :], reads=[hb[c]])
            rmsnorm(gt2, gt2b)
            for c in range(16):
                P.dma('sync', X['gin2'][c // 2][(c % 2) * 128:(c % 2 + 1) * 128, t0:t0 + TH], xn[:, c, :], reads=[xnb[c]])
        else:
            sumsq_rstd()
            for c in range(16):
                oi = nxt('ost', 3)
                P.op('vector', lambda e, c=c, oi=oi: e.scalar_tensor_tensor(
                    ost[oi][:], h[:, c, :], gt2[:, c:c + 1], rstd[:], op0=ALU.mult, op1=ALU.add if False else ALU.mult),
                    reads=[hb[c], gt2b, rstdb], writes=[ostb[oi]])
                P.dma('sync', X['yT'][c * 128:(c + 1) * 128, t0:t0 + TH], ost[oi][:], reads=[ostb[oi]])
    if mode == 'C':
        g2b_ = P.buf('gin2all'); go2b = P.buf('gout2')
        P.wait_all('gpsimd', list(P.dbufs))
        for k_ in range(8):
            P.collective("AllGather", X['gin2'][k_], X['gout2'][k_], GROUPS, reads=[g2b_], writes=[P.buf('gout2')])
    P.finish()


SQ = 4096
NF1 = 1408
NT1 = 512


def _proj_common(P, nk_f, nk_t):
    pass


def phase_A(nc, X):
    P = Prog(nc)
    TH = 512
    h2 = [P.sbuf([128, 16, TH], F32, f'h{i}') for i in range(2)]; hb2 = [P.bufs(16, 'h') for _ in range(2)]
    xn = P.sbuf([128, 16, TH], BF16, 'xn'); xnb = P.bufs(16, 'xn')
    ones = P.sbuf([128, 128], BF16, 'ones'); onesb = P.buf()
    sq = [P.sbuf([128, TH], BF16, f'sq{i}') for i in range(2)]; sqb = P.bufs(2)
    rs = P.sbuf([128, TH], F32, 'rs'); rsb = P.buf()
    rstd = P.sbuf([128, TH], F32, 'rstd'); rstdb = P.buf()
    wf = P.sbuf([128, 16, NF1], BF16, 'wf'); wfb = P.buf()
    wt = P.sbuf([128, 16, NT1], BF16, 'wt'); wtb = P.buf()
    ost = [P.sbuf([128, TH], F32, f'ost{i}') for i in range(4)]; ostb = P.bufs(4)
    gt = P.sbuf([128, 16], F32, 'gt'); gtb = P.buf()
    zt = P.sbuf([128, 1], F32, 'zt'); ztb = P.buf()
    NPS = 6
    ps = [P.psum([128, TH], F32, f'ps{i}') for i in range(NPS)]; psb = P.bufs(NPS)
    ssps = P.psum([128, TH], F32, 'ssps'); sspsb = P.buf()
    ctr = {'ps': 0, 'ost': 0, 'sq': 0}

    def nxt(k, n):
        v = ctr[k] % n
        ctr[k] += 1
        return v

    P.op('vector', lambda e: e.memset(ones[:], 1.0), writes=[onesb])
    P.op('vector', lambda e: e.memset(zt[:], 0.0), writes=[ztb])
    P.dma('sync', gt[:], X['g1'], writes=[gtb])
    wff = wf[:].rearrange("p c n -> p (c n)"); wtf = wt[:].rearrange("p c n -> p (c n)")
    for c0 in range(0, 16, 8):
        P.dma('gpsimd', wff[:, c0 * NF1:(c0 + 8) * NF1], X['w1f'][:, c0 * NF1:(c0 + 8) * NF1], writes=[wfb])
    P.dma('gpsimd', wtf, X['w1t'], writes=[wtb])
    for nm, rows in (('rr', 256), ('rk', 256), ('rv', 256), ('lw', 64), ('la', 64), ('lg1', 128), ('lg2', 32)):
        for r0 in range(0, rows, 128):
            n = min(128, rows - r0)
            P.dma('sync', X[nm][r0:r0 + n, 0:1], zt[0:n, :], reads=[ztb], allow_slow_non_contiguous=True)
    dests = [
        [('gq', 0, 128, 0, 0)], [('gk', 0, 128, 0, 0)],
        [('rr', 0, 128, 0, 1)], [('rr', 128, 128, 0, 1)], [('rk', 0, 128, 0, 1)], [('rk', 128, 128, 0, 1)],
        [('rv', 0, 128, 0, 1)], [('rv', 128, 128, 0, 1)],
        [('lw', 0, 64, 0, 1), ('la', 0, 64, 64, 1)], [('lg1', 0, 128, 0, 1)],
        [('lg2', 0, 32, 0, 1), ('glow', 0, 16, 32, 0)],
    ]
    def load_x(tt):
        for c in range(16):
            P.dma('sync', h2[tt % 2][:, c, :], X['xfull'][c * 128:(c + 1) * 128, tt * TH:(tt + 1) * TH], writes=[hb2[tt % 2][c]])
    load_x(0)
    for tt in range(SQ // TH):
        t0 = tt * TH
        if tt + 1 < SQ // TH:
            load_x(tt + 1)
        h = h2[tt % 2]; hb = hb2[tt % 2]
        for c in range(16):
            si = nxt('sq', 2)
            P.op('scalar', lambda e, si=si, c=c, h=h: e.activation(sq[si][:], h[:, c, :], AF.Square), reads=[hb[c]], writes=[sqb[si]])
            P.op('tensor', lambda e, si=si, c=c: e.matmul(ssps[:], ones[:], sq[si][:], start=(c == 0), stop=(c == 15)),
                 reads=[onesb, sqb[si]], writes=[sspsb])
        P.op('scalar', lambda e: e.activation(rs[:], ssps[:], AF.Sqrt, bias=RMS_EPS, scale=1.0 / D), reads=[sspsb], writes=[rsb])
        P.op('vector', lambda e: e.reciprocal(rstd[:], rs[:]), reads=[rsb], writes=[rstdb])
        for c in range(16):
            P.op('vector', lambda e, c=c, h=h: e.scalar_tensor_tensor(xn[:, c, :], h[:, c, :], gt[:, c:c + 1], rstd[:], op0=ALU.mult, op1=ALU.mult),
                 reads=[hb[c], gtb, rstdb], writes=[xnb[c]])
        for fc in range(NF1 // 128):
            pi = nxt('ps', NPS)
            for c in range(16):
                P.op('tensor', lambda e, pi=pi, c=c, fc=fc: e.matmul(ps[pi][:], wf[:, c, fc * 128:(fc + 1) * 128], xn[:, c, :],
                                                                      start=(c == 0), stop=(c == 15)),
                     reads=[wfb, xnb[c]], writes=[psb[pi]])
            oi = nxt('ost', 4)
            P.op('scalar', lambda e, oi=oi, pi=pi: e.copy(ost[oi][:], ps[pi][:]), reads=[psb[pi]], writes=[ostb[oi]])
            for (nm, r0, nr, p0, co) in dests[fc]:
                P.dma('sync', X[nm][r0:r0 + nr, co + t0:co + t0 + TH], ost[oi][p0:p0 + nr, :], reads=[ostb[oi]])
        for ts in range(TH // 128):
            pi = nxt('ps', NPS)
            for c in range(16):
                P.op('tensor', lambda e, pi=pi, c=c, ts=ts: e.matmul(ps[pi][:], xn[:, c, ts * 128:(ts + 1) * 128], wt[:, c, :],
                                                                      start=(c == 0), stop=(c == 15)),
                     reads=[wtb, xnb[c]], writes=[psb[pi]])
            oi = nxt('ost', 4)
            P.op('vector', lambda e, oi=oi, pi=pi: e.tensor_copy(ost[oi][:], ps[pi][:]), reads=[psb[pi]], writes=[ostb[oi]])
            tk = t0 + ts * 128
            P.dma('sync', X['gv'][tk:tk + 128, :], ost[oi][:, 0:256], reads=[ostb[oi]])
            P.dma('sync', X['gog'][tk:tk + 128, :], ost[oi][:, 256:512], reads=[ostb[oi]])
    P.finish()


def phase_C2(nc, X):
    P = Prog(nc)
    TH = 512
    xn = [P.sbuf([128, 16, TH], BF16, f'xn{i}') for i in range(2)]; xnb = [P.bufs(16) for _ in range(2)]
    wq = P.sbuf([128, 16, 1536], BF16, 'wq'); wqb = P.buf()
    ost = [P.sbuf([128, TH], F32, f'ost{i}') for i in range(4)]; ostb = P.bufs(4)
    NPS = 6
    ps = [P.psum([128, TH], F32, f'ps{i}') for i in range(NPS)]; psb = P.bufs(NPS)
    ctr = {'ps': 0, 'ost': 0}

    def nxt(k, n):
        v = ctr[k] % n
        ctr[k] += 1
        return v

    wqf = wq[:].rearrange("p c n -> p (c n)")
    for c0 in range(0, 16, 8):
        P.dma('gpsimd', wqf[:, c0 * 1536:(c0 + 8) * 1536], X['wqkv'][:, c0 * 1536:(c0 + 8) * 1536], writes=[wqb])
    def load_xn(tt):
        r, hf, xi = tt // 2, tt % 2, tt % 2
        for c in range(16):
            P.dma('sync', xn[xi][:, c, :], X['gout2'][c // 2][r * 256 + (c % 2) * 128:r * 256 + (c % 2 + 1) * 128, hf * TH:(hf + 1) * TH], writes=[xnb[xi][c]])
    load_xn(0)
    for tt in range(SQ // TH):
        t0 = tt * TH
        xi = tt % 2
        if tt + 1 < SQ // TH:
            load_xn(tt + 1)
        for fc in range(8):
            pi = nxt('ps', NPS)
            for c in range(16):
                P.op('tensor', lambda e, pi=pi, c=c, fc=fc, xi=xi: e.matmul(ps[pi][:], wq[:, c, fc * 128:(fc + 1) * 128], xn[xi][:, c, :],
                                                                             start=(c == 0), stop=(c == 15)),
                     reads=[wqb, xnb[xi][c]], writes=[psb[pi]])
            oi = nxt('ost', 4)
            P.op('scalar', lambda e, oi=oi, pi=pi: e.copy(ost[oi][:], ps[pi][:]), reads=[psb[pi]], writes=[ostb[oi]])
            dst = X['qT'] if fc < 4 else X['kT']
            P.dma('sync', dst[fc % 4, :, t0:t0 + TH], ost[oi][:], reads=[ostb[oi]])
        for ts in range(TH // 128):
            pi = nxt('ps', NPS)
            for c in range(16):
                P.op('tensor', lambda e, pi=pi, c=c, ts=ts, xi=xi: e.matmul(ps[pi][:], xn[xi][:, c, ts * 128:(ts + 1) * 128], wq[:, c, 1024:1536],
                                                                             start=(c == 0), stop=(c == 15)),
                     reads=[wqb, xnb[xi][c]], writes=[psb[pi]])
            oi = nxt('ost', 4)
            P.op('vector', lambda e, oi=oi, pi=pi: e.tensor_copy(ost[oi][:], ps[pi][:]), reads=[psb[pi]], writes=[ostb[oi]])
            tk = t0 + ts * 128
            for hh in range(4):
                P.dma('sync', X['v'][hh, tk:tk + 128, :], ost[oi][:, hh * 128:(hh + 1) * 128], reads=[ostb[oi]])
    P.finish()


S = 4096
HD = 128
NH = 4
NT = S // 128
NEG = -1e30
MBIG = -30000.0


def moba_consts(head0):
    ident = np.eye(128, dtype=np.float32)
    tri = (np.arange(128)[None, :] >= np.arange(128)[:, None]).astype(np.float32)
    maskadd = np.zeros((128, 16, 16), np.float32)
    for bq in range(16):
        maskadd[:, bq, bq:] = NEG
    eneg = np.zeros((16, 16, 128), np.float32)
    for n in range(16):
        eneg[n, n, :] = MBIG
    slopes = np.exp2(-8.0 * np.arange(1, 17, dtype=np.float32) / 16)
    bias = np.zeros((128, NH, NT), np.float32)
    for hh in range(NH):
        for dl in range(NT):
            bias[:, hh, dl] = slopes[head0 + hh] * (np.arange(128) - 64 - 128 * dl)
    return {"ident": ident, "tri": tri, "maskadd": maskadd.reshape(128, 256), "eneg": eneg.reshape(16, 2048),
            "abias": bias.reshape(128, NH * NT)}


def build_moba(nc=None, X=None):
    fused = nc is not None
    if not fused:
        nc = bass.Bass("TRN2", target_bir_lowering=False)
    P = Prog(nc)
    fm = X['gin3'] if fused else None

    def din(name, shape):
        if fused:
            return X[name]
        return nc.dram_tensor(name, list(shape), F32, kind="ExternalInput").ap()

    qT_d = din("qT", [NH, HD, S])
    kT_d = din("kT", [NH, HD, S])
    v_d = din("v", [NH, S, HD])
    ident_d = din("ident", [128, 128])
    tri_d = din("tri", [128, 128])
    maskadd_d = din("maskadd", [128, 256])
    eneg_d = din("eneg", [16, 2048])
    abias_d = din("abias", [128, NH * NT])
    if not fused:
        o_d = nc.dram_tensor("o", [NH, S, HD], F32, kind="ExternalOutput").ap()

    ident = P.sbuf([128, 128], F32, 'identsb'); identb = P.buf()
    tri = P.sbuf([128, 128], BF16, 'trisb'); trib = P.buf()
    maskadd = P.sbuf([128, 16, 16], F32, 'maskaddsb'); maskaddb = P.buf()
    eneg = P.sbuf([16, 16, 128], BF16, 'enegsb'); enegb = P.buf()
    abias = P.sbuf([128, NH, NT], F32, 'abiassb'); abiasb = P.buf()
    P.dma('sync', ident[:], ident_d, writes=[identb])
    P.dma('gpsimd', tri[:], tri_d, writes=[trib])
    P.dma('sync', maskadd[:].rearrange("p a b -> p (a b)"), maskadd_d, writes=[maskaddb])
    P.dma('gpsimd', eneg[:].rearrange("p a b -> p (a b)"), eneg_d, writes=[enegb])
    P.dma('sync', abias[:].rearrange("p a b -> p (a b)"), abias_d, writes=[abiasb])

    q32s = [P.sbuf([128, S], F32, f'q32_{i}') for i in range(2)]; q32bs = P.bufs(2)
    k32s = [P.sbuf([128, S], F32, f'k32_{i}') for i in range(2)]; k32bs = P.bufs(2)
    qbf = P.sbuf([128, S], BF16, 'qbf'); qbfb = P.buf()
    kbf = P.sbuf([128, S], BF16, 'kbf'); kbfb = P.buf()
    vaugs = [P.sbuf([128, NT, 132], BF16, f'vaug{i}') for i in range(2)]; vaugbs = P.bufs(2)
    km = P.sbuf([128, 16], F32, 'km'); kmb = P.buf()
    km2 = P.sbuf([128, 16], F32, 'km2'); km2b = P.buf()
    gm = [P.sbuf([128, 16], F32, f'gm{i}') for i in range(2)]; gmb = P.bufs(2)
    m8 = [P.sbuf([128, 8], F32, f'm8{i}') for i in range(2)]; m8b = P.bufs(2)
    thr = [P.sbuf([128, 1], F32, f'thr{i}') for i in range(2)]; thrb = P.bufs(2)
    nsel = [P.sbuf([128, 16], F32, f'nsel{i}') for i in range(2)]; nselb = P.bufs(2)
    nselT = P.sbuf([16, S], BF16, 'nselT'); nselTb = P.bufs(NT)
    NPT = 8
    pt = [P.sbuf([128, 128], BF16, f'pt{i}') for i in range(NPT)]; ptb = P.bufs(NPT)
    rc = [P.sbuf([128, 1], F32, f'rc{i}') for i in range(2)]; rcb = P.bufs(2)
    osb = [P.sbuf([128, 128], F32, f'osb{i}') for i in range(2)]; osbb = P.bufs(2)
    osT = [P.sbuf([128, 128], F32, f'osT{i}') for i in range(2)]; osTb = P.bufs(2)

    NSL = 4
    sps = [P.psum([128, 128], F32, f'sps{i}') for i in range(NSL)]; spsb = P.bufs(NSL)
    ops_ = [P.psum([128, 132], F32, f'ops{i}') for i in range(2)]; opsb = P.bufs(2)
    gtp = P.psum([128, 144], F32, 'gtp'); gpsb = P.buf(); tpob = gpsb
    gps = gtp[:, 128:144]; tpo = gtp[:, 0:128]
    tps = P.psum([16, 128], F32, 'tps'); tpsb = P.buf()

    scale = float(HD) ** -0.5
    ctr = {'s': 0, 'pt': 0, 'g': 0, 'o': 0}

    def nxt(k, n):
        v = ctr[k] % n
        ctr[k] += 1
        return v

    for i in range(2):
        P.op('vector', lambda e, i=i: e.memset(vaugs[i][:, :, 128:129], 1.0), writes=[vaugbs[i]])

    def load_head(hh):
        q32, q32b, k32, k32b, vaug, vaugb = q32s[hh % 2], q32bs[hh % 2], k32s[hh % 2], k32bs[hh % 2], vaugs[hh % 2], vaugbs[hh % 2]
        for c in range(8):
            P.dma('sync', q32[:, c * 512:(c + 1) * 512], qT_d[hh, :, c * 512:(c + 1) * 512], writes=[q32b])
            P.dma('sync', k32[:, c * 512:(c + 1) * 512], kT_d[hh, :, c * 512:(c + 1) * 512], writes=[k32b])
        vv = v_d[hh].rearrange("(t p) d -> p t d", p=128)
        for c in range(8):
            P.dma('gpsimd', vaug[:, c * 4:(c + 1) * 4, 0:128], vv[:, c * 4:(c + 1) * 4, :], writes=[vaugb])
    load_head(0)

    def head(hh):
        q32, q32b, k32, k32b, vaug, vaugb = q32s[hh % 2], q32bs[hh % 2], k32s[hh % 2], k32bs[hh % 2], vaugs[hh % 2], vaugbs[hh % 2]
        if hh + 1 < NH:
            load_head(hh + 1)
        head_body(hh, q32, q32b, k32, k32b, vaug, vaugb)

    def head_body(hh, q32, q32b, k32, k32b, vaug, vaugb):
        for c in range(4):
            P.op('scalar', lambda e, c=c: e.copy(qbf[:, c * 1024:(c + 1) * 1024], q32[:, c * 1024:(c + 1) * 1024]),
                 reads=[q32b], writes=[qbfb])
            P.op('vector', lambda e, c=c: e.tensor_copy(kbf[:, c * 1024:(c + 1) * 1024], k32[:, c * 1024:(c + 1) * 1024]),
                 reads=[k32b], writes=[kbfb])
        P.op('vector', lambda e: e.tensor_reduce(km[:], k32[:].rearrange("p (n s) -> p n s", s=256), axis=AX.X, op=ALU.add),
             reads=[k32b], writes=[kmb])
        P.op('vector', lambda e: e.tensor_scalar(km2[:], km[:], 1.0 / 256, None, op0=ALU.mult), reads=[kmb], writes=[km2b])
        for qi in range(2, NT):
            bq = qi // 2
            gi = nxt('g', 2)
            P.op('tensor', lambda e, qi=qi: e.matmul(gps, q32[:, qi * 128:(qi + 1) * 128], km2[:], start=True, stop=True),
                 reads=[q32b, km2b], writes=[gpsb])
            P.op('vector', lambda e, gi=gi, bq=bq: e.tensor_tensor(gm[gi][:], gps, maskadd[:, bq, :], op=ALU.add),
                 reads=[gpsb, maskaddb], writes=[gmb[gi]])
            P.op('vector', lambda e, gi=gi: e.max(m8[gi][:], gm[gi][:]), reads=[gmb[gi]], writes=[m8b[gi]])
            P.op('vector', lambda e, gi=gi: e.tensor_scalar(thr[gi][:], m8[gi][:, 2:3], -1e29, None, op0=ALU.max),
                 reads=[m8b[gi]], writes=[thrb[gi]])
            P.op('vector', lambda e, gi=gi: e.tensor_scalar(nsel[gi][:], gm[gi][:], thr[gi][:, 0:1], None, op0=ALU.is_lt),
                 reads=[gmb[gi], thrb[gi]], writes=[nselb[gi]])
            P.op('tensor', lambda e, gi=gi: e.transpose(tps[:], nsel[gi][:], ident[:]),
                 reads=[nselb[gi], identb], writes=[tpsb])
            P.op('scalar', lambda e, qi=qi: e.copy(nselT[:, qi * 128:(qi + 1) * 128], tps[:]),
                 reads=[tpsb], writes=[nselTb[qi]])
        SKEW = 3
        for qi in range(NT):
            bq = qi // 2
            oi = nxt('o', 2)
            pend = []

            def flush_one():
                pi_, kt_ = pend.pop(0)
                P.op('tensor', lambda e, pi_=pi_, oi=oi, kt_=kt_, qi=qi: e.matmul(
                    ops_[oi][:, 0:129], pt[pi_][:], vaug[:, kt_, 0:129], start=(kt_ == 0), stop=(kt_ == qi)),
                    reads=[ptb[pi_], vaugb], writes=[opsb[oi]])
            for kt in range(qi + 1):
                si = nxt('s', NSL)
                spt = sps[si][:]
                own = kt >= 2 * bq
                P.op('tensor', lambda e, spt=spt, kt=kt, qi=qi, own=own: e.matmul(
                    spt, kbf[:, kt * 128:(kt + 1) * 128], qbf[:, qi * 128:(qi + 1) * 128], start=True, stop=own),
                    reads=[kbfb, qbfb], writes=[spsb[si]])
                if not own:
                    P.op('tensor', lambda e, spt=spt, kt=kt, qi=qi: e.matmul(
                        spt, eneg[:, kt // 2, :], nselT[:, qi * 128:(qi + 1) * 128], start=False, stop=True),
                        reads=[enegb, nselTb[qi]], writes=[spsb[si]])
                if len(pend) >= SKEW:
                    flush_one()
                pi = nxt('pt', NPT)
                P.op('scalar', lambda e, pi=pi, spt=spt, hh=hh, dl=qi - kt: e.activation(
                    pt[pi][:], spt, AF.Exp, bias=abias[:, hh, dl:dl + 1], scale=scale),
                    reads=[spsb[si], abiasb], writes=[ptb[pi]])
                if kt == qi:
                    P.op('vector', lambda e, pi=pi: e.tensor_tensor(pt[pi][:], pt[pi][:], tri[:], op=ALU.mult),
                         reads=[ptb[pi], trib], writes=[ptb[pi]])
                pend.append((pi, kt))
            while pend:
                flush_one()
            P.op('vector', lambda e, oi=oi: e.reciprocal(rc[oi][:], ops_[oi][:, 128:129]), reads=[opsb[oi]], writes=[rcb[oi]])
            P.op('vector', lambda e, oi=oi: e.tensor_scalar(osb[oi][:], ops_[oi][:, 0:128], rc[oi][:, 0:1], None, op0=ALU.mult),
                 reads=[opsb[oi], rcb[oi]], writes=[osbb[oi]])
            if not fused:
                P.dma('sync', o_d[hh, qi * 128:(qi + 1) * 128, :], osb[oi][:], reads=[osbb[oi]])
            else:
                P.op('tensor', lambda e, oi=oi: e.transpose(tpo, osb[oi][:], ident[:]), reads=[osbb[oi], identb], writes=[tpob])
                P.op('scalar', lambda e, oi=oi: e.copy(osT[oi][:], tpo), reads=[tpob], writes=[osTb[oi]])
                P.dma('sync', fm[hh // 2][qi // 8][(hh % 2) * 128:(hh % 2 + 1) * 128, (qi % 8) * 128:(qi % 8 + 1) * 128], osT[oi][:], reads=[osTb[oi]])
        if fused and hh % 2 == 1:
            P.wait_all('gpsimd', osTb)
            for tq in range(4):
                P.collective("AllGather", X['gin3'][hh // 2][tq], X['gout3'][hh // 2][tq], [[0, 1, 2, 3], [4, 5, 6, 7]],
                             reads=[P.buf('gi')], writes=[P.buf('go')])
    for hh in range(NH):
        head(hh)
    if fused:
        P.finish()
        return nc
    P.wait_all('sync', osbb)
    P.emit()
    P.close()
    return nc


def moba_ref(q, k, v, head0):
    out = np.zeros_like(q)
    slopes = np.exp2(-8.0 * np.arange(1, 17, dtype=np.float64) / 16)
    t = np.arange(S)
    for hh in range(q.shape[0]):
        qq, kk, vv = q[hh].astype(np.float64), k[hh].astype(np.float64), v[hh].astype(np.float64)
        kmean = kk.reshape(16, 256, HD).mean(1)
        gate = qq @ kmean.T
        blk = t // 256
        gate = np.where(np.arange(16)[None, :] < blk[:, None], gate, -np.inf)
        order = np.argsort(-gate, axis=1)[:, :3]
        selm = np.zeros((S, 16), bool)
        for r in range(3):
            valid = r < blk
            selm[t[valid], order[valid, r]] = True
        selm[t, blk] = True
        for b0 in range(16):
            ts = slice(b0 * 256, (b0 + 1) * 256)
            lg = qq[ts] @ kk[: (b0 + 1) * 256].T * HD ** -0.5
            dist = t[ts][:, None] - t[None, : (b0 + 1) * 256]
            lg = lg - slopes[head0 + hh] * dist
            m = np.repeat(selm[ts, : b0 + 1], 256, axis=1) & (dist >= 0)
            lg = np.where(m, lg, -np.inf)
            p = np.exp(lg - lg.max(1, keepdims=True))
            p /= p.sum(1, keepdims=True)
            out[hh, ts] = p @ vv[: (b0 + 1) * 256]
    return out


SEG = 256
RC = 64
GC = 128
LN_EPS = 64 * 1e-5


def mix_consts():
    ident = np.eye(128, dtype=np.float32)
    s = np.arange(64)[:, None]; t = np.arange(64)[None, :]
    m2 = np.concatenate([(t > s), (t >= s)], axis=1).astype(np.float32)
    m0 = (t < s).astype(np.float32)
    s2 = np.arange(128)[:, None]; t2 = np.arange(128)[None, :]
    gm = (t2 >= s2).astype(np.float32)
    r64 = np.ones((128, SEG), np.float32); r64[:, ::RC] = 0.0
    r128 = np.ones((128, SEG), np.float32); r128[:, ::GC] = 0.0
    return {"ident": ident, "m2": np.tile(m2, (1, 4)), "m0": np.tile(m0, (1, 4)), "gmask": gm, "rst64": r64, "rst128": r128,
            "id4": np.tile(np.eye(64, dtype=np.float32), (1, 4))}


def build_mix(S, nc=None, X=None):
    fused = nc is not None
    if not fused:
        nc = bass.Bass("TRN2", target_bir_lowering=False)
    P = Prog(nc)
    NSEG = S // SEG
    fm = X['gin1'] if fused else None

    def din(name, shape):
        if fused:
            return X[name]
        return nc.dram_tensor(name, list(shape), F32, kind="ExternalInput").ap()

    ident_d = din("ident", [128, 128]); m2_d = din("m2", [64, 512]); m0_d = din("m0", [64, 256]); id4_d = din("id4", [64, 256])
    gmask_d = din("gmask", [128, 128]); rst64_d = din("rst64", [128, SEG]); rst128_d = din("rst128", [128, SEG])
    gq_d = din("gq", [128, S]); gk_d = din("gk", [128, S]); gv_d = din("gv", [S, 256]); gog_d = din("gog", [S, 256])
    glow_d = din("glow", [16, S]); gw2_d = din("gw2", [16, 128]); ggb_d = din("ggb", [128, 1]); gnorm_d = din("gnorm", [128, 256])
    rr_d = din("rr", [4, 64, S + 1]); rk_d = din("rk", [4, 64, S + 1]); rv_d = din("rv", [4, 64, S + 1])
    lw_d = din("lw", [64, S + 1]); la_d = din("la", [64, S + 1]); lg1_d = din("lg1", [128, S + 1]); lg2_d = din("lg2", [32, S + 1])
    hp_d = din("hp", [64, 4 * 10])
    mul_d = din("mul", [128, 4])
    w2_d = din("w2", [64, 256]); a2_d = din("a2", [64, 256]); g2a_d = din("g2a", [128, 256]); g2b_d = din("g2b", [32, 256])
    if not fused:
        og_d = nc.dram_tensor("o_gla", [S, 256], F32, kind="ExternalOutput").ap()
        or_d = nc.dram_tensor("o_rwkv", [4, 64, S], F32, kind="ExternalOutput").ap()

    cnt = [0]

    def T(shape, dt=F32):
        cnt[0] += 1
        return P.sbuf(shape, dt, f'sb{cnt[0]}'), P.buf(f'sb{cnt[0]}')

    def const(d_ap, shape):
        t, b = T(shape)
        P.dma('sync', t[:], d_ap, writes=[b])
        return t, b

    ident, identb = const(ident_d, [128, 128])
    m2, m2b = const(m2_d, [64, 512]); m0, m0b = const(m0_d, [64, 256]); id4, id4b = const(id4_d, [64, 256])
    gmask, gmaskb = const(gmask_d, [128, 128])
    rst64, rst64b = const(rst64_d, [128, SEG]); rst128, rst128b = const(rst128_d, [128, SEG])
    gw2, gw2b = const(gw2_d, [16, 128]); ggb, ggbb = const(ggb_d, [128, 1]); gnorm, gnormb = const(gnorm_d, [128, 256])
    hp, hpb = const(hp_d, [64, 40]); mul, mulb = const(mul_d, [128, 4])
    w2, w2b = const(w2_d, [64, 256]); a2, a2b = const(a2_d, [64, 256])
    g2a, g2ab = const(g2a_d, [128, 256]); g2b_, g2bb = const(g2b_d, [32, 256])
    ones64, ones64b = T([64, 64])
    P.op('vector', lambda e: e.memset(ones64[:], 1.0 / 64), writes=[ones64b])
    onesk, oneskb = T([64, 64])
    P.op('vector', lambda e: e.memset(onesk[:], 1.0), writes=[oneskb])
    omka, omkab = T([64, 4])
    for hh in range(4):
        P.op('vector', lambda e, hh=hh: e.tensor_scalar(omka[:, hh:hh + 1], hp[:, hh * 10 + 6:hh * 10 + 7], -1.0, 1.0,
                                                         op0=ALU.mult, op1=ALU.add), reads=[hpb], writes=[omkab])
    rkm = []
    for hh in range(4):
        t, b = T([64, 64])
        P.op('vector', lambda e, t=t, hh=hh: e.tensor_scalar(t[:], onesk[:], hp[:, hh * 10 + 7:hh * 10 + 8], None, op0=ALU.mult),
             reads=[oneskb, hpb], writes=[b])
        rkm.append((t, b))

    def HP(hh, i):
        return hp[:, hh * 10 + i:hh * 10 + i + 1]

    NPS = 8
    pss = [P.psum([128, 512], F32, f'ps{i}') for i in range(NPS)]; pssb = P.bufs(NPS, 'ps')
    pctr = [0]

    def PS():
        i = pctr[0] % NPS
        pctr[0] += 1
        return pss[i], pssb[i]

    engs = ['vector', 'gpsimd']
    ectr = [0]

    def EW():
        ectr[0] += 1
        return engs[ectr[0] % 2]

    def ew_tt(out, ob, a, ab, b, bb, op, eng=None):
        P.op(eng or 'vector', lambda e: e.tensor_tensor(out, a, b, op=op), reads=[ab, bb], writes=[ob])

    Sg = [T([128, 256]) for _ in range(2)]
    P.op('vector', lambda e: e.memset(Sg[0][0][:], 0.0), writes=[Sg[0][1]])
    Sr = [T([64, 256]) for _ in range(2)]
    Srb = [T([64, 256], BF16) for _ in range(2)]
    P.op('vector', lambda e: e.memset(Sr[0][0][:], 0.0), writes=[Sr[0][1]])
    P.op('vector', lambda e: e.memset(Srb[0][0][:], 0.0), writes=[Srb[0][1]])
    gpar = [0]
    rpar = [0]
    ABm4 = T([64, 512], BF16); AKm4 = T([64, 512], BF16)
    M4 = [T([64, 256], BF16) for _ in range(2)]; MT4 = [T([64, 256], BF16) for _ in range(2)]; TT4 = [T([64, 256], BF16) for _ in range(2)]
    tok4 = T([64, 768], BF16); AkV4 = T([64, 256]); X4 = T([64, 256], BF16); U4 = T([64, 256], BF16)
    y4 = T([64, 4, SEG])

    def TS(p=128, w=SEG):
        return T([p, w])

    g_q = T([128, SEG]); g_k = T([128, SEG]); g_low = T([16, SEG])
    g_sig = TS(); g_la = TS(); g_cb = TS(); g_e = TS(); g_en = TS(); g_qd = TS(); g_ki = TS(); g_dec = TS(); g_kd = TS()
    g_v = [T([128, 256]) for _ in range(2)]; g_og = [T([128, 256]) for _ in range(2)]
    g_att = [T([128, 128]) for _ in range(2)]; g_kdt = [T([128, 128]) for _ in range(2)]
    g_sq = T([128, 256]); g_ss = T([128, 1]); g_rs = T([128, 1]); g_rstd = T([128, 1]); g_on = T([128, 256]); g_so = T([128, 256])
    g_out = [T([128, 256]) for _ in range(2)]
    g_outT = [T([128, 256]) for _ in range(2)]

    l_w = T([64, SEG + 1]); l_a = T([64, SEG + 1]); l_g1 = T([128, SEG + 1]); l_g2 = T([32, SEG + 1])
    l_tmp = T([128, SEG]); l_wl = T([64, SEG]); l_al = T([64, SEG]); l_g1l = T([128, SEG]); l_g2l = T([32, SEG])
    l_th = T([64, SEG]); l_s1 = T([128, SEG]); l_s2 = T([32, SEG])

    class H:
        pass
    sc = H()
    sc.raw = [T([64, SEG + 1]) for _ in range(3)]
    sc.tmp = T([64, SEG]); sc.r = T([64, SEG]); sc.k = T([64, SEG])
    sc.lw = T([64, SEG]); sc.a = T([64, SEG])
    sc.kk2 = T([64, SEG]); sc.inv = T([64, SEG]); sc.kkn = T([64, SEG]); sc.t1 = T([64, SEG]); sc.kp = T([64, SEG]); sc.bb = T([64, SEG])
    sc.cb = T([64, SEG]); sc.cbx = T([64, SEG]); sc.en = T([64, SEG]); sc.ex = T([64, SEG]); sc.dec = T([64, SEG])
    pc = H()
    pc.yc = T([64, SEG]); pc.sq = T([64, SEG]); pc.rs = T([64, SEG]); pc.rstd = T([64, SEG]); pc.out = T([64, SEG])
    hs = []
    for hh in range(4):
        o = H()
        o.v = T([64, SEG]); o.g = T([64, SEG]); o.e = T([64, SEG]); o.rkr = T([64, SEG])
        o.AR = T([64, SEG // RC, 128], BF16)
        o.bt = T([64, SEG], BF16); o.kt = T([64, SEG], BF16); o.bd = T([64, SEG]); o.kd = T([64, SEG])
        hs.append(o)

    def act(out, ob, in_, ib, func, extra_reads=(), eng='scalar', **kw):
        P.op('scalar', lambda e: e.activation(out, in_, func, **kw), reads=[ib] + list(extra_reads), writes=[ob])

    for seg in range(NSEG):
        t0 = seg * SEG
        P.dma('sync', g_q[0][:], gq_d[:, t0:t0 + SEG], writes=[g_q[1]])
        P.dma('sync', g_k[0][:], gk_d[:, t0:t0 + SEG], writes=[g_k[1]])
        P.dma('sync', g_low[0][:], glow_d[:, t0:t0 + SEG], writes=[g_low[1]])
        pz, pzb = PS()
        P.op('tensor', lambda e, pz=pz: e.matmul(pz[:, 0:SEG], gw2[:], g_low[0][:], start=True, stop=True),
             reads=[gw2b, g_low[1]], writes=[pzb])
        act(g_sig[0][:], g_sig[1], pz[:, 0:SEG], pzb, AF.Sigmoid, extra_reads=[ggbb], bias=ggb[:, 0:1])
        act(g_la[0][:], g_la[1], g_sig[0][:], g_sig[1], AF.Ln)
        P.op('vector', lambda e: e.tensor_scalar(g_la[0][:], g_la[0][:], 1.0 / 16, None, op0=ALU.mult), reads=[g_la[1]], writes=[g_la[1]])
        P.op('vector', lambda e: e.tensor_tensor_scan(g_cb[0][:], rst128[:], g_la[0][:], 0.0, op0=ALU.mult, op1=ALU.add),
             reads=[rst128b, g_la[1]], writes=[g_cb[1]])
        act(g_e[0][:], g_e[1], g_cb[0][:], g_cb[1], AF.Exp)
        act(g_en[0][:], g_en[1], g_cb[0][:], g_cb[1], AF.Exp, scale=-1.0)
        P.op('vector', lambda e: e.scalar_tensor_tensor(g_qd[0][:], g_q[0][:], 128.0 ** -0.5, g_e[0][:], op0=ALU.mult, op1=ALU.mult),
             reads=[g_q[1], g_e[1]], writes=[g_qd[1]])
        ew_tt(g_ki[0][:], g_ki[1], g_k[0][:], g_k[1], g_en[0][:], g_en[1], ALU.mult, 'gpsimd')
        for n in range(SEG // GC):
            cs = slice(n * GC, (n + 1) * GC)
            act(g_dec[0][:, cs], g_dec[1], g_cb[0][:, cs], g_cb[1], AF.Exp, scale=-1.0, bias=g_cb[0][:, n * GC + GC - 1:n * GC + GC])
        ew_tt(g_kd[0][:], g_kd[1], g_k[0][:], g_k[1], g_dec[0][:], g_dec[1], ALU.mult, 'gpsimd')
        for n in range(SEG // GC):
            cs = slice(n * GC, (n + 1) * GC)
            tok0 = t0 + n * GC
            vi = n % 2
            gv, gvb = g_v[vi]; gog, gogb = g_og[vi]
            P.dma('sync', gv[:], gv_d[tok0:tok0 + GC, :], writes=[gvb])
            P.dma('sync', gog[:], gog_d[tok0:tok0 + GC, :], writes=[gogb])
            pa, pab = PS()
            P.op('tensor', lambda e, pa=pa, cs=cs: e.matmul(pa[:, 0:128], g_ki[0][:, cs], g_qd[0][:, cs], start=True, stop=True),
                 reads=[g_ki[1], g_qd[1]], writes=[pab])
            att, attb = g_att[vi]
            P.op('vector', lambda e, pa=pa, att=att: e.tensor_tensor(att[:], pa[:, 0:128], gmask[:], op=ALU.mult),
                 reads=[pab, gmaskb], writes=[attb])
            pt_, ptb_ = PS()
            P.op('tensor', lambda e, pt_=pt_, cs=cs: e.transpose(pt_[:, 0:128], g_kd[0][:, cs], ident[:]),
                 reads=[g_kd[1], identb], writes=[ptb_])
            kdt, kdtb = g_kdt[vi]
            P.op('scalar', lambda e, pt_=pt_, kdt=kdt: e.copy(kdt[:], pt_[:, 0:128]), reads=[ptb_], writes=[kdtb])
            So, Sob = Sg[gpar[0]]; Sn, Snb = Sg[1 - gpar[0]]
            po, pob = PS()
            P.op('tensor', lambda e, po=po, att=att, gv=gv: e.matmul(po[:, 0:256], att[:], gv[:], start=True, stop=False),
                 reads=[attb, gvb], writes=[pob])
            P.op('tensor', lambda e, po=po, cs=cs, So=So: e.matmul(po[:, 0:256], g_qd[0][:, cs], So[:], start=False, stop=True),
                 reads=[g_qd[1], Sob], writes=[pob])
            pst, pstb = PS()
            P.op('tensor', lambda e, pst=pst, kdt=kdt, gv=gv: e.matmul(pst[:, 0:256], kdt[:], gv[:], start=True, stop=True),
                 reads=[kdtb, gvb], writes=[pstb])
            cl = n * GC + GC - 1
            P.op('vector', lambda e, pst=pst, So=So, Sn=Sn, cl=cl: e.scalar_tensor_tensor(
                Sn[:], So[:], g_e[0][:, cl:cl + 1], pst[:, 0:256], op0=ALU.mult, op1=ALU.add),
                reads=[Sob, g_e[1], pstb], writes=[Snb])
            gpar[0] = 1 - gpar[0]
            act(g_sq[0][:], g_sq[1], po[:, 0:256], pob, AF.Square)
            P.op('vector', lambda e: e.tensor_reduce(g_ss[0][:], g_sq[0][:], axis=AX.X, op=ALU.add), reads=[g_sq[1]], writes=[g_ss[1]])
            act(g_rs[0][:], g_rs[1], g_ss[0][:], g_ss[1], AF.Sqrt, scale=1.0 / 256, bias=1e-6)
            P.op('vector', lambda e: e.reciprocal(g_rstd[0][:], g_rs[0][:]), reads=[g_rs[1]], writes=[g_rstd[1]])
            P.op('vector', lambda e, po=po: e.scalar_tensor_tensor(g_on[0][:], po[:, 0:256], g_rstd[0][:, 0:1], gnorm[:], op0=ALU.mult, op1=ALU.mult),
                 reads=[pob, g_rstd[1], gnormb], writes=[g_on[1]])
            act(g_so[0][:], g_so[1], gog[:], gogb, AF.Silu)
            go, gob = g_out[vi]
            ew_tt(go[:], gob, g_on[0][:], g_on[1], g_so[0][:], g_so[1], ALU.mult, 'gpsimd')
            if not fused:
                P.dma('sync', og_d[tok0:tok0 + GC, :], go[:], reads=[gob])
            else:
                ptT, ptTb = PS()
                for i in range(2):
                    P.op('tensor', lambda e, ptT=ptT, go=go, i=i: e.transpose(ptT[:, i * 128:(i + 1) * 128], go[:, i * 128:(i + 1) * 128], ident[:]),
                         reads=[gob, identb], writes=[ptTb])
                gT, gTb = g_outT[vi]
                P.op('scalar', lambda e, ptT=ptT, gT=gT: e.copy(gT[:], ptT[:, 0:256]), reads=[ptTb], writes=[gTb])
                for i in range(2):
                    P.dma('sync', fm[0][tok0 // 1024][i * 128:(i + 1) * 128, tok0 % 1024:tok0 % 1024 + GC], gT[:, i * 128:(i + 1) * 128], reads=[gTb])

        for (lt, ld, rows) in ((l_w, lw_d, 64), (l_a, la_d, 64), (l_g1, lg1_d, 128), (l_g2, lg2_d, 32)):
            P.dma('sync', lt[0][:], ld[:, t0:t0 + SEG + 1], writes=[lt[1]])

        def lerp(raw, rawb, out, outb, mu_ap, mub, tmp, tmpb, rows, eng):
            P.op(eng, lambda e: e.tensor_tensor(tmp, raw[:, 0:SEG], raw[:, 1:SEG + 1], op=ALU.subtract), reads=[rawb], writes=[tmpb])
            P.op('vector', lambda e: e.scalar_tensor_tensor(out, tmp, mu_ap, raw[:, 1:SEG + 1], op0=ALU.mult, op1=ALU.add),
                 reads=[tmpb, mub, rawb], writes=[outb])
        lerp(l_w[0][:], l_w[1], l_wl[0][:], l_wl[1], mul[0:64, 0:1], mulb, l_tmp[0][0:64, :], l_tmp[1], 64, 'gpsimd')
        lerp(l_a[0][:], l_a[1], l_al[0][:], l_al[1], mul[0:64, 1:2], mulb, l_tmp[0][0:64, :], l_tmp[1], 64, 'gpsimd')
        lerp(l_g1[0][:], l_g1[1], l_g1l[0][:], l_g1l[1], mul[:, 2:3], mulb, l_tmp[0][:, :], l_tmp[1], 128, 'gpsimd')
        lerp(l_g2[0][:], l_g2[1], l_g2l[0][:], l_g2l[1], mul[0:32, 3:4], mulb, l_tmp[0][0:32, :], l_tmp[1], 32, 'gpsimd')
        act(l_th[0][:], l_th[1], l_wl[0][:], l_wl[1], AF.Tanh)
        act(l_s1[0][:], l_s1[1], l_g1l[0][:], l_g1l[1], AF.Sigmoid)
        act(l_s2[0][:], l_s2[1], l_g2l[0][:], l_g2l[1], AF.Sigmoid)

        for hh in range(4):
            o = hs[hh]
            hc = slice(hh * 64, (hh + 1) * 64)
            for i, dd in enumerate((rr_d, rk_d, rv_d)):
                P.dma('sync', sc.raw[i][0][:], dd[hh, :, t0:t0 + SEG + 1], writes=[sc.raw[i][1]])
            for i, dst in enumerate((sc.r, sc.k, o.v)):
                lerp(sc.raw[i][0][:], sc.raw[i][1], dst[0][:], dst[1], HP(hh, i), hpb, sc.tmp[0][:], sc.tmp[1], 64, 'gpsimd')
            pw, pwb = PS()
            P.op('tensor', lambda e, pw=pw, hc=hc: e.matmul(pw[0:64, 0:SEG], w2[:, hc], l_th[0][:], start=True, stop=True),
                 reads=[w2b, l_th[1]], writes=[pwb])
            act(sc.lw[0][:], sc.lw[1], pw[0:64, 0:SEG], pwb, AF.Sigmoid, extra_reads=[hpb], bias=HP(hh, 3))
            P.op('vector', lambda e, o=o: e.tensor_scalar(sc.lw[0][:], sc.lw[0][:], -float(np.exp(-0.5)), None, op0=ALU.mult),
                 reads=[sc.lw[1]], writes=[sc.lw[1]])
            pa_, pab_ = PS()
            P.op('tensor', lambda e, pa_=pa_, hc=hc: e.matmul(pa_[0:64, 0:SEG], a2[:, hc], l_al[0][:], start=True, stop=True),
                 reads=[a2b, l_al[1]], writes=[pab_])
            act(sc.a[0][:], sc.a[1], pa_[0:64, 0:SEG], pab_, AF.Sigmoid, extra_reads=[hpb], bias=HP(hh, 4))
            pg, pgb = PS()
            P.op('tensor', lambda e, pg=pg, hc=hc: e.matmul(pg[0:64, 0:SEG], g2a[:, hc], l_s1[0][:], start=True, stop=False),
                 reads=[g2ab, l_s1[1]], writes=[pgb])
            P.op('tensor', lambda e, pg=pg, hc=hc: e.matmul(pg[0:64, 0:SEG], g2b_[:, hc], l_s2[0][:], start=False, stop=True),
                 reads=[g2bb, l_s2[1]], writes=[pgb])
            P.op('scalar', lambda e, pg=pg, o=o: e.copy(o.g[0][:], pg[0:64, 0:SEG]), reads=[pgb], writes=[o.g[1]])
            act(sc.kk2[0][:], sc.kk2[1], sc.k[0][:], sc.k[1], AF.Square, extra_reads=[hpb], scale=HP(hh, 5))
            pn, pnb = PS()
            P.op('tensor', lambda e, pn=pn, o=o: e.matmul(pn[0:64, 0:SEG], onesk[:], sc.kk2[0][:], start=True, stop=True),
                 reads=[oneskb, sc.kk2[1]], writes=[pnb])
            act(sc.inv[0][:], sc.inv[1], pn[0:64, 0:SEG], pnb, AF.Sqrt)
            P.op('vector', lambda e, o=o: e.tensor_scalar(sc.inv[0][:], sc.inv[0][:], 1e-12, None, op0=ALU.max), reads=[sc.inv[1]], writes=[sc.inv[1]])
            P.op('vector', lambda e, o=o: e.reciprocal(sc.inv[0][:], sc.inv[0][:]), reads=[sc.inv[1]], writes=[sc.inv[1]])
            P.op('vector', lambda e, o=o, hh=hh: e.scalar_tensor_tensor(sc.kkn[0][:], sc.k[0][:], HP(hh, 5), sc.inv[0][:], op0=ALU.mult, op1=ALU.mult),
                 reads=[sc.k[1], hpb, sc.inv[1]], writes=[sc.kkn[1]])
            P.op('vector', lambda e, o=o, hh=hh: e.tensor_scalar(sc.t1[0][:], sc.a[0][:], HP(hh, 6), omka[:, hh:hh + 1], op0=ALU.mult, op1=ALU.add),
                 reads=[sc.a[1], hpb, omkab], writes=[sc.t1[1]])
            ew_tt(sc.kp[0][:], sc.kp[1], sc.k[0][:], sc.k[1], sc.t1[0][:], sc.t1[1], ALU.mult, 'gpsimd')
            ew_tt(sc.bb[0][:], sc.bb[1], sc.kkn[0][:], sc.kkn[1], sc.a[0][:], sc.a[1], ALU.mult, 'gpsimd')
            P.op('vector', lambda e, o=o: e.tensor_tensor_scan(sc.cb[0][:], rst64[0:64, :], sc.lw[0][:], 0.0, op0=ALU.mult, op1=ALU.add),
                 reads=[rst64b, sc.lw[1]], writes=[sc.cb[1]])
            ew_tt(sc.cbx[0][:], sc.cbx[1], sc.cb[0][:], sc.cb[1], sc.lw[0][:], sc.lw[1], ALU.subtract, 'gpsimd')
            act(o.e[0][:], o.e[1], sc.cb[0][:], sc.cb[1], AF.Exp)
            act(sc.en[0][:], sc.en[1], sc.cb[0][:], sc.cb[1], AF.Exp, scale=-1.0)
            act(sc.ex[0][:], sc.ex[1], sc.cbx[0][:], sc.cbx[1], AF.Exp)
            ARv = o.AR[0][:]
            P.op('vector', lambda e, o=o, ARv=ARv: e.scalar_tensor_tensor(
                ARv[:, :, 0:64], sc.kkn[0][:].rearrange("p (n c) -> p n c", c=RC), -1.0, sc.ex[0][:].rearrange("p (n c) -> p n c", c=RC),
                op0=ALU.mult, op1=ALU.mult), reads=[sc.kkn[1], sc.ex[1]], writes=[o.AR[1]])
            P.op('vector', lambda e, o=o, ARv=ARv: e.tensor_tensor(
                ARv[:, :, 64:128], sc.r[0][:].rearrange("p (n c) -> p n c", c=RC), o.e[0][:].rearrange("p (n c) -> p n c", c=RC),
                op=ALU.mult), reads=[sc.r[1], o.e[1]], writes=[o.AR[1]])
            ew_tt(o.bt[0][:], o.bt[1], sc.bb[0][:], sc.bb[1], sc.en[0][:], sc.en[1], ALU.mult, 'gpsimd')
            ew_tt(o.kt[0][:], o.kt[1], sc.kp[0][:], sc.kp[1], sc.en[0][:], sc.en[1], ALU.mult, 'gpsimd')
            for n in range(SEG // RC):
                cs = slice(n * RC, (n + 1) * RC)
                act(sc.dec[0][:, cs], sc.dec[1], sc.cb[0][:, cs], sc.cb[1], AF.Exp, scale=-1.0, bias=sc.cb[0][:, n * RC + RC - 1:n * RC + RC])
            ew_tt(o.bd[0][:], o.bd[1], sc.bb[0][:], sc.bb[1], sc.dec[0][:], sc.dec[1], ALU.mult, 'gpsimd')
            ew_tt(o.kd[0][:], o.kd[1], sc.kp[0][:], sc.kp[1], sc.dec[0][:], sc.dec[1], ALU.mult, 'gpsimd')
            ew_tt(o.rkr[0][:], o.rkr[1], sc.r[0][:], sc.r[1], sc.kp[0][:], sc.kp[1], ALU.mult, 'gpsimd')

        def H4(t, w):
            return [t[:, hh * w:(hh + 1) * w] for hh in range(4)]

        def chunk_iter(n):
            cs = slice(n * RC, (n + 1) * RC)
            ARs = [hs[hh].AR[0][:, n, :] for hh in range(4)]
            ARb = [hs[hh].AR[1] for hh in range(4)]
            btb = [hs[hh].bt[1] for hh in range(4)]; ktb = [hs[hh].kt[1] for hh in range(4)]
            p1, p1b = PS()
            for hh in range(4):
                P.op('tensor', lambda e, hh=hh, p1=p1: e.matmul(p1[0:64, hh * 128:(hh + 1) * 128], hs[hh].bt[0][:, cs], ARs[hh], start=True, stop=True),
                     reads=[btb[hh], ARb[hh]], writes=[p1b])
            P.op('vector', lambda e, p1=p1: e.tensor_tensor(ABm4[0][:], p1[0:64, 0:512], m2[:], op=ALU.mult), reads=[p1b, m2b], writes=[ABm4[1]])
            p2, p2b = PS()
            for hh in range(4):
                P.op('tensor', lambda e, hh=hh, p2=p2: e.matmul(p2[0:64, hh * 128:(hh + 1) * 128], hs[hh].kt[0][:, cs], ARs[hh], start=True, stop=True),
                     reads=[ktb[hh], ARb[hh]], writes=[p2b])
            P.op('vector', lambda e, p2=p2: e.tensor_tensor(AKm4[0][:], p2[0:64, 0:512], m2[:], op=ALU.mult), reads=[p2b, m2b], writes=[AKm4[1]])
            p3, p3b = PS()
            for hh in range(4):
                P.op('tensor', lambda e, hh=hh, p3=p3: e.matmul(p3[0:64, hh * 64:(hh + 1) * 64], ARs[hh][:, 0:64], hs[hh].bt[0][:, cs], start=True, stop=True),
                     reads=[btb[hh], ARb[hh]], writes=[p3b])
            P.op('vector', lambda e, p3=p3: e.tensor_tensor(M4[0][0][:], p3[0:64, 0:256], m0[:], op=ALU.mult), reads=[p3b, m0b], writes=[M4[0][1]])
            ABv = ABm4[0][:].rearrange("p (h c) -> p h c", c=128)
            AKv = AKm4[0][:].rearrange("p (h c) -> p h c", c=128)
            P.op('gpsimd', lambda e, ABv=ABv: e.tensor_copy(MT4[0][0][:].rearrange("p (h c) -> p h c", c=64), ABv[:, :, 0:64]),
                 reads=[ABm4[1]], writes=[MT4[0][1]])
            P.op('gpsimd', lambda e, ABv=ABv: e.tensor_tensor(TT4[0][0][:].rearrange("p (h c) -> p h c", c=64), ABv[:, :, 0:64],
                                                              id4[:].rearrange("p (h c) -> p h c", c=64), op=ALU.add),
                 reads=[ABm4[1], id4b], writes=[TT4[0][1]])
            for half in range(2):
                p4, p4b = PS()
                for h2 in range(2):
                    hh = half * 2 + h2
                    for i, src in enumerate((hs[hh].v, hs[hh].bd, hs[hh].kd)):
                        P.op('tensor', lambda e, p4=p4, h2=h2, i=i, src=src: e.transpose(p4[0:64, h2 * 192 + i * 64:h2 * 192 + (i + 1) * 64], src[0][:, cs], ident[0:64, 0:64]),
                             reads=[src[1], identb], writes=[p4b])
                P.op('scalar', lambda e, p4=p4, half=half: e.copy(tok4[0][:, half * 384:(half + 1) * 384], p4[0:64, 0:384]), reads=[p4b], writes=[tok4[1]])
            tk = H4(tok4[0], 192)
            ABh = H4(ABm4[0], 128); AKh = H4(AKm4[0], 128)
            pa, pab = PS()
            for hh in range(4):
                P.op('tensor', lambda e, hh=hh, pa=pa: e.matmul(pa[0:64, hh * 64:(hh + 1) * 64], AKh[hh][:, 0:64], tk[hh][:, 0:64], start=True, stop=True),
                     reads=[AKm4[1], tok4[1]], writes=[pab])
            P.op('scalar', lambda e, pa=pa: e.copy(AkV4[0][:], pa[0:64, 0:256]), reads=[pab], writes=[AkV4[1]])
            for i in range(5):
                a_, b_ = i % 2, (i + 1) % 2
                Mh = H4(M4[a_][0], 64); MTh = H4(MT4[a_][0], 64); Mnh = H4(M4[b_][0], 64); TTh = H4(TT4[a_][0], 64)
                pm, pmb = PS()
                for hh in range(4):
                    P.op('tensor', lambda e, hh=hh, pm=pm, MTh=MTh, Mh=Mh: e.matmul(pm[0:64, hh * 64:(hh + 1) * 64], MTh[hh], Mh[hh], start=True, stop=True),
                         reads=[MT4[a_][1], M4[a_][1]], writes=[pmb])
                P.op('vector', lambda e, pm=pm, b_=b_: e.tensor_copy(M4[b_][0][:], pm[0:64, 0:256]), reads=[pmb], writes=[M4[b_][1]])
                if i < 4:
                    pq, pqb = PS()
                    for hh in range(4):
                        P.op('tensor', lambda e, hh=hh, pq=pq, MTh=MTh, Mh=Mh: e.matmul(pq[0:64, hh * 64:(hh + 1) * 64], Mh[hh], MTh[hh], start=True, stop=True),
                             reads=[MT4[a_][1], M4[a_][1]], writes=[pqb])
                    P.op('scalar', lambda e, pq=pq, b_=b_: e.copy(MT4[b_][0][:], pq[0:64, 0:256]), reads=[pqb], writes=[MT4[b_][1]])
                pu, pub = PS()
                for hh in range(4):
                    P.op('tensor', lambda e, hh=hh, pu=pu, Mnh=Mnh, TTh=TTh: e.matmul(pu[0:64, hh * 64:(hh + 1) * 64], Mnh[hh], TTh[hh], start=True, stop=True),
                         reads=[M4[b_][1], TT4[a_][1]], writes=[pub])
                P.op('vector', lambda e, pu=pu, a_=a_, b_=b_: e.tensor_tensor(TT4[b_][0][:], pu[0:64, 0:256], TT4[a_][0][:], op=ALU.add),
                     reads=[pub, TT4[a_][1]], writes=[TT4[b_][1]])
            TTf = H4(TT4[1][0], 64)
            So, Sob = Sr[rpar[0]]; Sn, Snb = Sr[1 - rpar[0]]
            So16, So16b = Srb[rpar[0]]; Sn16, Sn16b = Srb[1 - rpar[0]]
            Soh = H4(So16, 64)
            px, pxb = PS()
            for hh in range(4):
                P.op('tensor', lambda e, hh=hh, px=px, Soh=Soh: e.matmul(px[0:64, hh * 64:(hh + 1) * 64], ARs[hh][:, 0:64], Soh[hh], start=True, stop=True),
                     reads=[ARb[hh], So16b], writes=[pxb])
            P.op('vector', lambda e, px=px: e.tensor_tensor(X4[0][:], px[0:64, 0:256], AkV4[0][:], op=ALU.add), reads=[pxb, AkV4[1]], writes=[X4[1]])
            Xh = H4(X4[0], 64); Uh = H4(U4[0], 64)
            pU, pUb = PS()
            for hh in range(4):
                P.op('tensor', lambda e, hh=hh, pU=pU, Xh=Xh: e.matmul(pU[0:64, hh * 64:(hh + 1) * 64], TTf[hh], Xh[hh], start=True, stop=True),
                     reads=[TT4[1][1], X4[1]], writes=[pUb])
            P.op('scalar', lambda e, pU=pU: e.copy(U4[0][:], pU[0:64, 0:256]), reads=[pUb], writes=[U4[1]])
            pS, pSb = PS()
            for hh in range(4):
                P.op('tensor', lambda e, hh=hh, pS=pS, Uh=Uh: e.matmul(pS[0:64, hh * 64:(hh + 1) * 64], tk[hh][:, 64:128], Uh[hh], start=True, stop=False),
                     reads=[tok4[1], U4[1]], writes=[pSb])
                P.op('tensor', lambda e, hh=hh, pS=pS: e.matmul(pS[0:64, hh * 64:(hh + 1) * 64], tk[hh][:, 128:192], tk[hh][:, 0:64], start=False, stop=True),
                     reads=[tok4[1]], writes=[pSb])
            cl = n * RC + RC - 1
            for hh in range(4):
                P.op('vector', lambda e, hh=hh, pS=pS, So=So, Sn=Sn, cl=cl: e.scalar_tensor_tensor(
                    Sn[:, hh * 64:(hh + 1) * 64], So[:, hh * 64:(hh + 1) * 64], hs[hh].e[0][:, cl:cl + 1], pS[0:64, hh * 64:(hh + 1) * 64],
                    op0=ALU.mult, op1=ALU.add), reads=[Sob, hs[hh].e[1], pSb], writes=[Snb])
            P.op('gpsimd', lambda e, Sn=Sn, Sn16=Sn16: e.tensor_copy(Sn16[:], Sn[:]), reads=[Snb], writes=[Sn16b])
            pY, pYb = PS()
            for hh in range(4):
                P.op('tensor', lambda e, hh=hh, pY=pY, Soh=Soh: e.matmul(pY[0:64, hh * 64:(hh + 1) * 64], Soh[hh], ARs[hh][:, 64:128], start=True, stop=False),
                     reads=[So16b, ARb[hh]], writes=[pYb])
                P.op('tensor', lambda e, hh=hh, pY=pY, Uh=Uh: e.matmul(pY[0:64, hh * 64:(hh + 1) * 64], Uh[hh], ABh[hh][:, 64:128], start=False, stop=False),
                     reads=[U4[1], ABm4[1]], writes=[pYb])
                P.op('tensor', lambda e, hh=hh, pY=pY: e.matmul(pY[0:64, hh * 64:(hh + 1) * 64], tk[hh][:, 0:64], AKh[hh][:, 64:128], start=False, stop=True),
                     reads=[tok4[1], AKm4[1]], writes=[pYb])
            P.op('scalar', lambda e, pY=pY, cs=cs: e.copy(y4[0][:, :, cs], pY[0:64, 0:256].rearrange("p (h c) -> p h c", c=64)), reads=[pYb], writes=[y4[1]])
            rpar[0] = 1 - rpar[0]

        for n in range(SEG // RC):
            chunk_iter(n)

        for hh in range(4):
            o = hs[hh]
            pm, pmb = PS()
            P.op('tensor', lambda e, o=o, pm=pm, hh=hh: e.matmul(pm[0:64, 0:SEG], ones64[:], y4[0][:, hh, :], start=True, stop=True), reads=[ones64b, y4[1]], writes=[pmb])
            P.op('vector', lambda e, o=o, pm=pm, hh=hh: e.tensor_tensor(pc.yc[0][:], y4[0][:, hh, :], pm[0:64, 0:SEG], op=ALU.subtract), reads=[y4[1], pmb], writes=[pc.yc[1]])
            act(pc.sq[0][:], pc.sq[1], pc.yc[0][:], pc.yc[1], AF.Square)
            pv, pvb = PS()
            P.op('tensor', lambda e, o=o, pv=pv: e.matmul(pv[0:64, 0:SEG], ones64[:], pc.sq[0][:], start=True, stop=True), reads=[ones64b, pc.sq[1]], writes=[pvb])
            act(pc.rs[0][:], pc.rs[1], pv[0:64, 0:SEG], pvb, AF.Sqrt, bias=LN_EPS)
            P.op('vector', lambda e, o=o: e.reciprocal(pc.rstd[0][:], pc.rs[0][:]), reads=[pc.rs[1]], writes=[pc.rstd[1]])
            ew_tt(pc.yc[0][:], pc.yc[1], pc.yc[0][:], pc.yc[1], pc.rstd[0][:], pc.rstd[1], ALU.mult, 'gpsimd')
            P.op('vector', lambda e, o=o, hh=hh: e.tensor_scalar(pc.yc[0][:], pc.yc[0][:], HP(hh, 8), HP(hh, 9), op0=ALU.mult, op1=ALU.add),
                 reads=[pc.yc[1], hpb], writes=[pc.yc[1]])
            pb_, pbb = PS()
            P.op('tensor', lambda e, o=o, pb_=pb_, hh=hh: e.matmul(pb_[0:64, 0:SEG], rkm[hh][0][:], o.rkr[0][:], start=True, stop=True),
                 reads=[rkm[hh][1], o.rkr[1]], writes=[pbb])
            P.op('vector', lambda e, o=o, pb_=pb_: e.tensor_tensor(pc.out[0][:], pb_[0:64, 0:SEG], o.v[0][:], op=ALU.mult), reads=[pbb, o.v[1]], writes=[pc.out[1]])
            ew_tt(pc.out[0][:], pc.out[1], pc.out[0][:], pc.out[1], pc.yc[0][:], pc.yc[1], ALU.add, 'gpsimd')
            ew_tt(pc.out[0][:], pc.out[1], pc.out[0][:], pc.out[1], o.g[0][:], o.g[1], ALU.mult, 'gpsimd')
            if not fused:
                P.dma('sync', or_d[hh, :, t0:t0 + SEG], pc.out[0][:], reads=[pc.out[1]])
            else:
                P.dma('sync', fm[1][t0 // 1024][hh * 64:(hh + 1) * 64, t0 % 1024:t0 % 1024 + SEG], pc.out[0][:], reads=[pc.out[1]])

        if fused and (t0 + SEG) % 1024 == 0:
            P.wait_all('gpsimd', [g_outT[0][1], g_outT[1][1], pc.out[1]])
            for rh in range(2):
                P.collective("AllGather", X['gin1'][rh][t0 // 1024], X['gout1'][rh][t0 // 1024], [[0, 1, 2, 3], [4, 5, 6, 7]],
                             reads=[P.buf('gi')], writes=[P.buf('go')])

    if fused:
        P.finish()
        return nc
    P.wait_all('sync', [g_out[0][1], g_out[1][1]] + [pc.out[1]])
    P.emit()
    P.close()
    return nc

GQ, GK, GV, GOG, RR, RK, RV, GLOW, WLOW, ALOW, GLR, NINP = 0, 512, 1024, 2048, 3072, 4096, 5120, 6144, 6160, 6224, 6288, 6528
PERM = np.concatenate([np.arange(0, 3072), np.arange(3088, 6160), np.arange(3072, 3088), np.arange(6160, 6448)])


def pad_win(w_in):
    w = np.zeros((w_in.shape[0], NINP), np.float32)
    w[:, :6448] = w_in[:, PERM]
    return w


def padz(a):
    return np.ascontiguousarray(np.concatenate([np.zeros(a.shape[:-1] + (1,), np.float32), a], axis=-1))


def mix_inputs(pT, prm, j):
    S = pT.shape[1]
    c = np.ascontiguousarray
    d = {}
    d["gq"] = c(pT[GQ + 128 * j:GQ + 128 * j + 128]); d["gk"] = c(pT[GK + 128 * j:GK + 128 * j + 128])
    d["gv"] = c(pT[GV + 256 * j:GV + 256 * j + 256].T); d["gog"] = c(pT[GOG + 256 * j:GOG + 256 * j + 256].T)
    d["glow"] = c(pT[GLOW:GLOW + 16])
    r0 = 256 * j
    d["rr"] = padz(pT[RR + r0:RR + r0 + 256].reshape(4, 64, S)); d["rk"] = padz(pT[RK + r0:RK + r0 + 256].reshape(4, 64, S))
    d["rv"] = padz(pT[RV + r0:RV + r0 + 256].reshape(4, 64, S))
    d["lw"] = padz(pT[WLOW:WLOW + 64]); d["la"] = padz(pT[ALOW:ALOW + 64]); d["lg1"] = padz(pT[GLR:GLR + 128]); d["lg2"] = padz(pT[GLR + 128:GLR + 160])
    d.update(mix_params(prm, j))
    return d


def mix_params(prm, j):
    c = np.ascontiguousarray
    d = {}
    r0 = 256 * j
    d["gw2"] = c(prm['gla_gate_w2'][:, 128 * j:128 * j + 128]); d["ggb"] = c(prm['gla_gate_b'][128 * j:128 * j + 128][:, None])
    d["gnorm"] = c(np.tile(prm['gla_norm'][None, :], (128, 1)))
    mu = prm['rwkv_mu']
    hp = np.zeros((64, 40), np.float32)
    for hh in range(4):
        cols = r0 + 64 * hh + np.arange(64)
        vals = [mu[cols], mu[1024 + cols], mu[2048 + cols], prm['rwkv_w0'][cols], prm['rwkv_a0'][cols], prm['rwkv_k_k'][cols],
                prm['rwkv_k_a'][cols], prm['rwkv_r_k'].reshape(-1)[cols], prm['rwkv_ln_w'][cols], prm['rwkv_ln_b'][cols]]
        for i, v in enumerate(vals):
            hp[:, hh * 10 + i] = v
    d["hp"] = hp
    mul = np.zeros((128, 4), np.float32)
    mul[0:64, 0] = mu[3072:3136]; mul[0:64, 1] = mu[3136:3200]; mul[:, 2] = mu[3200:3328]; mul[0:32, 3] = mu[3328:3360]
    d["mul"] = mul
    d["w2"] = c(prm['rwkv_w2'][:, r0:r0 + 256]); d["a2"] = c(prm['rwkv_a2'][:, r0:r0 + 256])
    d["g2a"] = c(prm['rwkv_g2'][0:128, r0:r0 + 256]); d["g2b"] = c(prm['rwkv_g2'][128:160, r0:r0 + 256])
    return d


SQL = 4096


def build_fused():
    nc = bass.Bass("TRN2", target_bir_lowering=False)
    S = SQL

    def din(name, shape, dt=F32):
        return nc.dram_tensor(name, list(shape), dt, kind="ExternalInput").ap()

    def scr(name, shape, dt=F32):
        return nc.dram_tensor(name, list(shape), dt).ap()

    X = {}
    X['xfull'] = din('xfull', [2048, S]); X['xs'] = din('xs', [2048, 1024]); X['g1'] = din('g1', [128, 16])
    X['w1f'] = din('w1f', [128, 16 * NF1]); X['w1t'] = din('w1t', [128, 16 * NT1])
    for nm, shp in (('ident', [128, 128]), ('m2', [64, 512]), ('m0', [64, 256]), ('id4', [64, 256]), ('gmask', [128, 128]), ('rst64', [128, SEG]), ('rst128', [128, SEG]),
                    ('gw2', [16, 128]), ('ggb', [128, 1]), ('gnorm', [128, 256]), ('hp', [64, 40]), ('mul', [128, 4]),
                    ('w2', [64, 256]), ('a2', [64, 256]), ('g2a', [128, 256]), ('g2b', [32, 256]), ('selm', [128, 4]),
                    ('wo0', [4, 128, 8192]), ('gf0', [128, 16]), ('wg0', [11, 128, 8192]), ('wu0', [11, 128, 8192]), ('wd0', [16, 128, 5632]), ('g20', [128, 16]),
                    ('wqkv', [128, 16 * 1536]), ('tri', [128, 128]), ('maskadd', [128, 256]), ('eneg', [16, 2048]), ('abias', [128, NH * NT]),
                    ('wo1', [4, 128, 8192]), ('gf1', [128, 16]), ('wg1', [11, 128, 8192]), ('wu1', [11, 128, 8192]), ('wd1', [16, 128, 5632]), ('gfin', [128, 16])):
        X[nm] = din(nm, shp)
    X['yT'] = nc.dram_tensor('yT', [2048, 1024], F32, kind="ExternalOutput").ap()
    for nm, shp in (('gq', [128, S]), ('gk', [128, S]), ('glow', [16, S]), ('rr', [256, S + 1]), ('rk', [256, S + 1]), ('rv', [256, S + 1]),
                    ('lw', [64, S + 1]), ('la', [64, S + 1]), ('lg1', [128, S + 1]), ('lg2', [32, S + 1]), ('gv', [S, 256]), ('gog', [S, 256]),
                    ('h2s', [2048, 1024]), ('qT', [4, 128, S]), ('kT', [4, 128, S]), ('v', [4, S, 128])):
        X[nm] = scr(nm, shp)
    for ex in ('1', '3'):
        X['gin' + ex] = [[scr(f'gin{ex}_{rh}_{tq}', [256, 1024]) for tq in range(4)] for rh in range(2)]
        X['gout' + ex] = [[scr(f'gout{ex}_{rh}_{tq}', [1024, 1024]) for tq in range(4)] for rh in range(2)]
    X['gin2'] = [scr(f'gin2_{k}', [256, 1024], BF16) for k in range(8)]
    X['gout2'] = [scr(f'gout2_{k}', [1024, 1024], BF16) for k in range(8)]

    Prog.pool = SemPool(nc)
    phase_A(nc, X)
    XB = dict(X)
    for nm in ('rr', 'rk', 'rv'):
        XB[nm] = X[nm].rearrange("(h k) s -> h k s", k=64)
    build_mix(S, nc=nc, X=XB)
    XC = dict(X); XC.update(hin=X['xs'], gout=X['gout1'], wo=X['wo0'], gf=X['gf0'], wg=X['wg0'], wu=X['wu0'], wd=X['wd0'], g2=X['g20'])
    row_phase(nc, 'C', XC)
    phase_C2(nc, X)
    build_moba(nc=nc, X=X)
    XE = dict(X); XE.update(hin=X['h2s'], gout=X['gout3'], wo=X['wo1'], gf=X['gf1'], wg=X['wg1'], wu=X['wu1'], wd=X['wd1'], g2=X['gfin'])
    row_phase(nc, 'E', XE)
    Prog.pool.close()
    Prog.pool = None
    return nc


_FUSED_NC = []


def kernel(**inp):
    f32 = np.float32
    inp = {k: np.asarray(v, dtype=f32) for k, v in inp.items()}
    x = inp['x']
    c = np.ascontiguousarray
    cores = [(b, j) for b in range(2) for j in range(4)]
    w_in = inp['mix_in_w'][0]
    RB = 3088
    prm = {k: inp[k][0] for k in ['gla_gate_w2', 'gla_gate_b', 'gla_norm', 'rwkv_mu', 'rwkv_w0', 'rwkv_w2', 'rwkv_a0', 'rwkv_a2',
                                  'rwkv_g2', 'rwkv_k_k', 'rwkv_k_a', 'rwkv_r_k', 'rwkv_ln_w', 'rwkv_ln_b']}
    mc = mix_consts()
    xT = [c(x[b].T) for b in range(2)]
    wo0 = inp['mix_out_w'][0]
    perm = np.concatenate([np.concatenate([np.arange(256 * j, 256 * j + 256), np.arange(1024 + 256 * j, 1024 + 256 * j + 256)]) for j in range(4)])
    wo0p = c(wo0[perm])
    wqkv_full = inp['attn_qkv_w'][0]
    shared = {"g1": gtab(inp['norm_mix'][0]), "wo0": wgroups(wo0p, 512), "gf0": gtab(inp['norm_ffn'][0]),
              "wg0": wgroups(inp['ffn_gate_w'][0], 512), "wu0": wgroups(inp['ffn_up_w'][0], 512), "wd0": wgroups(inp['ffn_down_w'][0], 128),
              "g20": gtab(inp['norm_mix'][1]), "wo1": wgroups(inp['attn_out_w'][0], 512), "gf1": gtab(inp['norm_ffn'][1]),
              "wg1": wgroups(inp['ffn_gate_w'][1], 512), "wu1": wgroups(inp['ffn_up_w'][1], 512), "wd1": wgroups(inp['ffn_down_w'][1], 128),
              "gfin": gtab(inp['norm_final'])}
    maps = []
    for (b, j) in cores:
        d = dict(shared)
        d["xfull"] = xT[b]
        d["xs"] = c(xT[b][:, j * 1024:(j + 1) * 1024])
        cols = np.concatenate([np.arange(128 * j, 128 * j + 128), 512 + np.arange(128 * j, 128 * j + 128),
                               RB + np.arange(256 * j, 256 * j + 256), RB + 1024 + np.arange(256 * j, 256 * j + 256),
                               RB + 2048 + np.arange(256 * j, 256 * j + 256), RB + 3072 + np.arange(64), RB + 3136 + np.arange(64),
                               RB + 3200 + np.arange(128), RB + 3328 + np.arange(32), 3072 + np.arange(16)])
        w1f = np.zeros((2048, NF1), f32)
        w1f[:, :cols.size] = w_in[:, cols]
        d["w1f"] = wgroups(w1f, NF1)[0]
        d["w1t"] = wgroups(np.concatenate([w_in[:, 1024 + 256 * j:1024 + 256 * j + 256], w_in[:, 2048 + 256 * j:2048 + 256 * j + 256]], axis=1), NT1)[0]
        mi = mix_params(prm, j)
        d.update(mi)
        d.update(mc)
        selm = np.zeros((128, 4), f32); selm[:, j] = 1.0
        d["selm"] = selm
        hc = np.arange(512 * j, 512 * j + 512)
        d["wqkv"] = wgroups(np.concatenate([wqkv_full[:, hc], wqkv_full[:, 2048 + hc], wqkv_full[:, 4096 + hc]], axis=1), 1536)[0]
        mb = moba_consts(4 * j)
        for k_ in ('tri', 'maskadd', 'eneg', 'abias'):
            d[k_] = mb[k_]
        maps.append(d)
    if not _FUSED_NC:
        _FUSED_NC.append(build_fused())
    res = run_bass_kernel_spmd(_FUSED_NC[0], maps, core_ids=list(range(8))).results
    out = np.zeros(x.shape, f32)
    for i, (b, j) in enumerate(cores):
        out[b, j * 1024:(j + 1) * 1024] = res[i]["yT"].T
    return out
```

```python
import numpy as np
from contextlib import ExitStack
import concourse.bass as bass
import concourse.mybir as mybir
from concourse.bass_utils import run_bass_kernel_spmd

F32 = mybir.dt.float32
BF16 = mybir.dt.bfloat16
AF = mybir.ActivationFunctionType
ALU = mybir.AluOpType
AX = mybir.AxisListType
ENGS = ['tensor', 'vector', 'scalar', 'gpsimd', 'sync']


class Buf:
    __slots__ = ('name', 'last_w', 'readers', 'dsem', 'dcount')

    def __init__(self, name):
        self.name = name
        self.last_w = None
        self.readers = []
        self.dsem = None
        self.dcount = 0


class Op:
    __slots__ = ('eng', 'seq', 'fn', 'waits', 'signals', 'sigidx', 'kind', 'dsem', 'dval', 'inc')

    def __init__(self, eng, seq, fn, kind):
        self.eng = eng
        self.seq = seq
        self.fn = fn
        self.waits = []
        self.signals = False
        self.sigidx = None
        self.kind = kind
        self.dsem = None
        self.dval = 0
        self.inc = 16


class SemPool:
    def __init__(self, nc):
        self.nc = nc
        self.es = ExitStack()
        self.free = []
        self.n = 0

    def new(self, tag):
        self.n += 1
        return self.es.enter_context(self.nc.semaphore(f'{tag}{self.n}'))

    def get(self):
        if self.free:
            return self.free.pop()
        return [self.new('dp'), 0]

    def put(self, sem, count):
        self.free.append([sem, count])

    def close(self):
        self.es.close()


class Prog:
    pool = None

    def __init__(self, nc):
        self.nc = nc
        self.es = ExitStack()
        self.ops = {e: [] for e in ENGS}
        Prog._uid = getattr(Prog, '_uid', 0) + 1
        self.uid = Prog._uid
        if Prog.pool is not None:
            self.sem = {e: Prog.pool.new('ps_' + e) for e in ENGS}
        else:
            self.sem = {e: self.es.enter_context(nc.semaphore(f'sem{self.uid}_' + e)) for e in ENGS}
        self.seen = {e: {} for e in ENGS}
        self.nsem = 0
        self.ntens = 0
        self.dsems = []
        self.dbufs = []

    def sbuf(self, shape, dt, name=None):
        self.ntens += 1
        return self.es.enter_context(self.nc.sbuf_tensor(f'u{self.uid}_' + (name or f't{self.ntens}'), list(shape), dt))

    def psum(self, shape, dt=F32, name=None):
        self.ntens += 1
        return self.es.enter_context(self.nc.psum_tensor(f'u{self.uid}_' + (name or f'p{self.ntens}'), list(shape), dt))

    def buf(self, name='b'):
        return Buf(name)

    def bufs(self, n, name='b'):
        return [Buf(f'{name}{i}') for i in range(n)]

    def _collect(self, eng, reads, writes):
        deps = []
        for b in reads:
            if b.last_w is not None:
                deps.append(b.last_w)
        for b in writes:
            if b.last_w is not None:
                deps.append(b.last_w)
            deps.extend(b.readers)
        waits = []
        seen = self.seen[eng]
        for d in deps:
            if d.kind == 'c':
                if d.eng == eng and eng == 'tensor':
                    continue
                key = ('e', d.eng)
                if seen.get(key, 0) >= d.seq:
                    continue
                seen[key] = d.seq
                d.signals = True
                waits.append(d)
            else:
                key = ('d', id(d.dsem))
                if seen.get(key, 0) >= d.dval:
                    continue
                seen[key] = d.dval
                waits.append(d)
        return waits

    def _commit(self, op, reads, writes):
        for b in reads:
            b.readers.append(op)
        for b in writes:
            b.last_w = op
            b.readers = []

    def op(self, eng, fn, reads=(), writes=()):
        o = Op(eng, len(self.ops[eng]) + 1, fn, 'c')
        o.waits = self._collect(eng, reads, writes)
        self.ops[eng].append(o)
        self._commit(o, reads, writes)
        return o

    def _dsem(self, cb):
        if cb.dsem is None:
            self.nsem += 1
            if Prog.pool is not None:
                cb.dsem, cb.dcount = Prog.pool.get()
            else:
                cb.dsem = self.es.enter_context(self.nc.semaphore(f'd{self.uid}_{self.nsem}'))
            self.dsems.append(cb.dsem)
            self.dbufs.append(cb)

    def dma(self, q, out, in_, reads=(), writes=(), **kw):
        cb = writes[0] if writes else reads[0]
        self._dsem(cb)
        o = Op(q, len(self.ops[q]) + 1, None, 'd')
        o.waits = self._collect(q, reads, writes)
        cb.dcount += 16
        o.dsem = cb.dsem
        o.dval = cb.dcount
        o.fn = lambda e, out=out, in_=in_, kw=kw: e.dma_start(out=out, in_=in_, **kw)
        self.ops[q].append(o)
        self._commit(o, reads, writes)
        return o

    def collective(self, kind, in_ap, out_ap, groups, reads=(), writes=()):
        cb = writes[0]
        self._dsem(cb)
        o = Op('gpsimd', len(self.ops['gpsimd']) + 1, None, 'd')
        o.waits = self._collect('gpsimd', reads, writes)
        cb.dcount += 1
        o.dsem = cb.dsem
        o.dval = cb.dcount
        o.inc = 1
        o.fn = lambda e: e.collective_compute(kind, ALU.bypass, replica_groups=groups, ins=[in_ap], outs=[out_ap])
        self.ops['gpsimd'].append(o)
        self._commit(o, reads, writes)
        return o

    def wait_all(self, eng, bufs):
        o = Op(eng, len(self.ops[eng]) + 1, None, 'w')
        reads = list(bufs)
        o.waits = self._collect(eng, reads, reads)
        self.ops[eng].append(o)
        return o

    def emit(self):
        for e in ENGS:
            c = 0
            for o in self.ops[e]:
                if o.kind == 'c' and o.signals:
                    c += 1
                    o.sigidx = c
        with self.nc.Block() as block:
            for e in ENGS:
                def body(eh, e=e):
                    for o in self.ops[e]:
                        ws = {}
                        for d in o.waits:
                            if d.kind == 'c':
                                s, v = self.sem[d.eng], d.sigidx
                            else:
                                s, v = d.dsem, d.dval
                            k = id(s)
                            if k not in ws or ws[k][1] < v:
                                ws[k] = (s, v)
                        for s, v in ws.values():
                            eh.wait_ge(s, v)
                        if o.kind == 'w':
                            continue
                        ins = o.fn(eh)
                        if o.kind == 'd':
                            ins.then_inc(o.dsem, o.inc)
                        elif o.signals:
                            ins.then_inc(self.sem[e], 1)
                getattr(block, e)(body)

    def close(self):
        self.es.close()

    def finish(self):
        self.wait_all('sync', list(self.dbufs))
        self.emit()
        if Prog.pool is not None:
            for b in self.dbufs:
                Prog.pool.put(b.dsem, b.dcount)
        self.close()

    def stats(self):
        return {e: len(self.ops[e]) for e in ENGS}


D = 2048
FF = 5632
TC = 1024
TH = 512
RMS_EPS = 1e-6
GROUPS = [[0, 1, 2, 3], [4, 5, 6, 7]]


def wgroups(W, GW):
    K, N = W.shape
    KC, NG = K // 128, N // GW
    return np.ascontiguousarray(W.reshape(KC, 128, NG, GW).transpose(2, 1, 0, 3).reshape(NG, 128, KC * GW))


def gtab(g):
    return np.ascontiguousarray(np.asarray(g, np.float32).reshape(16, 128).T)


def row_phase(nc, mode, X):
    P = Prog(nc)
    hT_d = X['hin']; gout_d = X['gout']; selm_d = X['selm']
    wo_d = X['wo']; gf_d = X['gf']; wg_d = X['wg']; wu_d = X['wu']; wd_d = X['wd']; g2_d = X['g2']

    h = P.sbuf([128, 16, TH], F32, 'h'); hb = P.bufs(16, 'h')
    xn = P.sbuf([128, 16, TH], BF16, 'xn'); xnb = P.bufs(16, 'xn')
    ones = P.sbuf([128, 128], BF16, 'ones'); onesb = P.buf('ones')
    sq = [P.sbuf([128, TH], BF16, f'sq{i}') for i in range(2)]; sqb = P.bufs(2, 'sq')
    rs = P.sbuf([128, TH], F32, 'rs'); rsb = P.buf('rs')
    rstd = P.sbuf([128, TH], F32, 'rstd'); rstdb = P.buf('rstd')
    NW = 3
    wbuf = [P.sbuf([128, 8192], BF16, f'wbuf{i}') for i in range(NW)]; wb = P.bufs(NW, 'w')
    ost = [P.sbuf([128, TH], F32, f'ost{i}') for i in range(3)]; ostb = P.bufs(3, 'ost')
    NPS = 6
    ps = [P.psum([128, TH], F32, f'ps{i}') for i in range(NPS)]; psb = P.bufs(NPS, 'ps')
    ssps = P.psum([128, TH], F32, 'ssps'); sspsb = P.buf('ssps')
    gt1 = P.sbuf([128, 16], F32, 'gt1'); gt1b = P.buf('gt1')
    gt2 = P.sbuf([128, 16], F32, 'gt2'); gt2b = P.buf('gt2')
    selm = P.sbuf([128, 4], F32, 'selm'); selmb = P.buf('selm')
    act = P.sbuf([128, 44, TH], BF16, 'act'); actb = P.bufs(44, 'act')
    sg = [P.sbuf([128, TH], F32, f'sg{i}') for i in range(2)]; sgb = P.bufs(2, 'sg')
    cand = [P.sbuf([128, TH], F32, f'cand{i}') for i in range(4)]; candb = P.bufs(4, 'cand')
    acc = P.sbuf([128, TH], F32, 'acc'); accb = P.buf('acc')

    st = {'w': 0, 'ps': 0, 'ost': 0, 'sq': 0, 'sg': 0}

    def nxt(k, n):
        v = st[k] % n
        st[k] += 1
        return v

    P.op('vector', lambda e: e.memset(ones[:], 1.0), writes=[onesb])
    P.dma('sync', gt1[:], gf_d, writes=[gt1b])
    P.dma('sync', gt2[:], g2_d, writes=[gt2b])
    P.dma('sync', selm[:], selm_d, writes=[selmb])

    def load_w(Wd_ap, KC, g, GW):
        wi = nxt('w', NW)
        wt = wbuf[wi][:, 0:KC * GW].rearrange("p (c n) -> p c n", n=GW)
        P.dma('gpsimd', wbuf[wi][:, 0:KC * GW], Wd_ap[g], writes=[wb[wi]])
        return wt, wb[wi]

    def linear(xin, xinb, KC, Wd_ap, N, GW, consume):
        ng = (N + GW - 1) // GW
        for g in range(ng):
            gw = min(GW, N - g * GW)
            wt, wtb = load_w(Wd_ap, KC, g, gw)
            for s in range(gw // 128):
                j = (g * GW) // 128 + s
                pi = nxt('ps', NPS)
                for c in range(KC):
                    P.op('tensor', lambda e, pi=pi, wt=wt, c=c, s=s: e.matmul(
                        ps[pi][:], wt[:, c, s * 128:(s + 1) * 128], xin[:, c, :], start=(c == 0), stop=(c == KC - 1)),
                        reads=[wtb, xinb[c]], writes=[psb[pi]])
                consume(j, ps[pi], psb[pi])

    def sumsq_rstd():
        for c in range(16):
            si = nxt('sq', 2)
            P.op('scalar', lambda e, si=si, c=c: e.activation(sq[si][:], h[:, c, :], AF.Square),
                 reads=[hb[c]], writes=[sqb[si]])
            P.op('tensor', lambda e, si=si, c=c: e.matmul(ssps[:], ones[:], sq[si][:], start=(c == 0), stop=(c == 15)),
                 reads=[onesb, sqb[si]], writes=[sspsb])
        P.op('scalar', lambda e: e.activation(rs[:], ssps[:], AF.Sqrt, bias=RMS_EPS, scale=1.0 / D),
             reads=[sspsb], writes=[rsb])
        P.op('vector', lambda e: e.reciprocal(rstd[:], rs[:]), reads=[rsb], writes=[rstdb])

    def rmsnorm(gt, gtb):
        sumsq_rstd()
        for c in range(16):
            P.op('vector', lambda e, c=c: e.scalar_tensor_tensor(
                xn[:, c, :], h[:, c, :], gt[:, c:c + 1], rstd[:], op0=ALU.mult, op1=ALU.mult),
                reads=[hb[c], gtb, rstdb], writes=[xnb[c]])

    def res_add(j, pst, pstb):
        P.op('vector', lambda e, j=j, pst=pst: e.tensor_tensor(h[:, j, :], pst[:], h[:, j, :], op=ALU.add),
             reads=[pstb, hb[j]], writes=[hb[j]])

    for half in range(TC // TH):
        t0 = half * TH
        for c in range(16):
            P.dma('sync', h[:, c, :], hT_d[c * 128:(c + 1) * 128, t0:t0 + TH], writes=[hb[c]])
        for c in range(16):
            for r in range(4):
                rk_, lr_ = c // 4, (c % 4) * 128
                P.dma('scalar' if r % 2 else 'sync', cand[r][:], gout_d[lr_ // 256][r][rk_ * 256 + lr_ % 256:rk_ * 256 + lr_ % 256 + 128, t0:t0 + TH], writes=[candb[r]])
            P.op('vector', lambda e: e.tensor_scalar(acc[:], cand[0][:], selm[:, 0:1], None, op0=ALU.mult),
                 reads=[candb[0], selmb], writes=[accb])
            for r in (1, 2):
                P.op('vector', lambda e, r=r: e.scalar_tensor_tensor(acc[:], cand[r][:], selm[:, r:r + 1], acc[:], op0=ALU.mult, op1=ALU.add),
                     reads=[candb[r], selmb, accb], writes=[accb])
            P.op('vector', lambda e, c=c: e.scalar_tensor_tensor(act[:, c, :], cand[3][:], selm[:, 3:4], acc[:], op0=ALU.mult, op1=ALU.add),
                 reads=[candb[3], selmb, accb], writes=[actb[c]])
        linear(act, actb, 16, wo_d, D, 512, res_add)
        rmsnorm(gt1, gt1b)
        for g in range(FF // 512):
            wgt, wgb = load_w(wg_d, 16, g, 512)
            wut, wub = load_w(wu_d, 16, g, 512)
            for s in range(4):
                j = g * 4 + s
                pg = nxt('ps', NPS)
                for c in range(16):
                    P.op('tensor', lambda e, pg=pg, c=c, s=s, wgt=wgt: e.matmul(
                        ps[pg][:], wgt[:, c, s * 128:(s + 1) * 128], xn[:, c, :], start=(c == 0), stop=(c == 15)),
                        reads=[wgb, xnb[c]], writes=[psb[pg]])
                pu = nxt('ps', NPS)
                for c in range(16):
                    P.op('tensor', lambda e, pu=pu, c=c, s=s, wut=wut: e.matmul(
                        ps[pu][:], wut[:, c, s * 128:(s + 1) * 128], xn[:, c, :], start=(c == 0), stop=(c == 15)),
                        reads=[wub, xnb[c]], writes=[psb[pu]])
                si = nxt('sg', 2)
                P.op('scalar', lambda e, si=si, pg=pg: e.activation(sg[si][:], ps[pg][:], AF.Silu),
                     reads=[psb[pg]], writes=[sgb[si]])
                P.op('vector', lambda e, si=si, pu=pu, j=j: e.tensor_tensor(act[:, j, :], ps[pu][:], sg[si][:], op=ALU.mult),
                     reads=[psb[pu], sgb[si]], writes=[actb[j]])
        linear(act, actb, 44, wd_d, D, 128, res_add)
        if mode == 'C':
            for c in range(16):
                P.dma('sync', X['h2s'][c * 128:(c + 1) * 128, t0:t0 + TH], h[:, c, :], reads=[hb[c]])
            rmsnorm(gt2, gt2b)
            for c in range(16):
                P.dma('sync', X['gin2'][c // 2][(c % 2) * 128:(c % 2 + 1) * 128, t0:t0 + TH], xn[:, c, :], reads=[xnb[c]])
        else:
            sumsq_rstd()
            for c in range(16):
                oi = nxt('ost', 3)
                P.op('vector', lambda e, c=c, oi=oi: e.scalar_tensor_tensor(
                    ost[oi][:], h[:, c, :], gt2[:, c:c + 1], rstd[:], op0=ALU.mult, op1=ALU.add if False else ALU.mult),
                    reads=[hb[c], gt2b, rstdb], writes=[ostb[oi]])
                P.dma('sync', X['yT'][c * 128:(c + 1) * 128, t0:t0 + TH], ost[oi][:], reads=[ostb[oi]])
    if mode == 'C':
        g2b_ = P.buf('gin2all'); go2b = P.buf('gout2')
        P.wait_all('gpsimd', list(P.dbufs))
        for k_ in range(8):
            P.collective("AllGather", X['gin2'][k_], X['gout2'][k_], GROUPS, reads=[g2b_], writes=[P.buf('gout2')])
    P.finish()


SQ = 4096
NF1 = 1408
NT1 = 512


def _proj_common(P, nk_f, nk_t):
    pass


def phase_A(nc, X):
    P = Prog(nc)
    TH = 512
    h2 = [P.sbuf([128, 16, TH], F32, f'h{i}') for i in range(2)]; hb2 = [P.bufs(16, 'h') for _ in range(2)]
    xn = P.sbuf([128, 16, TH], BF16, 'xn'); xnb = P.bufs(16, 'xn')
    ones = P.sbuf([128, 128], BF16, 'ones'); onesb = P.buf()
    sq = [P.sbuf([128, TH], BF16, f'sq{i}') for i in range(2)]; sqb = P.bufs(2)
    rs = P.sbuf([128, TH], F32, 'rs'); rsb = P.buf()
    rstd = P.sbuf([128, TH], F32, 'rstd'); rstdb = P.buf()
    wf = P.sbuf([128, 16, NF1], BF16, 'wf'); wfb = P.buf()
    wt = P.sbuf([128, 16, NT1], BF16, 'wt'); wtb = P.buf()
    ost = [P.sbuf([128, TH], F32, f'ost{i}') for i in range(4)]; ostb = P.bufs(4)
    gt = P.sbuf([128, 16], F32, 'gt'); gtb = P.buf()
    zt = P.sbuf([128, 1], F32, 'zt'); ztb = P.buf()
    NPS = 6
    ps = [P.psum([128, TH], F32, f'ps{i}') for i in range(NPS)]; psb = P.bufs(NPS)
    ssps = P.psum([128, TH], F32, 'ssps'); sspsb = P.buf()
    ctr = {'ps': 0, 'ost': 0, 'sq': 0}

    def nxt(k, n):
        v = ctr[k] % n
        ctr[k] += 1
        return v

    P.op('vector', lambda e: e.memset(ones[:], 1.0), writes=[onesb])
    P.op('vector', lambda e: e.memset(zt[:], 0.0), writes=[ztb])
    P.dma('sync', gt[:], X['g1'], writes=[gtb])
    wff = wf[:].rearrange("p c n -> p (c n)"); wtf = wt[:].rearrange("p c n -> p (c n)")
    for c0 in range(0, 16, 8):
        P.dma('gpsimd', wff[:, c0 * NF1:(c0 + 8) * NF1], X['w1f'][:, c0 * NF1:(c0 + 8) * NF1], writes=[wfb])
    P.dma('gpsimd', wtf, X['w1t'], writes=[wtb])
    for nm, rows in (('rr', 256), ('rk', 256), ('rv', 256), ('lw', 64), ('la', 64), ('lg1', 128), ('lg2', 32)):
        for r0 in range(0, rows, 128):
            n = min(128, rows - r0)
            P.dma('sync', X[nm][r0:r0 + n, 0:1], zt[0:n, :], reads=[ztb], allow_slow_non_contiguous=True)
    dests = [
        [('gq', 0, 128, 0, 0)], [('gk', 0, 128, 0, 0)],
        [('rr', 0, 128, 0, 1)], [('rr', 128, 128, 0, 1)], [('rk', 0, 128, 0, 1)], [('rk', 128, 128, 0, 1)],
        [('rv', 0, 128, 0, 1)], [('rv', 128, 128, 0, 1)],
        [('lw', 0, 64, 0, 1), ('la', 0, 64, 64, 1)], [('lg1', 0, 128, 0, 1)],
        [('lg2', 0, 32, 0, 1), ('glow', 0, 16, 32, 0)],
    ]
    def load_x(tt):
        for c in range(16):
            P.dma('sync', h2[tt % 2][:, c, :], X['xfull'][c * 128:(c + 1) * 128, tt * TH:(tt + 1) * TH], writes=[hb2[tt % 2][c]])
    load_x(0)
    for tt in range(SQ // TH):
        t0 = tt * TH
        if tt + 1 < SQ // TH:
            load_x(tt + 1)
        h = h2[tt % 2]; hb = hb2[tt % 2]
        for c in range(16):
            si = nxt('sq', 2)
            P.op('scalar', lambda e, si=si, c=c, h=h: e.activation(sq[si][:], h[:, c, :], AF.Square), reads=[hb[c]], writes=[sqb[si]])
            P.op('tensor', lambda e, si=si, c=c: e.matmul(ssps[:], ones[:], sq[si][:], start=(c == 0), stop=(c == 15)),
                 reads=[onesb, sqb[si]], writes=[sspsb])
        P.op('scalar', lambda e: e.activation(rs[:], ssps[:], AF.Sqrt, bias=RMS_EPS, scale=1.0 / D), reads=[sspsb], writes=[rsb])
        P.op('vector', lambda e: e.reciprocal(rstd[:], rs[:]), reads=[rsb], writes=[rstdb])
        for c in range(16):
            P.op('vector', lambda e, c=c, h=h: e.scalar_tensor_tensor(xn[:, c, :], h[:, c, :], gt[:, c:c + 1], rstd[:], op0=ALU.mult, op1=ALU.mult),
                 reads=[hb[c], gtb, rstdb], writes=[xnb[c]])
        for fc in range(NF1 // 128):
            pi = nxt('ps', NPS)
            for c in range(16):
                P.op('tensor', lambda e, pi=pi, c=c, fc=fc: e.matmul(ps[pi][:], wf[:, c, fc * 128:(fc + 1) * 128], xn[:, c, :],
                                                                      start=(c == 0), stop=(c == 15)),
                     reads=[wfb, xnb[c]], writes=[psb[pi]])
            oi = nxt('ost', 4)
            P.op('scalar', lambda e, oi=oi, pi=pi: e.copy(ost[oi][:], ps[pi][:]), reads=[psb[pi]], writes=[ostb[oi]])
            for (nm, r0, nr, p0, co) in dests[fc]:
                P.dma('sync', X[nm][r0:r0 + nr, co + t0:co + t0 + TH], ost[oi][p0:p0 + nr, :], reads=[ostb[oi]])
        for ts in range(TH // 128):
            pi = nxt('ps', NPS)
            for c in range(16):
                P.op('tensor', lambda e, pi=pi, c=c, ts=ts: e.matmul(ps[pi][:], xn[:, c, ts * 128:(ts + 1) * 128], wt[:, c, :],
                                                                      start=(c == 0), stop=(c == 15)),
                     reads=[wtb, xnb[c]], writes=[psb[pi]])
            oi = nxt('ost', 4)
            P.op('vector', lambda e, oi=oi, pi=pi: e.tensor_copy(ost[oi][:], ps[pi][:]), reads=[psb[pi]], writes=[ostb[oi]])
            tk = t0 + ts * 128
            P.dma('sync', X['gv'][tk:tk + 128, :], ost[oi][:, 0:256], reads=[ostb[oi]])
            P.dma('sync', X['gog'][tk:tk + 128, :], ost[oi][:, 256:512], reads=[ostb[oi]])
    P.finish()


def phase_C2(nc, X):
    P = Prog(nc)
    TH = 512
    xn = [P.sbuf([128, 16, TH], BF16, f'xn{i}') for i in range(2)]; xnb = [P.bufs(16) for _ in range(2)]
    wq = P.sbuf([128, 16, 1536], BF16, 'wq'); wqb = P.buf()
    ost = [P.sbuf([128, TH], F32, f'ost{i}') for i in range(4)]; ostb = P.bufs(4)
    NPS = 6
    ps = [P.psum([128, TH], F32, f'ps{i}') for i in range(NPS)]; psb = P.bufs(NPS)
    ctr = {'ps': 0, 'ost': 0}

    def nxt(k, n):
        v = ctr[k] % n
        ctr[k] += 1
        return v

    wqf = wq[:].rearrange("p c n -> p (c n)")
    for c0 in range(0, 16, 8):
        P.dma('gpsimd', wqf[:, c0 * 1536:(c0 + 8) * 1536], X['wqkv'][:, c0 * 1536:(c0 + 8) * 1536], writes=[wqb])
    def load_xn(tt):
        r, hf, xi = tt // 2, tt % 2, tt % 2
        for c in range(16):
            P.dma('sync', xn[xi][:, c, :], X['gout2'][c // 2][r * 256 + (c % 2) * 128:r * 256 + (c % 2 + 1) * 128, hf * TH:(hf + 1) * TH], writes=[xnb[xi][c]])
    load_xn(0)
    for tt in range(SQ // TH):
        t0 = tt * TH
        xi = tt % 2
        if tt + 1 < SQ // TH:
            load_xn(tt + 1)
        for fc in range(8):
            pi = nxt('ps', NPS)
            for c in range(16):
                P.op('tensor', lambda e, pi=pi, c=c, fc=fc, xi=xi: e.matmul(ps[pi][:], wq[:, c, fc * 128:(fc + 1) * 128], xn[xi][:, c, :],
                                                                             start=(c == 0), stop=(c == 15)),
                     reads=[wqb, xnb[xi][c]], writes=[psb[pi]])
            oi = nxt('ost', 4)
            P.op('scalar', lambda e, oi=oi, pi=pi: e.copy(ost[oi][:], ps[pi][:]), reads=[psb[pi]], writes=[ostb[oi]])
            dst = X['qT'] if fc < 4 else X['kT']
            P.dma('sync', dst[fc % 4, :, t0:t0 + TH], ost[oi][:], reads=[ostb[oi]])
        for ts in range(TH // 128):
            pi = nxt('ps', NPS)
            for c in range(16):
                P.op('tensor', lambda e, pi=pi, c=c, ts=ts, xi=xi: e.matmul(ps[pi][:], xn[xi][:, c, ts * 128:(ts + 1) * 128], wq[:, c, 1024:1536],
                                                                             start=(c == 0), stop=(c == 15)),
                     reads=[wqb, xnb[xi][c]], writes=[psb[pi]])
            oi = nxt('ost', 4)
            P.op('vector', lambda e, oi=oi, pi=pi: e.tensor_copy(ost[oi][:], ps[pi][:]), reads=[psb[pi]], writes=[ostb[oi]])
            tk = t0 + ts * 128
            for hh in range(4):
                P.dma('sync', X['v'][hh, tk:tk + 128, :], ost[oi][:, hh * 128:(hh + 1) * 128], reads=[ostb[oi]])
    P.finish()


S = 4096
HD = 128
NH = 4
NT = S // 128
NEG = -1e30
MBIG = -30000.0


def moba_consts(head0):
    ident = np.eye(128, dtype=np.float32)
    tri = (np.arange(128)[None, :] >= np.arange(128)[:, None]).astype(np.float32)
    maskadd = np.zeros((128, 16, 16), np.float32)
    for bq in range(16):
        maskadd[:, bq, bq:] = NEG
    eneg = np.zeros((16, 16, 128), np.float32)
    for n in range(16):
        eneg[n, n, :] = MBIG
    slopes = np.exp2(-8.0 * np.arange(1, 17, dtype=np.float32) / 16)
    bias = np.zeros((128, NH, NT), np.float32)
    for hh in range(NH):
        for dl in range(NT):
            bias[:, hh, dl] = slopes[head0 + hh] * (np.arange(128) - 64 - 128 * dl)
    return {"ident": ident, "tri": tri, "maskadd": maskadd.reshape(128, 256), "eneg": eneg.reshape(16, 2048),
            "abias": bias.reshape(128, NH * NT)}


def build_moba(nc=None, X=None):
    fused = nc is not None
    if not fused:
        nc = bass.Bass("TRN2", target_bir_lowering=False)
    P = Prog(nc)
    fm = X['gin3'] if fused else None

    def din(name, shape):
        if fused:
            return X[name]
        return nc.dram_tensor(name, list(shape), F32, kind="ExternalInput").ap()

    qT_d = din("qT", [NH, HD, S])
    kT_d = din("kT", [NH, HD, S])
    v_d = din("v", [NH, S, HD])
    ident_d = din("ident", [128, 128])
    tri_d = din("tri", [128, 128])
    maskadd_d = din("maskadd", [128, 256])
    eneg_d = din("eneg", [16, 2048])
    abias_d = din("abias", [128, NH * NT])
    if not fused:
        o_d = nc.dram_tensor("o", [NH, S, HD], F32, kind="ExternalOutput").ap()

    ident = P.sbuf([128, 128], F32, 'identsb'); identb = P.buf()
    tri = P.sbuf([128, 128], BF16, 'trisb'); trib = P.buf()
    maskadd = P.sbuf([128, 16, 16], F32, 'maskaddsb'); maskaddb = P.buf()
    eneg = P.sbuf([16, 16, 128], BF16, 'enegsb'); enegb = P.buf()
    abias = P.sbuf([128, NH, NT], F32, 'abiassb'); abiasb = P.buf()
    P.dma('sync', ident[:], ident_d, writes=[identb])
    P.dma('gpsimd', tri[:], tri_d, writes=[trib])
    P.dma('sync', maskadd[:].rearrange("p a b -> p (a b)"), maskadd_d, writes=[maskaddb])
    P.dma('gpsimd', eneg[:].rearrange("p a b -> p (a b)"), eneg_d, writes=[enegb])
    P.dma('sync', abias[:].rearrange("p a b -> p (a b)"), abias_d, writes=[abiasb])

    q32s = [P.sbuf([128, S], F32, f'q32_{i}') for i in range(2)]; q32bs = P.bufs(2)
    k32s = [P.sbuf([128, S], F32, f'k32_{i}') for i in range(2)]; k32bs = P.bufs(2)
    qbf = P.sbuf([128, S], BF16, 'qbf'); qbfb = P.buf()
    kbf = P.sbuf([128, S], BF16, 'kbf'); kbfb = P.buf()
    vaugs = [P.sbuf([128, NT, 132], BF16, f'vaug{i}') for i in range(2)]; vaugbs = P.bufs(2)
    km = P.sbuf([128, 16], F32, 'km'); kmb = P.buf()
    km2 = P.sbuf([128, 16], F32, 'km2'); km2b = P.buf()
    gm = [P.sbuf([128, 16], F32, f'gm{i}') for i in range(2)]; gmb = P.bufs(2)
    m8 = [P.sbuf([128, 8], F32, f'm8{i}') for i in range(2)]; m8b = P.bufs(2)
    thr = [P.sbuf([128, 1], F32, f'thr{i}') for i in range(2)]; thrb = P.bufs(2)
    nsel = [P.sbuf([128, 16], F32, f'nsel{i}') for i in range(2)]; nselb = P.bufs(2)
    nselT = P.sbuf([16, S], BF16, 'nselT'); nselTb = P.bufs(NT)
    NPT = 8
    pt = [P.sbuf([128, 128], BF16, f'pt{i}') for i in range(NPT)]; ptb = P.bufs(NPT)
    rc = [P.sbuf([128, 1], F32, f'rc{i}') for i in range(2)]; rcb = P.bufs(2)
    osb = [P.sbuf([128, 128], F32, f'osb{i}') for i in range(2)]; osbb = P.bufs(2)
    osT = [P.sbuf([128, 128], F32, f'osT{i}') for i in range(2)]; osTb = P.bufs(2)

    NSL = 4
    sps = [P.psum([128, 128], F32, f'sps{i}') for i in range(NSL)]; spsb = P.bufs(NSL)
    ops_ = [P.psum([128, 132], F32, f'ops{i}') for i in range(2)]; opsb = P.bufs(2)
    gtp = P.psum([128, 144], F32, 'gtp'); gpsb = P.buf(); tpob = gpsb
    gps = gtp[:, 128:144]; tpo = gtp[:, 0:128]
    tps = P.psum([16, 128], F32, 'tps'); tpsb = P.buf()

    scale = float(HD) ** -0.5
    ctr = {'s': 0, 'pt': 0, 'g': 0, 'o': 0}

    def nxt(k, n):
        v = ctr[k] % n
        ctr[k] += 1
        return v

    for i in range(2):
        P.op('vector', lambda e, i=i: e.memset(vaugs[i][:, :, 128:129], 1.0), writes=[vaugbs[i]])

    def load_head(hh):
        q32, q32b, k32, k32b, vaug, vaugb = q32s[hh % 2], q32bs[hh % 2], k32s[hh % 2], k32bs[hh % 2], vaugs[hh % 2], vaugbs[hh % 2]
        for c in range(8):
            P.dma('sync', q32[:, c * 512:(c + 1) * 512], qT_d[hh, :, c * 512:(c + 1) * 512], writes=[q32b])
            P.dma('sync', k32[:, c * 512:(c + 1) * 512], kT_d[hh, :, c * 512:(c + 1) * 512], writes=[k32b])
        vv = v_d[hh].rearrange("(t p) d -> p t d", p=128)
        for c in range(8):
            P.dma('gpsimd', vaug[:, c * 4:(c + 1) * 4, 0:128], vv[:, c * 4:(c + 1) * 4, :], writes=[vaugb])
    load_head(0)

    def head(hh):
        q32, q32b, k32, k32b, vaug, vaugb = q32s[hh % 2], q32bs[hh % 2], k32s[hh % 2], k32bs[hh % 2], vaugs[hh % 2], vaugbs[hh % 2]
        if hh + 1 < NH:
            load_head(hh + 1)
        head_body(hh, q32, q32b, k32, k32b, vaug, vaugb)

    def head_body(hh, q32, q32b, k32, k32b, vaug, vaugb):
        for c in range(4):
            P.op('scalar', lambda e, c=c: e.copy(qbf[:, c * 1024:(c + 1) * 1024], q32[:, c * 1024:(c + 1) * 1024]),
                 reads=[q32b], writes=[qbfb])
            P.op('vector', lambda e, c=c: e.tensor_copy(kbf[:, c * 1024:(c + 1) * 1024], k32[:, c * 1024:(c + 1) * 1024]),
                 reads=[k32b], writes=[kbfb])
        P.op('vector', lambda e: e.tensor_reduce(km[:], k32[:].rearrange("p (n s) -> p n s", s=256), axis=AX.X, op=ALU.add),
             reads=[k32b], writes=[kmb])
        P.op('vector', lambda e: e.tensor_scalar(km2[:], km[:], 1.0 / 256, None, op0=ALU.mult), reads=[kmb], writes=[km2b])
        for qi in range(2, NT):
            bq = qi // 2
            gi = nxt('g', 2)
            P.op('tensor', lambda e, qi=qi: e.matmul(gps, q32[:, qi * 128:(qi + 1) * 128], km2[:], start=True, stop=True),
                 reads=[q32b, km2b], writes=[gpsb])
            P.op('vector', lambda e, gi=gi, bq=bq: e.tensor_tensor(gm[gi][:], gps, maskadd[:, bq, :], op=ALU.add),
                 reads=[gpsb, maskaddb], writes=[gmb[gi]])
            P.op('vector', lambda e, gi=gi: e.max(m8[gi][:], gm[gi][:]), reads=[gmb[gi]], writes=[m8b[gi]])
            P.op('vector', lambda e, gi=gi: e.tensor_scalar(thr[gi][:], m8[gi][:, 2:3], -1e29, None, op0=ALU.max),
                 reads=[m8b[gi]], writes=[thrb[gi]])
            P.op('vector', lambda e, gi=gi: e.tensor_scalar(nsel[gi][:], gm[gi][:], thr[gi][:, 0:1], None, op0=ALU.is_lt),
                 reads=[gmb[gi], thrb[gi]], writes=[nselb[gi]])
            P.op('tensor', lambda e, gi=gi: e.transpose(tps[:], nsel[gi][:], ident[:]),
                 reads=[nselb[gi], identb], writes=[tpsb])
            P.op('scalar', lambda e, qi=qi: e.copy(nselT[:, qi * 128:(qi + 1) * 128], tps[:]),
                 reads=[tpsb], writes=[nselTb[qi]])
        SKEW = 3
        for qi in range(NT):
            bq = qi // 2
            oi = nxt('o', 2)
            pend = []

            def flush_one():
                pi_, kt_ = pend.pop(0)
                P.op('tensor', lambda e, pi_=pi_, oi=oi, kt_=kt_, qi=qi: e.matmul(
                    ops_[oi][:, 0:129], pt[pi_][:], vaug[:, kt_, 0:129], start=(kt_ == 0), stop=(kt_ == qi)),
                    reads=[ptb[pi_], vaugb], writes=[opsb[oi]])
            for kt in range(qi + 1):
                si = nxt('s', NSL)
                spt = sps[si][:]
                own = kt >= 2 * bq
                P.op('tensor', lambda e, spt=spt, kt=kt, qi=qi, own=own: e.matmul(
                    spt, kbf[:, kt * 128:(kt + 1) * 128], qbf[:, qi * 128:(qi + 1) * 128], start=True, stop=own),
                    reads=[kbfb, qbfb], writes=[spsb[si]])
                if not own:
                    P.op('tensor', lambda e, spt=spt, kt=kt, qi=qi: e.matmul(
                        spt, eneg[:, kt // 2, :], nselT[:, qi * 128:(qi + 1) * 128], start=False, stop=True),
                        reads=[enegb, nselTb[qi]], writes=[spsb[si]])
                if len(pend) >= SKEW:
                    flush_one()
                pi = nxt('pt', NPT)
                P.op('scalar', lambda e, pi=pi, spt=spt, hh=hh, dl=qi - kt: e.activation(
                    pt[pi][:], spt, AF.Exp, bias=abias[:, hh, dl:dl + 1], scale=scale),
                    reads=[spsb[si], abiasb], writes=[ptb[pi]])
                if kt == qi:
                    P.op('vector', lambda e, pi=pi: e.tensor_tensor(pt[pi][:], pt[pi][:], tri[:], op=ALU.mult),
                         reads=[ptb[pi], trib], writes=[ptb[pi]])
                pend.append((pi, kt))
            while pend:
                flush_one()
            P.op('vector', lambda e, oi=oi: e.reciprocal(rc[oi][:], ops_[oi][:, 128:129]), reads=[opsb[oi]], writes=[rcb[oi]])
            P.op('vector', lambda e, oi=oi: e.tensor_scalar(osb[oi][:], ops_[oi][:, 0:128], rc[oi][:, 0:1], None, op0=ALU.mult),
                 reads=[opsb[oi], rcb[oi]], writes=[osbb[oi]])
            if not fused:
                P.dma('sync', o_d[hh, qi * 128:(qi + 1) * 128, :], osb[oi][:], reads=[osbb[oi]])
            else:
                P.op('tensor', lambda e, oi=oi: e.transpose(tpo, osb[oi][:], ident[:]), reads=[osbb[oi], identb], writes=[tpob])
                P.op('scalar', lambda e, oi=oi: e.copy(osT[oi][:], tpo), reads=[tpob], writes=[osTb[oi]])
                P.dma('sync', fm[hh // 2][qi // 8][(hh % 2) * 128:(hh % 2 + 1) * 128, (qi % 8) * 128:(qi % 8 + 1) * 128], osT[oi][:], reads=[osTb[oi]])
        if fused and hh % 2 == 1:
            P.wait_all('gpsimd', osTb)
            for tq in range(4):
                P.collective("AllGather", X['gin3'][hh // 2][tq], X['gout3'][hh // 2][tq], [[0, 1, 2, 3], [4, 5, 6, 7]],
                             reads=[P.buf('gi')], writes=[P.buf('go')])
    for hh in range(NH):
        head(hh)
    if fused:
        P.finish()
        return nc
    P.wait_all('sync', osbb)
    P.emit()
    P.close()
    return nc


def moba_ref(q, k, v, head0):
    out = np.zeros_like(q)
    slopes = np.exp2(-8.0 * np.arange(1, 17, dtype=np.float64) / 16)
    t = np.arange(S)
    for hh in range(q.shape[0]):
        qq, kk, vv = q[hh].astype(np.float64), k[hh].astype(np.float64), v[hh].astype(np.float64)
        kmean = kk.reshape(16, 256, HD).mean(1)
        gate = qq @ kmean.T
        blk = t // 256
        gate = np.where(np.arange(16)[None, :] < blk[:, None], gate, -np.inf)
        order = np.argsort(-gate, axis=1)[:, :3]
        selm = np.zeros((S, 16), bool)
        for r in range(3):
            valid = r < blk
            selm[t[valid], order[valid, r]] = True
        selm[t, blk] = True
        for b0 in range(16):
            ts = slice(b0 * 256, (b0 + 1) * 256)
            lg = qq[ts] @ kk[: (b0 + 1) * 256].T * HD ** -0.5
            dist = t[ts][:, None] - t[None, : (b0 + 1) * 256]
            lg = lg - slopes[head0 + hh] * dist
            m = np.repeat(selm[ts, : b0 + 1], 256, axis=1) & (dist >= 0)
            lg = np.where(m, lg, -np.inf)
            p = np.exp(lg - lg.max(1, keepdims=True))
            p /= p.sum(1, keepdims=True)
            out[hh, ts] = p @ vv[: (b0 + 1) * 256]
    return out


SEG = 256
RC = 64
GC = 128
LN_EPS = 64 * 1e-5


def mix_consts():
    ident = np.eye(128, dtype=np.float32)
    s = np.arange(64)[:, None]; t = np.arange(64)[None, :]
    m2 = np.concatenate([(t > s), (t >= s)], axis=1).astype(np.float32)
    m0 = (t < s).astype(np.float32)
    s2 = np.arange(128)[:, None]; t2 = np.arange(128)[None, :]
    gm = (t2 >= s2).astype(np.float32)
    r64 = np.ones((128, SEG), np.float32); r64[:, ::RC] = 0.0
    r128 = np.ones((128, SEG), np.float32); r128[:, ::GC] = 0.0
    return {"ident": ident, "m2": np.tile(m2, (1, 4)), "m0": np.tile(m0, (1, 4)), "gmask": gm, "rst64": r64, "rst128": r128,
            "id4": np.tile(np.eye(64, dtype=np.float32), (1, 4))}


def build_mix(S, nc=None, X=None):
    fused = nc is not None
    if not fused:
        nc = bass.Bass("TRN2", target_bir_lowering=False)
    P = Prog(nc)
    NSEG = S // SEG
    fm = X['gin1'] if fused else None

    def din(name, shape):
        if fused:
            return X[name]
        return nc.dram_tensor(name, list(shape), F32, kind="ExternalInput").ap()

    ident_d = din("ident", [128, 128]); m2_d = din("m2", [64, 512]); m0_d = din("m0", [64, 256]); id4_d = din("id4", [64, 256])
    gmask_d = din("gmask", [128, 128]); rst64_d = din("rst64", [128, SEG]); rst128_d = din("rst128", [128, SEG])
    gq_d = din("gq", [128, S]); gk_d = din("gk", [128, S]); gv_d = din("gv", [S, 256]); gog_d = din("gog", [S, 256])
    glow_d = din("glow", [16, S]); gw2_d = din("gw2", [16, 128]); ggb_d = din("ggb", [128, 1]); gnorm_d = din("gnorm", [128, 256])
    rr_d = din("rr", [4, 64, S + 1]); rk_d = din("rk", [4, 64, S + 1]); rv_d = din("rv", [4, 64, S + 1])
    lw_d = din("lw", [64, S + 1]); la_d = din("la", [64, S + 1]); lg1_d = din("lg1", [128, S + 1]); lg2_d = din("lg2", [32, S + 1])
    hp_d = din("hp", [64, 4 * 10])
    mul_d = din("mul", [128, 4])
    w2_d = din("w2", [64, 256]); a2_d = din("a2", [64, 256]); g2a_d = din("g2a", [128, 256]); g2b_d = din("g2b", [32, 256])
    if not fused:
        og_d = nc.dram_tensor("o_gla", [S, 256], F32, kind="ExternalOutput").ap()
        or_d = nc.dram_tensor("o_rwkv", [4, 64, S], F32, kind="ExternalOutput").ap()

    cnt = [0]

    def T(shape, dt=F32):
        cnt[0] += 1
        return P.sbuf(shape, dt, f'sb{cnt[0]}'), P.buf(f'sb{cnt[0]}')

    def const(d_ap, shape):
        t, b = T(shape)
        P.dma('sync', t[:], d_ap, writes=[b])
        return t, b

    ident, identb = const(ident_d, [128, 128])
    m2, m2b = const(m2_d, [64, 512]); m0, m0b = const(m0_d, [64, 256]); id4, id4b = const(id4_d, [64, 256])
    gmask, gmaskb = const(gmask_d, [128, 128])
    rst64, rst64b = const(rst64_d, [128, SEG]); rst128, rst128b = const(rst128_d, [128, SEG])
    gw2, gw2b = const(gw2_d, [16, 128]); ggb, ggbb = const(ggb_d, [128, 1]); gnorm, gnormb = const(gnorm_d, [128, 256])
    hp, hpb = const(hp_d, [64, 40]); mul, mulb = const(mul_d, [128, 4])
    w2, w2b = const(w2_d, [64, 256]); a2, a2b = const(a2_d, [64, 256])
    g2a, g2ab = const(g2a_d, [128, 256]); g2b_, g2bb = const(g2b_d, [32, 256])
    ones64, ones64b = T([64, 64])
    P.op('vector', lambda e: e.memset(ones64[:], 1.0 / 64), writes=[ones64b])
    onesk, oneskb = T([64, 64])
    P.op('vector', lambda e: e.memset(onesk[:], 1.0), writes=[oneskb])
    omka, omkab = T([64, 4])
    for hh in range(4):
        P.op('vector', lambda e, hh=hh: e.tensor_scalar(omka[:, hh:hh + 1], hp[:, hh * 10 + 6:hh * 10 + 7], -1.0, 1.0,
                                                         op0=ALU.mult, op1=ALU.add), reads=[hpb], writes=[omkab])
    rkm = []
    for hh in range(4):
        t, b = T([64, 64])
        P.op('vector', lambda e, t=t, hh=hh: e.tensor_scalar(t[:], onesk[:], hp[:, hh * 10 + 7:hh * 10 + 8], None, op0=ALU.mult),
             reads=[oneskb, hpb], writes=[b])
        rkm.append((t, b))

    def HP(hh, i):
        return hp[:, hh * 10 + i:hh * 10 + i + 1]

    NPS = 8
    pss = [P.psum([128, 512], F32, f'ps{i}') for i in range(NPS)]; pssb = P.bufs(NPS, 'ps')
    pctr = [0]

    def PS():
        i = pctr[0] % NPS
        pctr[0] += 1
        return pss[i], pssb[i]

    engs = ['vector', 'gpsimd']
    ectr = [0]

    def EW():
        ectr[0] += 1
        return engs[ectr[0] % 2]

    def ew_tt(out, ob, a, ab, b, bb, op, eng=None):
        P.op(eng or 'vector', lambda e: e.tensor_tensor(out, a, b, op=op), reads=[ab, bb], writes=[ob])

    Sg = [T([128, 256]) for _ in range(2)]
    P.op('vector', lambda e: e.memset(Sg[0][0][:], 0.0), writes=[Sg[0][1]])
    Sr = [T([64, 256]) for _ in range(2)]
    Srb = [T([64, 256], BF16) for _ in range(2)]
    P.op('vector', lambda e: e.memset(Sr[0][0][:], 0.0), writes=[Sr[0][1]])
    P.op('vector', lambda e: e.memset(Srb[0][0][:], 0.0), writes=[Srb[0][1]])
    gpar = [0]
    rpar = [0]
    ABm4 = T([64, 512], BF16); AKm4 = T([64, 512], BF16)
    M4 = [T([64, 256], BF16) for _ in range(2)]; MT4 = [T([64, 256], BF16) for _ in range(2)]; TT4 = [T([64, 256], BF16) for _ in range(2)]
    tok4 = T([64, 768], BF16); AkV4 = T([64, 256]); X4 = T([64, 256], BF16); U4 = T([64, 256], BF16)
    y4 = T([64, 4, SEG])

    def TS(p=128, w=SEG):
        return T([p, w])

    g_q = T([128, SEG]); g_k = T([128, SEG]); g_low = T([16, SEG])
    g_sig = TS(); g_la = TS(); g_cb = TS(); g_e = TS(); g_en = TS(); g_qd = TS(); g_ki = TS(); g_dec = TS(); g_kd = TS()
    g_v = [T([128, 256]) for _ in range(2)]; g_og = [T([128, 256]) for _ in range(2)]
    g_att = [T([128, 128]) for _ in range(2)]; g_kdt = [T([128, 128]) for _ in range(2)]
    g_sq = T([128, 256]); g_ss = T([128, 1]); g_rs = T([128, 1]); g_rstd = T([128, 1]); g_on = T([128, 256]); g_so = T([128, 256])
    g_out = [T([128, 256]) for _ in range(2)]
    g_outT = [T([128, 256]) for _ in range(2)]

    l_w = T([64, SEG + 1]); l_a = T([64, SEG + 1]); l_g1 = T([128, SEG + 1]); l_g2 = T([32, SEG + 1])
    l_tmp = T([128, SEG]); l_wl = T([64, SEG]); l_al = T([64, SEG]); l_g1l = T([128, SEG]); l_g2l = T([32, SEG])
    l_th = T([64, SEG]); l_s1 = T([128, SEG]); l_s2 = T([32, SEG])

    class H:
        pass
    sc = H()
    sc.raw = [T([64, SEG + 1]) for _ in range(3)]
    sc.tmp = T([64, SEG]); sc.r = T([64, SEG]); sc.k = T([64, SEG])
    sc.lw = T([64, SEG]); sc.a = T([64, SEG])
    sc.kk2 = T([64, SEG]); sc.inv = T([64, SEG]); sc.kkn = T([64, SEG]); sc.t1 = T([64, SEG]); sc.kp = T([64, SEG]); sc.bb = T([64, SEG])
    sc.cb = T([64, SEG]); sc.cbx = T([64, SEG]); sc.en = T([64, SEG]); sc.ex = T([64, SEG]); sc.dec = T([64, SEG])
    pc = H()
    pc.yc = T([64, SEG]); pc.sq = T([64, SEG]); pc.rs = T([64, SEG]); pc.rstd = T([64, SEG]); pc.out = T([64, SEG])
    hs = []
    for hh in range(4):
        o = H()
        o.v = T([64, SEG]); o.g = T([64, SEG]); o.e = T([64, SEG]); o.rkr = T([64, SEG])
        o.AR = T([64, SEG // RC, 128], BF16)
        o.bt = T([64, SEG], BF16); o.kt = T([64, SEG], BF16); o.bd = T([64, SEG]); o.kd = T([64, SEG])
        hs.append(o)

    def act(out, ob, in_, ib, func, extra_reads=(), eng='scalar', **kw):
        P.op('scalar', lambda e: e.activation(out, in_, func, **kw), reads=[ib] + list(extra_reads), writes=[ob])

    for seg in range(NSEG):
        t0 = seg * SEG
        P.dma('sync', g_q[0][:], gq_d[:, t0:t0 + SEG], writes=[g_q[1]])
        P.dma('sync', g_k[0][:], gk_d[:, t0:t0 + SEG], writes=[g_k[1]])
        P.dma('sync', g_low[0][:], glow_d[:, t0:t0 + SEG], writes=[g_low[1]])
        pz, pzb = PS()
        P.op('tensor', lambda e, pz=pz: e.matmul(pz[:, 0:SEG], gw2[:], g_low[0][:], start=True, stop=True),
             reads=[gw2b, g_low[1]], writes=[pzb])
        act(g_sig[0][:], g_sig[1], pz[:, 0:SEG], pzb, AF.Sigmoid, extra_reads=[ggbb], bias=ggb[:, 0:1])
        act(g_la[0][:], g_la[1], g_sig[0][:], g_sig[1], AF.Ln)
        P.op('vector', lambda e: e.tensor_scalar(g_la[0][:], g_la[0][:], 1.0 / 16, None, op0=ALU.mult), reads=[g_la[1]], writes=[g_la[1]])
        P.op('vector', lambda e: e.tensor_tensor_scan(g_cb[0][:], rst128[:], g_la[0][:], 0.0, op0=ALU.mult, op1=ALU.add),
             reads=[rst128b, g_la[1]], writes=[g_cb[1]])
        act(g_e[0][:], g_e[1], g_cb[0][:], g_cb[1], AF.Exp)
        act(g_en[0][:], g_en[1], g_cb[0][:], g_cb[1], AF.Exp, scale=-1.0)
        P.op('vector', lambda e: e.scalar_tensor_tensor(g_qd[0][:], g_q[0][:], 128.0 ** -0.5, g_e[0][:], op0=ALU.mult, op1=ALU.mult),
             reads=[g_q[1], g_e[1]], writes=[g_qd[1]])
        ew_tt(g_ki[0][:], g_ki[1], g_k[0][:], g_k[1], g_en[0][:], g_en[1], ALU.mult, 'gpsimd')
        for n in range(SEG // GC):
            cs = slice(n * GC, (n + 1) * GC)
            act(g_dec[0][:, cs], g_dec[1], g_cb[0][:, cs], g_cb[1], AF.Exp, scale=-1.0, bias=g_cb[0][:, n * GC + GC - 1:n * GC + GC])
        ew_tt(g_kd[0][:], g_kd[1], g_k[0][:], g_k[1], g_dec[0][:], g_dec[1], ALU.mult, 'gpsimd')
        for n in range(SEG // GC):
            cs = slice(n * GC, (n + 1) * GC)
            tok0 = t0 + n * GC
            vi = n % 2
            gv, gvb = g_v[vi]; gog, gogb = g_og[vi]
            P.dma('sync', gv[:], gv_d[tok0:tok0 + GC, :], writes=[gvb])
            P.dma('sync', gog[:], gog_d[tok0:tok0 + GC, :], writes=[gogb])
            pa, pab = PS()
            P.op('tensor', lambda e, pa=pa, cs=cs: e.matmul(pa[:, 0:128], g_ki[0][:, cs], g_qd[0][:, cs], start=True, stop=True),
                 reads=[g_ki[1], g_qd[1]], writes=[pab])
            att, attb = g_att[vi]
            P.op('vector', lambda e, pa=pa, att=att: e.tensor_tensor(att[:], pa[:, 0:128], gmask[:], op=ALU.mult),
                 reads=[pab, gmaskb], writes=[attb])
            pt_, ptb_ = PS()
            P.op('tensor', lambda e, pt_=pt_, cs=cs: e.transpose(pt_[:, 0:128], g_kd[0][:, cs], ident[:]),
                 reads=[g_kd[1], identb], writes=[ptb_])
            kdt, kdtb = g_kdt[vi]
            P.op('scalar', lambda e, pt_=pt_, kdt=kdt: e.copy(kdt[:], pt_[:, 0:128]), reads=[ptb_], writes=[kdtb])
            So, Sob = Sg[gpar[0]]; Sn, Snb = Sg[1 - gpar[0]]
            po, pob = PS()
            P.op('tensor', lambda e, po=po, att=att, gv=gv: e.matmul(po[:, 0:256], att[:], gv[:], start=True, stop=False),
                 reads=[attb, gvb], writes=[pob])
            P.op('tensor', lambda e, po=po, cs=cs, So=So: e.matmul(po[:, 0:256], g_qd[0][:, cs], So[:], start=False, stop=True),
                 reads=[g_qd[1], Sob], writes=[pob])
            pst, pstb = PS()
            P.op('tensor', lambda e, pst=pst, kdt=kdt, gv=gv: e.matmul(pst[:, 0:256], kdt[:], gv[:], start=True, stop=True),
                 reads=[kdtb, gvb], writes=[pstb])
            cl = n * GC + GC - 1
            P.op('vector', lambda e, pst=pst, So=So, Sn=Sn, cl=cl: e.scalar_tensor_tensor(
                Sn[:], So[:], g_e[0][:, cl:cl + 1], pst[:, 0:256], op0=ALU.mult, op1=ALU.add),
                reads=[Sob, g_e[1], pstb], writes=[Snb])
            gpar[0] = 1 - gpar[0]
            act(g_sq[0][:], g_sq[1], po[:, 0:256], pob, AF.Square)
            P.op('vector', lambda e: e.tensor_reduce(g_ss[0][:], g_sq[0][:], axis=AX.X, op=ALU.add), reads=[g_sq[1]], writes=[g_ss[1]])
            act(g_rs[0][:], g_rs[1], g_ss[0][:], g_ss[1], AF.Sqrt, scale=1.0 / 256, bias=1e-6)
            P.op('vector', lambda e: e.reciprocal(g_rstd[0][:], g_rs[0][:]), reads=[g_rs[1]], writes=[g_rstd[1]])
            P.op('vector', lambda e, po=po: e.scalar_tensor_tensor(g_on[0][:], po[:, 0:256], g_rstd[0][:, 0:1], gnorm[:], op0=ALU.mult, op1=ALU.mult),
                 reads=[pob, g_rstd[1], gnormb], writes=[g_on[1]])
            act(g_so[0][:], g_so[1], gog[:], gogb, AF.Silu)
            go, gob = g_out[vi]
            ew_tt(go[:], gob, g_on[0][:], g_on[1], g_so[0][:], g_so[1], ALU.mult, 'gpsimd')
            if not fused:
                P.dma('sync', og_d[tok0:tok0 + GC, :], go[:], reads=[gob])
            else:
                ptT, ptTb = PS()
                for i in range(2):
                    P.op('tensor', lambda e, ptT=ptT, go=go, i=i: e.transpose(ptT[:, i * 128:(i + 1) * 128], go[:, i * 128:(i + 1) * 128], ident[:]),
                         reads=[gob, identb], writes=[ptTb])
                gT, gTb = g_outT[vi]
                P.op('scalar', lambda e, ptT=ptT, gT=gT: e.copy(gT[:], ptT[:, 0:256]), reads=[ptTb], writes=[gTb])
                for i in range(2):
                    P.dma('sync', fm[0][tok0 // 1024][i * 128:(i + 1) * 128, tok0 % 1024:tok0 % 1024 + GC], gT[:, i * 128:(i + 1) * 128], reads=[gTb])

        for (lt, ld, rows) in ((l_w, lw_d, 64), (l_a, la_d, 64), (l_g1, lg1_d, 128), (l_g2, lg2_d, 32)):
            P.dma('sync', lt[0][:], ld[:, t0:t0 + SEG + 1], writes=[lt[1]])

        def lerp(raw, rawb, out, outb, mu_ap, mub, tmp, tmpb, rows, eng):
            P.op(eng, lambda e: e.tensor_tensor(tmp, raw[:, 0:SEG], raw[:, 1:SEG + 1], op=ALU.subtract), reads=[rawb], writes=[tmpb])
            P.op('vector', lambda e: e.scalar_tensor_tensor(out, tmp, mu_ap, raw[:, 1:SEG + 1], op0=ALU.mult, op1=ALU.add),
                 reads=[tmpb, mub, rawb], writes=[outb])
        lerp(l_w[0][:], l_w[1], l_wl[0][:], l_wl[1], mul[0:64, 0:1], mulb, l_tmp[0][0:64, :], l_tmp[1], 64, 'gpsimd')
        lerp(l_a[0][:], l_a[1], l_al[0][:], l_al[1], mul[0:64, 1:2], mulb, l_tmp[0][0:64, :], l_tmp[1], 64, 'gpsimd')
        lerp(l_g1[0][:], l_g1[1], l_g1l[0][:], l_g1l[1], mul[:, 2:3], mulb, l_tmp[0][:, :], l_tmp[1], 128, 'gpsimd')
        lerp(l_g2[0][:], l_g2[1], l_g2l[0][:], l_g2l[1], mul[0:32, 3:4], mulb, l_tmp[0][0:32, :], l_tmp[1], 32, 'gpsimd')
        act(l_th[0][:], l_th[1], l_wl[0][:], l_wl[1], AF.Tanh)
        act(l_s1[0][:], l_s1[1], l_g1l[0][:], l_g1l[1], AF.Sigmoid)
        act(l_s2[0][:], l_s2[1], l_g2l[0][:], l_g2l[1], AF.Sigmoid)

        for hh in range(4):
            o = hs[hh]
            hc = slice(hh * 64, (hh + 1) * 64)
            for i, dd in enumerate((rr_d, rk_d, rv_d)):
                P.dma('sync', sc.raw[i][0][:], dd[hh, :, t0:t0 + SEG + 1], writes=[sc.raw[i][1]])
            for i, dst in enumerate((sc.r, sc.k, o.v)):
                lerp(sc.raw[i][0][:], sc.raw[i][1], dst[0][:], dst[1], HP(hh, i), hpb, sc.tmp[0][:], sc.tmp[1], 64, 'gpsimd')
            pw, pwb = PS()
            P.op('tensor', lambda e, pw=pw, hc=hc: e.matmul(pw[0:64, 0:SEG], w2[:, hc], l_th[0][:], start=True, stop=True),
                 reads=[w2b, l_th[1]], writes=[pwb])
            act(sc.lw[0][:], sc.lw[1], pw[0:64, 0:SEG], pwb, AF.Sigmoid, extra_reads=[hpb], bias=HP(hh, 3))
            P.op('vector', lambda e, o=o: e.tensor_scalar(sc.lw[0][:], sc.lw[0][:], -float(np.exp(-0.5)), None, op0=ALU.mult),
                 reads=[sc.lw[1]], writes=[sc.lw[1]])
            pa_, pab_ = PS()
            P.op('tensor', lambda e, pa_=pa_, hc=hc: e.matmul(pa_[0:64, 0:SEG], a2[:, hc], l_al[0][:], start=True, stop=True),
                 reads=[a2b, l_al[1]], writes=[pab_])
            act(sc.a[0][:], sc.a[1], pa_[0:64, 0:SEG], pab_, AF.Sigmoid, extra_reads=[hpb], bias=HP(hh, 4))
            pg, pgb = PS()
            P.op('tensor', lambda e, pg=pg, hc=hc: e.matmul(pg[0:64, 0:SEG], g2a[:, hc], l_s1[0][:], start=True, stop=False),
                 reads=[g2ab, l_s1[1]], writes=[pgb])
            P.op('tensor', lambda e, pg=pg, hc=hc: e.matmul(pg[0:64, 0:SEG], g2b_[:, hc], l_s2[0][:], start=False, stop=True),
                 reads=[g2bb, l_s2[1]], writes=[pgb])
            P.op('scalar', lambda e, pg=pg, o=o: e.copy(o.g[0][:], pg[0:64, 0:SEG]), reads=[pgb], writes=[o.g[1]])
            act(sc.kk2[0][:], sc.kk2[1], sc.k[0][:], sc.k[1], AF.Square, extra_reads=[hpb], scale=HP(hh, 5))
            pn, pnb = PS()
            P.op('tensor', lambda e, pn=pn, o=o: e.matmul(pn[0:64, 0:SEG], onesk[:], sc.kk2[0][:], start=True, stop=True),
                 reads=[oneskb, sc.kk2[1]], writes=[pnb])
            act(sc.inv[0][:], sc.inv[1], pn[0:64, 0:SEG], pnb, AF.Sqrt)
            P.op('vector', lambda e, o=o: e.tensor_scalar(sc.inv[0][:], sc.inv[0][:], 1e-12, None, op0=ALU.max), reads=[sc.inv[1]], writes=[sc.inv[1]])
            P.op('vector', lambda e, o=o: e.reciprocal(sc.inv[0][:], sc.inv[0][:]), reads=[sc.inv[1]], writes=[sc.inv[1]])
            P.op('vector', lambda e, o=o, hh=hh: e.scalar_tensor_tensor(sc.kkn[0][:], sc.k[0][:], HP(hh, 5), sc.inv[0][:], op0=ALU.mult, op1=ALU.mult),
                 reads=[sc.k[1], hpb, sc.inv[1]], writes=[sc.kkn[1]])
            P.op('vector', lambda e, o=o, hh=hh: e.tensor_scalar(sc.t1[0][:], sc.a[0][:], HP(hh, 6), omka[:, hh:hh + 1], op0=ALU.mult, op1=ALU.add),
                 reads=[sc.a[1], hpb, omkab], writes=[sc.t1[1]])
            ew_tt(sc.kp[0][:], sc.kp[1], sc.k[0][:], sc.k[1], sc.t1[0][:], sc.t1[1], ALU.mult, 'gpsimd')
            ew_tt(sc.bb[0][:], sc.bb[1], sc.kkn[0][:], sc.kkn[1], sc.a[0][:], sc.a[1], ALU.mult, 'gpsimd')
            P.op('vector', lambda e, o=o: e.tensor_tensor_scan(sc.cb[0][:], rst64[0:64, :], sc.lw[0][:], 0.0, op0=ALU.mult, op1=ALU.add),
                 reads=[rst64b, sc.lw[1]], writes=[sc.cb[1]])
            ew_tt(sc.cbx[0][:], sc.cbx[1], sc.cb[0][:], sc.cb[1], sc.lw[0][:], sc.lw[1], ALU.subtract, 'gpsimd')
            act(o.e[0][:], o.e[1], sc.cb[0][:], sc.cb[1], AF.Exp)
            act(sc.en[0][:], sc.en[1], sc.cb[0][:], sc.cb[1], AF.Exp, scale=-1.0)
            act(sc.ex[0][:], sc.ex[1], sc.cbx[0][:], sc.cbx[1], AF.Exp)
            ARv = o.AR[0][:]
            P.op('vector', lambda e, o=o, ARv=ARv: e.scalar_tensor_tensor(
                ARv[:, :, 0:64], sc.kkn[0][:].rearrange("p (n c) -> p n c", c=RC), -1.0, sc.ex[0][:].rearrange("p (n c) -> p n c", c=RC),
                op0=ALU.mult, op1=ALU.mult), reads=[sc.kkn[1], sc.ex[1]], writes=[o.AR[1]])
            P.op('vector', lambda e, o=o, ARv=ARv: e.tensor_tensor(
                ARv[:, :, 64:128], sc.r[0][:].rearrange("p (n c) -> p n c", c=RC), o.e[0][:].rearrange("p (n c) -> p n c", c=RC),
                op=ALU.mult), reads=[sc.r[1], o.e[1]], writes=[o.AR[1]])
            ew_tt(o.bt[0][:], o.bt[1], sc.bb[0][:], sc.bb[1], sc.en[0][:], sc.en[1], ALU.mult, 'gpsimd')
            ew_tt(o.kt[0][:], o.kt[1], sc.kp[0][:], sc.kp[1], sc.en[0][:], sc.en[1], ALU.mult, 'gpsimd')
            for n in range(SEG // RC):
                cs = slice(n * RC, (n + 1) * RC)
                act(sc.dec[0][:, cs], sc.dec[1], sc.cb[0][:, cs], sc.cb[1], AF.Exp, scale=-1.0, bias=sc.cb[0][:, n * RC + RC - 1:n * RC + RC])
            ew_tt(o.bd[0][:], o.bd[1], sc.bb[0][:], sc.bb[1], sc.dec[0][:], sc.dec[1], ALU.mult, 'gpsimd')
            ew_tt(o.kd[0][:], o.kd[1], sc.kp[0][:], sc.kp[1], sc.dec[0][:], sc.dec[1], ALU.mult, 'gpsimd')
            ew_tt(o.rkr[0][:], o.rkr[1], sc.r[0][:], sc.r[1], sc.kp[0][:], sc.kp[1], ALU.mult, 'gpsimd')

        def H4(t, w):
            return [t[:, hh * w:(hh + 1) * w] for hh in range(4)]

        def chunk_iter(n):
            cs = slice(n * RC, (n + 1) * RC)
            ARs = [hs[hh].AR[0][:, n, :] for hh in range(4)]
            ARb = [hs[hh].AR[1] for hh in range(4)]
            btb = [hs[hh].bt[1] for hh in range(4)]; ktb = [hs[hh].kt[1] for hh in range(4)]
            p1, p1b = PS()
            for hh in range(4):
                P.op('tensor', lambda e, hh=hh, p1=p1: e.matmul(p1[0:64, hh * 128:(hh + 1) * 128], hs[hh].bt[0][:, cs], ARs[hh], start=True, stop=True),
                     reads=[btb[hh], ARb[hh]], writes=[p1b])
            P.op('vector', lambda e, p1=p1: e.tensor_tensor(ABm4[0][:], p1[0:64, 0:512], m2[:], op=ALU.mult), reads=[p1b, m2b], writes=[ABm4[1]])
            p2, p2b = PS()
            for hh in range(4):
                P.op('tensor', lambda e, hh=hh, p2=p2: e.matmul(p2[0:64, hh * 128:(hh + 1) * 128], hs[hh].kt[0][:, cs], ARs[hh], start=True, stop=True),
                     reads=[ktb[hh], ARb[hh]], writes=[p2b])
            P.op('vector', lambda e, p2=p2: e.tensor_tensor(AKm4[0][:], p2[0:64, 0:512], m2[:], op=ALU.mult), reads=[p2b, m2b], writes=[AKm4[1]])
            p3, p3b = PS()
            for hh in range(4):
                P.op('tensor', lambda e, hh=hh, p3=p3: e.matmul(p3[0:64, hh * 64:(hh + 1) * 64], ARs[hh][:, 0:64], hs[hh].bt[0][:, cs], start=True, stop=True),
                     reads=[btb[hh], ARb[hh]], writes=[p3b])
            P.op('vector', lambda e, p3=p3: e.tensor_tensor(M4[0][0][:], p3[0:64, 0:256], m0[:], op=ALU.mult), reads=[p3b, m0b], writes=[M4[0][1]])
            ABv = ABm4[0][:].rearrange("p (h c) -> p h c", c=128)
            AKv = AKm4[0][:].rearrange("p (h c) -> p h c", c=128)
            P.op('gpsimd', lambda e, ABv=ABv: e.tensor_copy(MT4[0][0][:].rearrange("p (h c) -> p h c", c=64), ABv[:, :, 0:64]),
                 reads=[ABm4[1]], writes=[MT4[0][1]])
            P.op('gpsimd', lambda e, ABv=ABv: e.tensor_tensor(TT4[0][0][:].rearrange("p (h c) -> p h c", c=64), ABv[:, :, 0:64],
                                                              id4[:].rearrange("p (h c) -> p h c", c=64), op=ALU.add),
                 reads=[ABm4[1], id4b], writes=[TT4[0][1]])
            for half in range(2):
                p4, p4b = PS()
                for h2 in range(2):
                    hh = half * 2 + h2
                    for i, src in enumerate((hs[hh].v, hs[hh].bd, hs[hh].kd)):
                        P.op('tensor', lambda e, p4=p4, h2=h2, i=i, src=src: e.transpose(p4[0:64, h2 * 192 + i * 64:h2 * 192 + (i + 1) * 64], src[0][:, cs], ident[0:64, 0:64]),
                             reads=[src[1], identb], writes=[p4b])
                P.op('scalar', lambda e, p4=p4, half=half: e.copy(tok4[0][:, half * 384:(half + 1) * 384], p4[0:64, 0:384]), reads=[p4b], writes=[tok4[1]])
            tk = H4(tok4[0], 192)
            ABh = H4(ABm4[0], 128); AKh = H4(AKm4[0], 128)
            pa, pab = PS()
            for hh in range(4):
                P.op('tensor', lambda e, hh=hh, pa=pa: e.matmul(pa[0:64, hh * 64:(hh + 1) * 64], AKh[hh][:, 0:64], tk[hh][:, 0:64], start=True, stop=True),
                     reads=[AKm4[1], tok4[1]], writes=[pab])
            P.op('scalar', lambda e, pa=pa: e.copy(AkV4[0][:], pa[0:64, 0:256]), reads=[pab], writes=[AkV4[1]])
            for i in range(5):
                a_, b_ = i % 2, (i + 1) % 2
                Mh = H4(M4[a_][0], 64); MTh = H4(MT4[a_][0], 64); Mnh = H4(M4[b_][0], 64); TTh = H4(TT4[a_][0], 64)
                pm, pmb = PS()
                for hh in range(4):
                    P.op('tensor', lambda e, hh=hh, pm=pm, MTh=MTh, Mh=Mh: e.matmul(pm[0:64, hh * 64:(hh + 1) * 64], MTh[hh], Mh[hh], start=True, stop=True),
                         reads=[MT4[a_][1], M4[a_][1]], writes=[pmb])
                P.op('vector', lambda e, pm=pm, b_=b_: e.tensor_copy(M4[b_][0][:], pm[0:64, 0:256]), reads=[pmb], writes=[M4[b_][1]])
                if i < 4:
                    pq, pqb = PS()
                    for hh in range(4):
                        P.op('tensor', lambda e, hh=hh, pq=pq, MTh=MTh, Mh=Mh: e.matmul(pq[0:64, hh * 64:(hh + 1) * 64], Mh[hh], MTh[hh], start=True, stop=True),
                             reads=[MT4[a_][1], M4[a_][1]], writes=[pqb])
                    P.op('scalar', lambda e, pq=pq, b_=b_: e.copy(MT4[b_][0][:], pq[0:64, 0:256]), reads=[pqb], writes=[MT4[b_][1]])
                pu, pub = PS()
                for hh in range(4):
                    P.op('tensor', lambda e, hh=hh, pu=pu, Mnh=Mnh, TTh=TTh: e.matmul(pu[0:64, hh * 64:(hh + 1) * 64], Mnh[hh], TTh[hh], start=True, stop=True),
                         reads=[M4[b_][1], TT4[a_][1]], writes=[pub])
                P.op('vector', lambda e, pu=pu, a_=a_, b_=b_: e.tensor_tensor(TT4[b_][0][:], pu[0:64, 0:256], TT4[a_][0][:], op=ALU.add),
                     reads=[pub, TT4[a_][1]], writes=[TT4[b_][1]])
            TTf = H4(TT4[1][0], 64)
            So, Sob = Sr[rpar[0]]; Sn, Snb = Sr[1 - rpar[0]]
            So16, So16b = Srb[rpar[0]]; Sn16, Sn16b = Srb[1 - rpar[0]]
            Soh = H4(So16, 64)
            px, pxb = PS()
            for hh in range(4):
                P.op('tensor', lambda e, hh=hh, px=px, Soh=Soh: e.matmul(px[0:64, hh * 64:(hh + 1) * 64], ARs[hh][:, 0:64], Soh[hh], start=True, stop=True),
                     reads=[ARb[hh], So16b], writes=[pxb])
            P.op('vector', lambda e, px=px: e.tensor_tensor(X4[0][:], px[0:64, 0:256], AkV4[0][:], op=ALU.add), reads=[pxb, AkV4[1]], writes=[X4[1]])
            Xh = H4(X4[0], 64); Uh = H4(U4[0], 64)
            pU, pUb = PS()
            for hh in range(4):
                P.op('tensor', lambda e, hh=hh, pU=pU, Xh=Xh: e.matmul(pU[0:64, hh * 64:(hh + 1) * 64], TTf[hh], Xh[hh], start=True, stop=True),
                     reads=[TT4[1][1], X4[1]], writes=[pUb])
            P.op('scalar', lambda e, pU=pU: e.copy(U4[0][:], pU[0:64, 0:256]), reads=[pUb], writes=[U4[1]])
            pS, pSb = PS()
            for hh in range(4):
                P.op('tensor', lambda e, hh=hh, pS=pS, Uh=Uh: e.matmul(pS[0:64, hh * 64:(hh + 1) * 64], tk[hh][:, 64:128], Uh[hh], start=True, stop=False),
                     reads=[tok4[1], U4[1]], writes=[pSb])
                P.op('tensor', lambda e, hh=hh, pS=pS: e.matmul(pS[0:64, hh * 64:(hh + 1) * 64], tk[hh][:, 128:192], tk[hh][:, 0:64], start=False, stop=True),
                     reads=[tok4[1]], writes=[pSb])
            cl = n * RC + RC - 1
            for hh in range(4):
                P.op('vector', lambda e, hh=hh, pS=pS, So=So, Sn=Sn, cl=cl: e.scalar_tensor_tensor(
                    Sn[:, hh * 64:(hh + 1) * 64], So[:, hh * 64:(hh + 1) * 64], hs[hh].e[0][:, cl:cl + 1], pS[0:64, hh * 64:(hh + 1) * 64],
                    op0=ALU.mult, op1=ALU.add), reads=[Sob, hs[hh].e[1], pSb], writes=[Snb])
            P.op('gpsimd', lambda e, Sn=Sn, Sn16=Sn16: e.tensor_copy(Sn16[:], Sn[:]), reads=[Snb], writes=[Sn16b])
            pY, pYb = PS()
            for hh in range(4):
                P.op('tensor', lambda e, hh=hh, pY=pY, Soh=Soh: e.matmul(pY[0:64, hh * 64:(hh + 1) * 64], Soh[hh], ARs[hh][:, 64:128], start=True, stop=False),
                     reads=[So16b, ARb[hh]], writes=[pYb])
                P.op('tensor', lambda e, hh=hh, pY=pY, Uh=Uh: e.matmul(pY[0:64, hh * 64:(hh + 1) * 64], Uh[hh], ABh[hh][:, 64:128], start=False, stop=False),
                     reads=[U4[1], ABm4[1]], writes=[pYb])
                P.op('tensor', lambda e, hh=hh, pY=pY: e.matmul(pY[0:64, hh * 64:(hh + 1) * 64], tk[hh][:, 0:64], AKh[hh][:, 64:128], start=False, stop=True),
                     reads=[tok4[1], AKm4[1]], writes=[pYb])
            P.op('scalar', lambda e, pY=pY, cs=cs: e.copy(y4[0][:, :, cs], pY[0:64, 0:256].rearrange("p (h c) -> p h c", c=64)), reads=[pYb], writes=[y4[1]])
            rpar[0] = 1 - rpar[0]

        for n in range(SEG // RC):
            chunk_iter(n)

        for hh in range(4):
            o = hs[hh]
            pm, pmb = PS()
            P.op('tensor', lambda e, o=o, pm=pm, hh=hh: e.matmul(pm[0:64, 0:SEG], ones64[:], y4[0][:, hh, :], start=True, stop=True), reads=[ones64b, y4[1]], writes=[pmb])
            P.op('vector', lambda e, o=o, pm=pm, hh=hh: e.tensor_tensor(pc.yc[0][:], y4[0][:, hh, :], pm[0:64, 0:SEG], op=ALU.subtract), reads=[y4[1], pmb], writes=[pc.yc[1]])
            act(pc.sq[0][:], pc.sq[1], pc.yc[0][:], pc.yc[1], AF.Square)
            pv, pvb = PS()
            P.op('tensor', lambda e, o=o, pv=pv: e.matmul(pv[0:64, 0:SEG], ones64[:], pc.sq[0][:], start=True, stop=True), reads=[ones64b, pc.sq[1]], writes=[pvb])
            act(pc.rs[0][:], pc.rs[1], pv[0:64, 0:SEG], pvb, AF.Sqrt, bias=LN_EPS)
            P.op('vector', lambda e, o=o: e.reciprocal(pc.rstd[0][:], pc.rs[0][:]), reads=[pc.rs[1]], writes=[pc.rstd[1]])
            ew_tt(pc.yc[0][:], pc.yc[1], pc.yc[0][:], pc.yc[1], pc.rstd[0][:], pc.rstd[1], ALU.mult, 'gpsimd')
            P.op('vector', lambda e, o=o, hh=hh: e.tensor_scalar(pc.yc[0][:], pc.yc[0][:], HP(hh, 8), HP(hh, 9), op0=ALU.mult, op1=ALU.add),
                 reads=[pc.yc[1], hpb], writes=[pc.yc[1]])
            pb_, pbb = PS()
            P.op('tensor', lambda e, o=o, pb_=pb_, hh=hh: e.matmul(pb_[0:64, 0:SEG], rkm[hh][0][:], o.rkr[0][:], start=True, stop=True),
                 reads=[rkm[hh][1], o.rkr[1]], writes=[pbb])
            P.op('vector', lambda e, o=o, pb_=pb_: e.tensor_tensor(pc.out[0][:], pb_[0:64, 0:SEG], o.v[0][:], op=ALU.mult), reads=[pbb, o.v[1]], writes=[pc.out[1]])
            ew_tt(pc.out[0][:], pc.out[1], pc.out[0][:], pc.out[1], pc.yc[0][:], pc.yc[1], ALU.add, 'gpsimd')
            ew_tt(pc.out[0][:], pc.out[1], pc.out[0][:], pc.out[1], o.g[0][:], o.g[1], ALU.mult, 'gpsimd')
            if not fused:
                P.dma('sync', or_d[hh, :, t0:t0 + SEG], pc.out[0][:], reads=[pc.out[1]])
            else:
                P.dma('sync', fm[1][t0 // 1024][hh * 64:(hh + 1) * 64, t0 % 1024:t0 % 1024 + SEG], pc.out[0][:], reads=[pc.out[1]])

        if fused and (t0 + SEG) % 1024 == 0:
            P.wait_all('gpsimd', [g_outT[0][1], g_outT[1][1], pc.out[1]])
            for rh in range(2):
                P.collective("AllGather", X['gin1'][rh][t0 // 1024], X['gout1'][rh][t0 // 1024], [[0, 1, 2, 3], [4, 5, 6, 7]],
                             reads=[P.buf('gi')], writes=[P.buf('go')])

    if fused:
        P.finish()
        return nc
    P.wait_all('sync', [g_out[0][1], g_out[1][1]] + [pc.out[1]])
    P.emit()
    P.close()
    return nc

GQ, GK, GV, GOG, RR, RK, RV, GLOW, WLOW, ALOW, GLR, NINP = 0, 512, 1024, 2048, 3072, 4096, 5120, 6144, 6160, 6224, 6288, 6528
PERM = np.concatenate([np.arange(0, 3072), np.arange(3088, 6160), np.arange(3072, 3088), np.arange(6160, 6448)])


def pad_win(w_in):
    w = np.zeros((w_in.shape[0], NINP), np.float32)
    w[:, :6448] = w_in[:, PERM]
    return w


def padz(a):
    return np.ascontiguousarray(np.concatenate([np.zeros(a.shape[:-1] + (1,), np.float32), a], axis=-1))


def mix_inputs(pT, prm, j):
    S = pT.shape[1]
    c = np.ascontiguousarray
    d = {}
    d["gq"] = c(pT[GQ + 128 * j:GQ + 128 * j + 128]); d["gk"] = c(pT[GK + 128 * j:GK + 128 * j + 128])
    d["gv"] = c(pT[GV + 256 * j:GV + 256 * j + 256].T); d["gog"] = c(pT[GOG + 256 * j:GOG + 256 * j + 256].T)
    d["glow"] = c(pT[GLOW:GLOW + 16])
    r0 = 256 * j
    d["rr"] = padz(pT[RR + r0:RR + r0 + 256].reshape(4, 64, S)); d["rk"] = padz(pT[RK + r0:RK + r0 + 256].reshape(4, 64, S))
    d["rv"] = padz(pT[RV + r0:RV + r0 + 256].reshape(4, 64, S))
    d["lw"] = padz(pT[WLOW:WLOW + 64]); d["la"] = padz(pT[ALOW:ALOW + 64]); d["lg1"] = padz(pT[GLR:GLR + 128]); d["lg2"] = padz(pT[GLR + 128:GLR + 160])
    d.update(mix_params(prm, j))
    return d


def mix_params(prm, j):
    c = np.ascontiguousarray
    d = {}
    r0 = 256 * j
    d["gw2"] = c(prm['gla_gate_w2'][:, 128 * j:128 * j + 128]); d["ggb"] = c(prm['gla_gate_b'][128 * j:128 * j + 128][:, None])
    d["gnorm"] = c(np.tile(prm['gla_norm'][None, :], (128, 1)))
    mu = prm['rwkv_mu']
    hp = np.zeros((64, 40), np.float32)
    for hh in range(4):
        cols = r0 + 64 * hh + np.arange(64)
        vals = [mu[cols], mu[1024 + cols], mu[2048 + cols], prm['rwkv_w0'][cols], prm['rwkv_a0'][cols], prm['rwkv_k_k'][cols],
                prm['rwkv_k_a'][cols], prm['rwkv_r_k'].reshape(-1)[cols], prm['rwkv_ln_w'][cols], prm['rwkv_ln_b'][cols]]
        for i, v in enumerate(vals):
            hp[:, hh * 10 + i] = v
    d["hp"] = hp
    mul = np.zeros((128, 4), np.float32)
    mul[0:64, 0] = mu[3072:3136]; mul[0:64, 1] = mu[3136:3200]; mul[:, 2] = mu[3200:3328]; mul[0:32, 3] = mu[3328:3360]
    d["mul"] = mul
    d["w2"] = c(prm['rwkv_w2'][:, r0:r0 + 256]); d["a2"] = c(prm['rwkv_a2'][:, r0:r0 + 256])
    d["g2a"] = c(prm['rwkv_g2'][0:128, r0:r0 + 256]); d["g2b"] = c(prm['rwkv_g2'][128:160, r0:r0 + 256])
    return d


SQL = 4096


def build_fused():
    nc = bass.Bass("TRN2", target_bir_lowering=False)
    S = SQL

    def din(name, shape, dt=F32):
        return nc.dram_tensor(name, list(shape), dt, kind="ExternalInput").ap()

    def scr(name, shape, dt=F32):
        return nc.dram_tensor(name, list(shape), dt).ap()

    X = {}
    X['xfull'] = din('xfull', [2048, S]); X['xs'] = din('xs', [2048, 1024]); X['g1'] = din('g1', [128, 16])
    X['w1f'] = din('w1f', [128, 16 * NF1]); X['w1t'] = din('w1t', [128, 16 * NT1])
    for nm, shp in (('ident', [128, 128]), ('m2', [64, 512]), ('m0', [64, 256]), ('id4', [64, 256]), ('gmask', [128, 128]), ('rst64', [128, SEG]), ('rst128', [128, SEG]),
                    ('gw2', [16, 128]), ('ggb', [128, 1]), ('gnorm', [128, 256]), ('hp', [64, 40]), ('mul', [128, 4]),
                    ('w2', [64, 256]), ('a2', [64, 256]), ('g2a', [128, 256]), ('g2b', [32, 256]), ('selm', [128, 4]),
                    ('wo0', [4, 128, 8192]), ('gf0', [128, 16]), ('wg0', [11, 128, 8192]), ('wu0', [11, 128, 8192]), ('wd0', [16, 128, 5632]), ('g20', [128, 16]),
                    ('wqkv', [128, 16 * 1536]), ('tri', [128, 128]), ('maskadd', [128, 256]), ('eneg', [16, 2048]), ('abias', [128, NH * NT]),
                    ('wo1', [4, 128, 8192]), ('gf1', [128, 16]), ('wg1', [11, 128, 8192]), ('wu1', [11, 128, 8192]), ('wd1', [16, 128, 5632]), ('gfin', [128, 16])):
        X[nm] = din(nm, shp)
    X['yT'] = nc.dram_tensor('yT', [2048, 1024], F32, kind="ExternalOutput").ap()
    for nm, shp in (('gq', [128, S]), ('gk', [128, S]), ('glow', [16, S]), ('rr', [256, S + 1]), ('rk', [256, S + 1]), ('rv', [256, S + 1]),
                    ('lw', [64, S + 1]), ('la', [64, S + 1]), ('lg1', [128, S + 1]), ('lg2', [32, S + 1]), ('gv', [S, 256]), ('gog', [S, 256]),
                    ('h2s', [2048, 1024]), ('qT', [4, 128, S]), ('kT', [4, 128, S]), ('v', [4, S, 128])):
        X[nm] = scr(nm, shp)
    for ex in ('1', '3'):
        X['gin' + ex] = [[scr(f'gin{ex}_{rh}_{tq}', [256, 1024]) for tq in range(4)] for rh in range(2)]
        X['gout' + ex] = [[scr(f'gout{ex}_{rh}_{tq}', [1024, 1024]) for tq in range(4)] for rh in range(2)]
    X['gin2'] = [scr(f'gin2_{k}', [256, 1024], BF16) for k in range(8)]
    X['gout2'] = [scr(f'gout2_{k}', [1024, 1024], BF16) for k in range(8)]

    Prog.pool = SemPool(nc)
    phase_A(nc, X)
    XB = dict(X)
    for nm in ('rr', 'rk', 'rv'):
        XB[nm] = X[nm].rearrange("(h k) s -> h k s", k=64)
    build_mix(S, nc=nc, X=XB)
    XC = dict(X); XC.update(hin=X['xs'], gout=X['gout1'], wo=X['wo0'], gf=X['gf0'], wg=X['wg0'], wu=X['wu0'], wd=X['wd0'], g2=X['g20'])
    row_phase(nc, 'C', XC)
    phase_C2(nc, X)
    build_moba(nc=nc, X=X)
    XE = dict(X); XE.update(hin=X['h2s'], gout=X['gout3'], wo=X['wo1'], gf=X['gf1'], wg=X['wg1'], wu=X['wu1'], wd=X['wd1'], g2=X['gfin'])
    row_phase(nc, 'E', XE)
    Prog.pool.close()
    Prog.pool = None
    return nc


_FUSED_NC = []


def kernel(**inp):
    f32 = np.float32
    inp = {k: np.asarray(v, dtype=f32) for k, v in inp.items()}
    x = inp['x']
    c = np.ascontiguousarray
    cores = [(b, j) for b in range(2) for j in range(4)]
    w_in = inp['mix_in_w'][0]
    RB = 3088
    prm = {k: inp[k][0] for k in ['gla_gate_w2', 'gla_gate_b', 'gla_norm', 'rwkv_mu', 'rwkv_w0', 'rwkv_w2', 'rwkv_a0', 'rwkv_a2',
                                  'rwkv_g2', 'rwkv_k_k', 'rwkv_k_a', 'rwkv_r_k', 'rwkv_ln_w', 'rwkv_ln_b']}
    mc = mix_consts()
    xT = [c(x[b].T) for b in range(2)]
    wo0 = inp['mix_out_w'][0]
    perm = np.concatenate([np.concatenate([np.arange(256 * j, 256 * j + 256), np.arange(1024 + 256 * j, 1024 + 256 * j + 256)]) for j in range(4)])
    wo0p = c(wo0[perm])
    wqkv_full = inp['attn_qkv_w'][0]
    shared = {"g1": gtab(inp['norm_mix'][0]), "wo0": wgroups(wo0p, 512), "gf0": gtab(inp['norm_ffn'][0]),
              "wg0": wgroups(inp['ffn_gate_w'][0], 512), "wu0": wgroups(inp['ffn_up_w'][0], 512), "wd0": wgroups(inp['ffn_down_w'][0], 128),
              "g20": gtab(inp['norm_mix'][1]), "wo1": wgroups(inp['attn_out_w'][0], 512), "gf1": gtab(inp['norm_ffn'][1]),
              "wg1": wgroups(inp['ffn_gate_w'][1], 512), "wu1": wgroups(inp['ffn_up_w'][1], 512), "wd1": wgroups(inp['ffn_down_w'][1], 128),
              "gfin": gtab(inp['norm_final'])}
    maps = []
    for (b, j) in cores:
        d = dict(shared)
        d["xfull"] = xT[b]
        d["xs"] = c(xT[b][:, j * 1024:(j + 1) * 1024])
        cols = np.concatenate([np.arange(128 * j, 128 * j + 128), 512 + np.arange(128 * j, 128 * j + 128),
                               RB + np.arange(256 * j, 256 * j + 256), RB + 1024 + np.arange(256 * j, 256 * j + 256),
                               RB + 2048 + np.arange(256 * j, 256 * j + 256), RB + 3072 + np.arange(64), RB + 3136 + np.arange(64),
                               RB + 3200 + np.arange(128), RB + 3328 + np.arange(32), 3072 + np.arange(16)])
        w1f = np.zeros((2048, NF1), f32)
        w1f[:, :cols.size] = w_in[:, cols]
        d["w1f"] = wgroups(w1f, NF1)[0]
        d["w1t"] = wgroups(np.concatenate([w_in[:, 1024 + 256 * j:1024 + 256 * j + 256], w_in[:, 2048 + 256 * j:2048 + 256 * j + 256]], axis=1), NT1)[0]
        mi = mix_params(prm, j)
        d.update(mi)
        d.update(mc)
        selm = np.zeros((128, 4), f32); selm[:, j] = 1.0
        d["selm"] = selm
        hc = np.arange(512 * j, 512 * j + 512)
        d["wqkv"] = wgroups(np.concatenate([wqkv_full[:, hc], wqkv_full[:, 2048 + hc], wqkv_full[:, 4096 + hc]], axis=1), 1536)[0]
        mb = moba_consts(4 * j)
        for k_ in ('tri', 'maskadd', 'eneg', 'abias'):
            d[k_] = mb[k_]
        maps.append(d)
    if not _FUSED_NC:
        _FUSED_NC.append(build_fused())
    res = run_bass_kernel_spmd(_FUSED_NC[0], maps, core_ids=list(range(8))).results
    out = np.zeros(x.shape, f32)
    for i, (b, j) in enumerate(cores):
        out[b, j * 1024:(j + 1) * 1024] = res[i]["yT"].T
    return out
```
